# Optimizing a Trainium2 kernel written in Bass

```python
import math
import jax
import jax.numpy as jnp
from jax import lax
import numpy as np

D_MODEL = 2048
BATCH = 4
SEQ = 4096
DEPTH = 2

GRID_W = 64
CTX_LEN = 256
MIX = D_MODEL
N_MIXERS = 4
BR = MIX // N_MIXERS
S5_GSIZE = 16
S5_GROUPS = BR // S5_GSIZE
S5_STATE = 64
HY_ORDER = 2
HY_EMB = 33
HY_BANDS = (HY_EMB - 1) // 2
HY_FFN = 64
HY_SHORT = 3
HY_FAST_DECAY = 0.3
HY_SLOW_DECAY = 1.5
HY_TARGET = 1e-2
RET_HEADS = 4
RET_DK = BR // RET_HEADS
RET_DV = BR // RET_HEADS
RET_CHUNK = 128
GLA_HEADS = 4
GLA_KW = BR // 2
GLA_DK = GLA_KW // GLA_HEADS
GLA_DV = BR // GLA_HEADS
GLA_LR = 16
GLA_TAU = 16.0
GLA_CHUNK = 64
ROPE_BASE = 10000.0
EPS = 1e-6

SEGMENTS = (
    ('s5_u', BR), ('s5_g', BR),
    ('hy_x', 3 * BR), ('hy_g', BR),
    ('ret_q', RET_HEADS * RET_DK), ('ret_k', RET_HEADS * RET_DK), ('ret_v', BR), ('ret_g', BR),
    ('gla_q', GLA_KW), ('gla_k', GLA_KW), ('gla_v', BR), ('gla_lr', 2 * GLA_LR), ('gla_g', BR),
)
SEG_NAMES = tuple(name for name, _ in SEGMENTS)
IN_W = sum(size for _, size in SEGMENTS)
CTX_STATE_SEGMENTS = ('s5_u', 'ret_k', 'ret_v', 'gla_k', 'gla_v', 'gla_lr')

kernel_name = 'hybrid_s5_hyena_retnet_gla_prefix_dit'

F32 = jnp.float32


def _rmsnorm(x, w):
    xf = x.astype(F32)
    y = xf * lax.rsqrt(jnp.mean(xf * xf, axis=-1, keepdims=True) + EPS)
    return (y * w.astype(F32)).astype(x.dtype)


def _project(h, w_in, names):
    blocks, sizes, off = [], [], 0
    for name, size in SEGMENTS:
        if name in names:
            blocks.append(w_in[:, off:off + size])
            sizes.append((name, size))
        off += size
    w = w_in if len(blocks) == len(SEGMENTS) else jnp.concatenate(blocks, axis=1)
    p = h @ w
    out, off = {}, 0
    for name, size in sizes:
        out[name] = p[..., off:off + size]
        off += size
    return out


def _heads(t, n):
    b, l, _ = t.shape
    return t.astype(F32).reshape(b, l, n, -1).transpose(0, 2, 1, 3)


def _merge_norm(o, w):
    o = o * lax.rsqrt(jnp.mean(o * o, axis=-1, keepdims=True) + EPS)
    b, h, l, d = o.shape
    return o.transpose(0, 2, 1, 3).reshape(b, l, h * d) * w.astype(F32)


def _rope(x, ang):
    cos, sin = jnp.cos(ang), jnp.sin(ang)
    x1, x2 = jnp.split(x, 2, axis=-1)
    return jnp.concatenate([x1 * cos - x2 * sin, x1 * sin + x2 * cos], axis=-1)


def _latent_angles(rows):
    half = RET_DK // 4
    inv = ROPE_BASE ** (-jnp.arange(half, dtype=F32) / half)
    r = jnp.repeat(jnp.arange(rows, dtype=F32), GRID_W)
    cl = jnp.tile(jnp.arange(GRID_W, dtype=F32), rows)
    return jnp.concatenate([r[:, None] * inv, cl[:, None] * inv], axis=-1)


def _ctx_angles(n_ctx):
    n = RET_DK // 2
    inv = ROPE_BASE ** (-jnp.arange(n, dtype=F32) / n)
    return jnp.arange(n_ctx, dtype=F32)[:, None] * inv


def _s5_discretize(a_re, a_im, log_dt):
    a_re, a_im = a_re.astype(F32), a_im.astype(F32)
    dt = jnp.exp(log_dt.astype(F32))[:, None]
    mag = jnp.exp(a_re * dt)
    ab_re, ab_im = mag * jnp.cos(a_im * dt), mag * jnp.sin(a_im * dt)
    den = a_re * a_re + a_im * a_im
    nr = ab_re - 1.0
    co_re = (nr * a_re + ab_im * a_im) / den
    co_im = (ab_im * a_re - nr * a_im) / den
    return ab_re, ab_im, co_re, co_im


def _cplx_combine(e1, e2):
    ar1, ai1, br1, bi1 = e1
    ar2, ai2, br2, bi2 = e2
    return (ar2 * ar1 - ai2 * ai1, ar2 * ai1 + ai2 * ar1,
            ar2 * br1 - ai2 * bi1 + br2, ar2 * bi1 + ai2 * br1 + bi2)


def _s5_scan(u, h0, disc, b_re, b_im):
    ab_re, ab_im, co_re, co_im = disc
    r_re = jnp.einsum('blgi,gpi->lbgp', u, b_re.astype(F32))
    r_im = jnp.einsum('blgi,gpi->lbgp', u, b_im.astype(F32))
    bu_re = co_re * r_re - co_im * r_im
    bu_im = co_re * r_im + co_im * r_re
    if h0 is not None:
        h0_re, h0_im = h0
        bu_re = bu_re.at[0].add(ab_re * h0_re - ab_im * h0_im)
        bu_im = bu_im.at[0].add(ab_re * h0_im + ab_im * h0_re)
    L = u.shape[1]
    a_re = jnp.broadcast_to(ab_re, (L, 1) + ab_re.shape)
    a_im = jnp.broadcast_to(ab_im, (L, 1) + ab_im.shape)
    _, _, h_re, h_im = lax.associative_scan(_cplx_combine, (a_re, a_im, bu_re, bu_im), axis=0)
    return h_re, h_im


def _s5_readout(h, c_re, c_im):
    h_re, h_im = h
    return (jnp.einsum('lbgp,gip->blgi', h_re, c_re)
            - jnp.einsum('lbgp,gip->blgi', h_im, c_im))


def _s5_mixer(u_c, u_l, a_re, a_im, log_dt, b_re, b_im, c_re, c_im, d_skip,
              glu_w, glu_b, ctx_out):
    bsz, l_lat, _ = u_l.shape
    l_ctx = u_c.shape[1]
    uc = u_c.astype(F32).reshape(bsz, l_ctx, S5_GROUPS, S5_GSIZE)
    ul = u_l.astype(F32).reshape(bsz, l_lat, S5_GROUPS, S5_GSIZE)
    dsk = d_skip.astype(F32).reshape(S5_GROUPS, S5_GSIZE)
    y_l = ul * dsk
    y_c = uc * dsk if ctx_out else None
    for dr in range(2):
        disc = _s5_discretize(a_re[dr], a_im[dr], log_dt[dr])
        cr, ci = c_re[dr].astype(F32), c_im[dr].astype(F32)
        flip = (lambda t: t[:, ::-1]) if dr == 1 else (lambda t: t)
        h_c = _s5_scan(flip(uc), None, disc, b_re[dr], b_im[dr])
        h_l = _s5_scan(flip(ul), (h_c[0][-1], h_c[1][-1]), disc, b_re[dr], b_im[dr])
        y_l = y_l + flip(_s5_readout(h_l, cr, ci))
        if ctx_out:
            y_c = y_c + flip(_s5_readout(h_c, cr, ci))

    def glu(y):
        g = jax.nn.gelu(y.reshape(y.shape[0], y.shape[1], BR))
        return g * jax.nn.sigmoid(g @ glu_w.astype(F32) + glu_b.astype(F32))

    return (glu(y_c) if ctx_out else None), glu(y_l)


def _short_conv(t, w, b):
    ch = t.shape[-1]
    y = lax.conv_general_dilated(
        t, w[:, None, :].astype(t.dtype), window_strides=(1,),
        padding=[(HY_SHORT // 2, HY_SHORT // 2)],
        dimension_numbers=('NWC', 'WIO', 'NWC'), feature_group_count=ch)
    return y + b.astype(t.dtype)


def _hyena_filters(L, w1, b1, f1, w2, b2, f2, w3, b3, f3, w4):
    t = jnp.linspace(0.0, 1.0, L, dtype=F32)[:, None]
    wpos = 2.0 * math.pi * jnp.arange(L, dtype=F32)[:, None] / L
    f = jnp.linspace(1e-4, HY_BANDS - 1, HY_BANDS, dtype=F32)[None, :]
    z = jnp.concatenate([t, jnp.cos(f * wpos), -jnp.sin(f * wpos)], axis=-1)
    h = jnp.sin(f1 * (z @ w1 + b1))
    h = jnp.sin(f2 * (h @ w2 + b2))
    h = jnp.sin(f3 * (h @ w3 + b3))
    h = (h @ w4).reshape(L, 2, HY_ORDER, BR)
    deltas = jnp.linspace(math.log(HY_TARGET) / HY_SLOW_DECAY,
                          math.log(HY_TARGET) / HY_FAST_DECAY, BR, dtype=F32)
    h = h * jnp.exp(-t * jnp.abs(deltas))[:, None, None, :]
    fwd, bwd = h[:, 0], h[:, 1]
    k = jnp.concatenate([fwd, jnp.zeros_like(fwd[:1]), bwd[:0:-1]], axis=0)
    return k * lax.rsqrt(jnp.sum(k * k, axis=0, keepdims=True) + EPS)


def _hyena_seq(xp, conv_w, conv_b, filt, bias):
    L = xp.shape[1]
    xs = _short_conv(xp, conv_w, conv_b).astype(F32)
    v, g1, g2 = jnp.split(xs, 3, axis=-1)
    kf = jnp.fft.rfft(_hyena_filters(L, *[p.astype(F32) for p in filt]), n=2 * L, axis=0)
    z = v
    for o, g in enumerate((g1, g2)):
        zf = jnp.fft.rfft(z, n=2 * L, axis=1)
        conv = jnp.fft.irfft(zf * kf[None, :, o], n=2 * L, axis=1)[:, :L]
        z = g * (conv + bias[o].astype(F32) * z)
    return z


def _retention_dir(q, k, v, s0, log_g, with_output):
    bsz, nh, L, dk = k.shape
    dv = v.shape[-1]
    C = min(RET_CHUNK, L)
    n = L // C
    k = k.reshape(bsz, nh, n, C, dk)
    v = v.reshape(bsz, nh, n, C, dv)
    idx = jnp.arange(C, dtype=F32)
    lg = log_g[:, None]
    kv = jnp.einsum('bhncd,bhnce->bhnde',
                    k * jnp.exp((C - 1 - idx) * lg)[None, :, None, :, None], v)
    decay_c = jnp.exp(C * log_g)[None, :, None, None]
    s_init = jnp.zeros((bsz, nh, dk, dv), F32) if s0 is None else s0

    def step(s, kv_c):
        return decay_c * s + kv_c, s

    s_fin, s_prev = lax.scan(step, s_init, jnp.moveaxis(kv, 2, 0))
    if not with_output:
        return None, s_fin
    s_prev = jnp.moveaxis(s_prev, 0, 2)
    q = q.reshape(bsz, nh, n, C, dk)
    rel = idx[:, None] - idx[None, :]
    dmask = jnp.where(rel >= 0, jnp.exp(jnp.maximum(rel, 0.0)[None] * log_g[:, None, None]), 0.0)
    att = jnp.einsum('bhncd,bhnjd->bhncj', q, k) * dmask[None, :, None]
    o = jnp.einsum('bhncj,bhnje->bhnce', att, v)
    o = o + jnp.einsum('bhncd,bhnde->bhnce',
                       q * jnp.exp((idx + 1.0) * lg)[None, :, None, :, None], s_prev)
    return o.reshape(bsz, nh, L, dv), s_fin


def _retention_mixer(pc, pl, ang_c, ang_l, norm_w, ctx_out):
    log_g = jnp.log(1.0 - 2.0 ** (-5.0 - jnp.arange(RET_HEADS, dtype=F32)))

    def prep(p, ang, with_q):
        k = _rope(_heads(p['ret_k'], RET_HEADS), ang) * RET_DK ** -0.5
        v = _heads(p['ret_v'], RET_HEADS)
        q = _rope(_heads(p['ret_q'], RET_HEADS), ang) if with_q else None
        return q, k, v

    rev = lambda t: None if t is None else t[:, :, ::-1]
    qc, kc, vc = prep(pc, ang_c, ctx_out)
    ql, kl, vl = prep(pl, ang_l, True)
    oc_f, sc_f = _retention_dir(qc, kc, vc, None, log_g, ctx_out)
    oc_b, sc_b = _retention_dir(rev(qc), rev(kc), rev(vc), None, log_g, ctx_out)
    ol_f, _ = _retention_dir(ql, kl, vl, sc_f, log_g, True)
    ol_b, _ = _retention_dir(rev(ql), rev(kl), rev(vl), sc_b, log_g, True)
    y_l = _merge_norm(ol_f + rev(ol_b), norm_w)
    y_c = _merge_norm(oc_f + rev(oc_b), norm_w) if ctx_out else None
    return y_c, y_l


def _gla_dir(q, k, v, log_a, s0, with_output):
    bsz, nh, L, dk = k.shape
    dv = v.shape[-1]
    C = min(GLA_CHUNK, L)
    n = L // C
    r = lambda t: t.reshape(bsz, nh, n, C, t.shape[-1])
    k, v, log_a = r(k), r(v), r(log_a)
    b = jnp.cumsum(log_a, axis=3)
    b_last = b[:, :, :, -1:]
    kv = jnp.einsum('bhncd,bhnce->bhnde', k * jnp.exp(b_last - b), v)
    s_init = jnp.zeros((bsz, nh, dk, dv), F32) if s0 is None else s0

    def step(s, inp):
        dec, kv_c = inp
        return dec[..., None] * s + kv_c, s

    s_fin, s_prev = lax.scan(step, s_init, (jnp.moveaxis(jnp.exp(b_last[:, :, :, 0]), 2, 0),
                                            jnp.moveaxis(kv, 2, 0)))
    if not with_output:
        return None, s_fin
    s_prev = jnp.moveaxis(s_prev, 0, 2)
    q = r(q)
    ref = b[:, :, :, C // 2 - 1:C // 2]
    att = jnp.einsum('bhncd,bhnjd->bhncj', q * jnp.exp(b - ref), k * jnp.exp(ref - b))
    lower = jnp.tril(jnp.ones((C, C), dtype=bool))
    att = jnp.where(lower, att, 0.0)
    o = (jnp.einsum('bhncj,bhnje->bhnce', att, v)
         + jnp.einsum('bhncd,bhnde->bhnce', q * jnp.exp(b), s_prev))
    return o.reshape(bsz, nh, L, dv), s_fin


def _gla_mixer(pc, pl, gate_w, gate_b, norm_w, ctx_out):
    def prep(p, with_q):
        k = _heads(p['gla_k'], GLA_HEADS)
        v = _heads(p['gla_v'], GLA_HEADS)
        q = _heads(p['gla_q'], GLA_HEADS) * GLA_DK ** -0.5 if with_q else None
        lr = p['gla_lr'].astype(F32)
        la = [_heads(jax.nn.log_sigmoid(lr[..., d * GLA_LR:(d + 1) * GLA_LR] @ gate_w[d].astype(F32)
                                        + gate_b[d].astype(F32)) / GLA_TAU, GLA_HEADS)
              for d in range(2)]
        return q, k, v, la

    rev = lambda t: None if t is None else t[:, :, ::-1]
    qc, kc, vc, lac = prep(pc, ctx_out)
    ql, kl, vl, lal = prep(pl, True)
    oc_f, sc_f = _gla_dir(qc, kc, vc, lac[0], None, ctx_out)
    oc_b, sc_b = _gla_dir(rev(qc), rev(kc), rev(vc), rev(lac[1]), None, ctx_out)
    ol_f, _ = _gla_dir(ql, kl, vl, lal[0], sc_f, True)
    ol_b, _ = _gla_dir(rev(ql), rev(kl), rev(vl), rev(lal[1]), sc_b, True)
    y_l = _merge_norm(ol_f + rev(ol_b), norm_w)
    y_c = _merge_norm(oc_f + rev(oc_b), norm_w) if ctx_out else None
    return y_c, y_l


def _merge_branches(p, ys, w_out):
    gated = [y * jax.nn.silu(p[g].astype(F32))
             for y, g in zip(ys, ('s5_g', 'hy_g', 'ret_g', 'gla_g'))]
    return jnp.concatenate(gated, axis=-1).astype(w_out.dtype) @ w_out


def setup_inputs(seed: int = 0) -> dict:
    key = jax.random.key(seed)
    ks = iter(jax.random.split(key, 40))
    nrm = lambda shape, scale: jax.random.normal(next(ks), shape, jnp.float32) * scale
    G, P, S = S5_GROUPS, S5_STATE, S5_GSIZE
    n_idx = jnp.arange(P, dtype=jnp.float32)
    return {
        'x': nrm((BATCH, SEQ, D_MODEL), 1.0),
        'c': nrm((BATCH, D_MODEL), 1.0),
        'ctx': nrm((BATCH, CTX_LEN, D_MODEL), 1.0),
        'c_ctx': nrm((D_MODEL,), 1.0),
        'norm_w': 1.0 + nrm((DEPTH, D_MODEL), 0.02),
        'ada_w': nrm((DEPTH, D_MODEL, 3 * D_MODEL), D_MODEL ** -0.5),
        'ada_b': nrm((DEPTH, 3 * D_MODEL), 0.02),
        'w_in': nrm((DEPTH, D_MODEL, IN_W), D_MODEL ** -0.5),
        'w_out': nrm((DEPTH, MIX, D_MODEL), MIX ** -0.5),
        's5_a_re': -0.5 + nrm((DEPTH, 2, G, P), 0.01),
        's5_a_im': math.pi * n_idx + nrm((DEPTH, 2, G, P), 0.01),
        's5_log_dt': jax.random.uniform(next(ks), (DEPTH, 2, G), jnp.float32,
                                        math.log(1e-3), math.log(1e-1)),
        's5_b_re': nrm((DEPTH, 2, G, P, S), (2 * S) ** -0.5),
        's5_b_im': nrm((DEPTH, 2, G, P, S), (2 * S) ** -0.5),
        's5_c_re': nrm((DEPTH, 2, G, S, P), 0.5),
        's5_c_im': nrm((DEPTH, 2, G, S, P), 0.5),
        's5_d': nrm((DEPTH, BR), 1.0),
        's5_glu_w': nrm((DEPTH, BR, BR), BR ** -0.5),
        's5_glu_b': nrm((DEPTH, BR), 0.02),
        'hy_conv_w': nrm((DEPTH, HY_SHORT, 3 * BR), HY_SHORT ** -0.5),
        'hy_conv_b': nrm((DEPTH, 3 * BR), 0.02),
        'hy_w1': nrm((DEPTH, HY_EMB, HY_FFN), HY_EMB ** -0.5),
        'hy_b1': nrm((DEPTH, HY_FFN), 0.1),
        'hy_f1': 1.0 + nrm((DEPTH, HY_FFN), 0.02),
        'hy_w2': nrm((DEPTH, HY_FFN, HY_FFN), HY_FFN ** -0.5),
        'hy_b2': nrm((DEPTH, HY_FFN), 0.1),
        'hy_f2': 1.0 + nrm((DEPTH, HY_FFN), 0.02),
        'hy_w3': nrm((DEPTH, HY_FFN, HY_FFN), HY_FFN ** -0.5),
        'hy_b3': nrm((DEPTH, HY_FFN), 0.1),
        'hy_f3': 1.0 + nrm((DEPTH, HY_FFN), 0.02),
        'hy_w4': nrm((DEPTH, HY_FFN, 2 * HY_ORDER * BR), HY_FFN ** -0.5),
        'hy_bias': nrm((DEPTH, HY_ORDER, BR), 0.5),
        'ret_norm_w': 1.0 + nrm((DEPTH, BR), 0.02),
        'gla_gate_w': nrm((DEPTH, 2, GLA_LR, GLA_KW), GLA_LR ** -0.5),
        'gla_gate_b': nrm((DEPTH, 2, GLA_KW), 0.1),
        'gla_norm_w': 1.0 + nrm((DEPTH, BR), 0.02),
        'final_norm_w': 1.0 + nrm((D_MODEL,), 0.02),
    }


def reference(x, c, ctx, c_ctx, norm_w, ada_w, ada_b, w_in, w_out,
              s5_a_re, s5_a_im, s5_log_dt, s5_b_re, s5_b_im, s5_c_re, s5_c_im,
              s5_d, s5_glu_w, s5_glu_b,
              hy_conv_w, hy_conv_b, hy_w1, hy_b1, hy_f1, hy_w2, hy_b2, hy_f2,
              hy_w3, hy_b3, hy_f3, hy_w4, hy_bias,
              ret_norm_w, gla_gate_w, gla_gate_b, gla_norm_w, final_norm_w):
    rows = x.shape[1] // GRID_W
    ang_l = _latent_angles(rows)
    ang_c = _ctx_angles(ctx.shape[1])
    x_l, x_c = x, ctx
    for i in range(DEPTH):
        ctx_out = i < DEPTH - 1
        mod_l = jax.nn.silu(c) @ ada_w[i] + ada_b[i]
        mod_c = jax.nn.silu(c_ctx) @ ada_w[i] + ada_b[i]
        sh_l, sc_l, gt_l = jnp.split(mod_l[:, None, :], 3, axis=-1)
        sh_c, sc_c, gt_c = jnp.split(mod_c, 3, axis=-1)
        h_l = _rmsnorm(x_l, norm_w[i]) * (1.0 + sc_l) + sh_l
        h_c = _rmsnorm(x_c, norm_w[i]) * (1.0 + sc_c) + sh_c
        pl = _project(h_l, w_in[i], SEG_NAMES)
        pc = _project(h_c, w_in[i], SEG_NAMES if ctx_out else CTX_STATE_SEGMENTS)

        s5_c, s5_l = _s5_mixer(pc['s5_u'], pl['s5_u'], s5_a_re[i], s5_a_im[i], s5_log_dt[i],
                               s5_b_re[i], s5_b_im[i], s5_c_re[i], s5_c_im[i], s5_d[i],
                               s5_glu_w[i], s5_glu_b[i], ctx_out)
        hy_filt = (hy_w1[i], hy_b1[i], hy_f1[i], hy_w2[i], hy_b2[i], hy_f2[i],
                   hy_w3[i], hy_b3[i], hy_f3[i], hy_w4[i])
        hy_l = _hyena_seq(pl['hy_x'], hy_conv_w[i], hy_conv_b[i], hy_filt, hy_bias[i])
        ret_c, ret_l = _retention_mixer(pc, pl, ang_c, ang_l, ret_norm_w[i], ctx_out)
        gla_c, gla_l = _gla_mixer(pc, pl, gla_gate_w[i], gla_gate_b[i], gla_norm_w[i], ctx_out)

        x_l = x_l + gt_l * _merge_branches(pl, (s5_l, hy_l, ret_l, gla_l), w_out[i])
        if ctx_out:
            hy_c = _hyena_seq(pc['hy_x'], hy_conv_w[i], hy_conv_b[i], hy_filt, hy_bias[i])
            x_c = x_c + gt_c * _merge_branches(pc, (s5_c, hy_c, ret_c, gla_c), w_out[i])
    return _rmsnorm(x_l, final_norm_w)
```

```python
import math
import numpy as np
import ml_dtypes
import concourse.bass as bass
import concourse.mybir as mybir
from concourse.bass_utils import run_bass_kernel_spmd
from contextlib import ExitStack

F32 = mybir.dt.float32
BF16 = mybir.dt.bfloat16
ALU = mybir.AluOpType
AF = mybir.ActivationFunctionType

D = 2048
BR = 512
IN_W = 6688
DEPTH = 2
EPS = 1e-6
NK = D // 128
SAME_ENGINE_SYNC = True

SEG = {}
_off = 0
for _n, _s in (('s5_u', 512), ('s5_g', 512), ('hy_x', 1536), ('hy_g', 512), ('ret_q', 512), ('ret_k', 512),
               ('ret_v', 512), ('ret_g', 512), ('gla_q', 256), ('gla_k', 256), ('gla_v', 512), ('gla_lr', 32),
               ('gla_g', 512)):
    SEG[_n] = (_off, _s)
    _off += _s


class Buf:
    __slots__ = ("t", "w", "r", "name")

    def __init__(self, t, name=""):
        self.t = t
        self.w = None
        self.r = []
        self.name = name

    def __getitem__(self, idx):
        return self.t[idx]


class K:
    def __init__(self, nc, es, n_dma_sems=32):
        self.nc = nc
        self.eng = {"pe": nc.tensor, "act": nc.scalar, "dve": nc.vector, "pool": nc.gpsimd, "sp": nc.sync}
        self.sem, self.cnt, self.seen = {}, {}, {}
        for e in self.eng:
            self.sem[e] = es.enter_context(nc.semaphore("s_" + e))
            self.cnt[e] = 0
            self.seen[e] = {}
        self.dsem = [es.enter_context(nc.semaphore("d%d" % i)) for i in range(n_dma_sems)]
        self.dval = [0] * n_dma_sems
        self.dnext = 0
        self.pending_dma = []
        self.rr = 0

    def _wait(self, e, tok):
        if tok is None:
            return
        key, sh, val = tok
        if key == ("e", e) and (not SAME_ENGINE_SYNC or e in ("pe", "sp")):
            return
        if self.seen[e].get(key, 0) >= val:
            return
        self.eng[e].wait_ge(sh, val)
        self.seen[e][key] = val

    def _deps(self, e, r, w):
        for b in r:
            self._wait(e, b.w)
        for b in w:
            self._wait(e, b.w)
            for t in b.r:
                self._wait(e, t)

    def _mark(self, tok, r, w):
        for b in r:
            b.r.append(tok)
            if len(b.r) > 64:
                b.r = b.r[-48:]
        for b in w:
            b.w = tok
            b.r = []

    def op(self, e, fn, r=(), w=()):
        self._deps(e, r, w)
        inst = fn(self.eng[e])
        self.cnt[e] += 1
        inst.then_inc(self.sem[e], 1)
        tok = (("e", e), self.sem[e], self.cnt[e])
        self._mark(tok, r, w)
        return tok

    def dma(self, out, in_, r=(), w=(), q="sp"):
        i = self.dnext
        self.dnext = (self.dnext + 1) % len(self.dsem)
        key = ("d", i)
        if self.dval[i] > 0:
            self._wait(q, (key, self.dsem[i], self.dval[i]))
        self._deps(q, r, w)
        self.dval[i] += 16
        inst = self.eng[q].dma_start(out=out, in_=in_)
        inst.then_inc(self.dsem[i], 16)
        tok = (key, self.dsem[i], self.dval[i])
        self._mark(tok, r, w)
        self.pending_dma.append(tok)
        return tok

    def barrier(self):
        toks = [(("e", e), self.sem[e], self.cnt[e]) for e in self.eng if self.cnt[e] > 0]
        last = {}
        for t in self.pending_dma:
            last[t[0]] = t
        toks += list(last.values())
        for e in self.eng:
            for t in toks:
                if t[0] == ("e", e):
                    continue
                self._wait(e, t)
        self.pending_dma = []

    def ev(self):
        self.rr += 1
        return ("dve", "act")[self.rr % 2]


class Ctx:
    def __init__(self, nc, k):
        self.nc, self.k = nc, k
        self.n = 0

    def phase(self):
        return Phase(self)


class Phase:
    def __init__(self, c):
        self.c = c
        self.es = ExitStack()

    def __enter__(self):
        self.es.__enter__()
        return self

    def sb(self, shape, dt=F32, name=None):
        self.c.n += 1
        return Buf(self.es.enter_context(self.c.nc.sbuf_tensor("sb%d_%s" % (self.c.n, name or "t"), list(shape), dt)), name)

    def ps(self, shape, dt=F32, name=None):
        self.c.n += 1
        return Buf(self.es.enter_context(self.c.nc.psum_tensor("ps%d_%s" % (self.c.n, name or "p"), list(shape), dt)), name)

    def __exit__(self, *a):
        self.c.k.barrier()
        return self.es.__exit__(*a)


FM = {}
_o = 0
for _n, _s in (('s5_u', 512), ('s5_g', 512), ('hy_x', 1536), ('hy_g', 512), ('ret_q', 512), ('ret_qs', 512),
               ('ret_k', 512), ('ret_ks', 512), ('ret_g', 512), ('gla_q', 256), ('gla_k', 256), ('gla_lr', 32),
               ('gla_g', 512)):
    FM[_n] = (_o, _s)
    _o += _s
NFM = _o


def fm_tiles():
    out = []
    for name, (r0, sz) in FM.items():
        swap = name in ('ret_qs', 'ret_ks')
        src0 = SEG[name[:-1] if swap else name][0]
        for t in range(0, sz, 128):
            n = min(128, sz - t)
            if swap:
                pieces = [(src0 + t + 64, 64), (src0 + t, 64)]
            else:
                pieces = [(src0 + t, n)]
            out.append((r0 + t, n, pieces))
    return out


class G:
    pass


def build(L=4096, CTX=256, nlayers=DEPTH, debug=False, stop_after=None, only=None, skip=(), ext_p=False, cut=None):
    T = CTX + L
    NTB = T // 128
    nc = bass.Bass("TRN2", target_bir_lowering=False)
    g = G()
    g.nc, g.L, g.CTX, g.T = nc, L, CTX, T

    in_names = []

    def din(name, shape, dt=F32):
        in_names.append(name)
        return nc.dram_tensor(name, list(shape), dt, kind="ExternalInput").ap()

    def dscr(name, shape, dt=F32):
        return nc.dram_tensor(name, list(shape), dt, kind=("ExternalOutput" if debug else "Internal")).ap()

    I = {}
    I['x'] = din('x', [L, D])
    I['ctx'] = din('ctx', [CTX, D])
    I['cc'] = din('cc', [128, NK, 2])
    for nm, shp in (('norm_w', [DEPTH, D]), ('ada_w', [DEPTH, D, 3 * D]), ('ada_b', [DEPTH, 3 * D]),
                    ('w_in', [DEPTH, D, IN_W]), ('w_out', [DEPTH, D, D]), ('final_norm_w', [1, D])):
        if ext_p and nm in ('ada_w', 'w_in', 'w_out'):
            continue
        I[nm] = din(nm, shp)
    g.in_names = None
    I['ident_bf'] = din('ident_bf', [128, 128], BF16)
    I['ident_f'] = din('ident_f', [128, 128], F32)
    out = nc.dram_tensor("out", [L, D], F32, kind="ExternalOutput").ap()
    S = {}
    S['mod'] = dscr('s_mod', [2, 3 * D])
    S['wb'] = dscr('s_wb', [D, IN_W], BF16)
    S['wob'] = dscr('s_wob', [D, D], BF16)
    S['pfm'] = din('s_pfm', [NFM, T]) if ext_p else dscr('s_pfm', [NFM, T])
    S['ptm'] = din('s_ptm', [T, 1024]) if ext_p else dscr('s_ptm', [T, 1024])
    g.cut = cut
    S['y'] = dscr('s_y', [D, T], BF16)
    S['x1'] = dscr('s_x1', [L, D])
    S['xc1'] = dscr('s_xc1', [CTX, D])
    S['ys5'] = dscr('s_ys5', [512, T])
    I['tvals'] = din('tvals', [128, T])
    I['s5_are'] = din('s5_are', [DEPTH, 128, 32]); I['s5_aim'] = din('s5_aim', [DEPTH, 128, 32]); I['s5_ldt'] = din('s5_ldt', [DEPTH, 128, 32])
    I['s5_d32'] = din('s5_d32', [DEPTH, 32, 16])
    I['s5_Bre'] = din('s5_Bre', [DEPTH, 2, 16, 32, 128]); I['s5_Bim'] = din('s5_Bim', [DEPTH, 2, 16, 32, 128])
    I['s5_Cre'] = din('s5_Cre', [DEPTH, 2, 16, 128, 32]); I['s5_Cim'] = din('s5_Cim', [DEPTH, 2, 16, 128, 32])
    I['m01_128'] = din('m01_128', [128, T + 1]); I['m01_64'] = din('m01_64', [128, T + 1])
    I['maskF'] = din('maskF', [128, 128]); I['maskB'] = din('maskB', [128, 128])
    I['ropeC'] = din('ropeC', [128, T]); I['ropeS'] = din('ropeS', [128, T])
    I['ret_nw'] = din('ret_nw', [DEPTH, 128, 4]); I['gla_nw'] = din('gla_nw', [DEPTH, 128, 4])
    I['gla_gw'] = din('gla_gw', [DEPTH, 2, 32, 2, 128]); I['gla_gb'] = din('gla_gb', [DEPTH, 2, 128, 2])
    for tg_, Lh_ in (('C', CTX), ('L', L)):
        N_ = 2 * Lh_
        I['hy_z' + tg_] = din('hy_z' + tg_, [33, N_]); I['hy_dec' + tg_] = din('hy_dec' + tg_, [N_, 512])
        for ri_ in ('re', 'im'):
            I['hy_K' + ri_ + tg_] = din('hy_K' + ri_ + tg_, [Lh_ // 128, 128, N_ // 128, 128], BF16)
            I['hy_I' + ri_ + tg_] = din('hy_I' + ri_ + tg_, [Lh_ // 128, 128, Lh_ // 128, 128], BF16)
    I['hy_w1'] = din('hy_w1', [DEPTH, 33, 64]); I['hy_w2'] = din('hy_w2', [DEPTH, 64, 64]); I['hy_w3'] = din('hy_w3', [DEPTH, 64, 64]); I['hy_w4'] = din('hy_w4', [DEPTH, 64, 2048])
    I['hy_bf'] = din('hy_bf', [DEPTH, 64, 6]); I['hy_cw'] = din('hy_cw', [DEPTH, 128, 12, 4]); I['hy_bias4'] = din('hy_bias4', [DEPTH, 128, 4, 2])
    S['hk'] = dscr('s_hk', [2 * L, 1024], BF16); S['hrs'] = dscr('s_hrs', [128, 1024]); S['hKf'] = dscr('s_hKf', [2, 2, L, 512])
    S['hxs'] = dscr('s_hxs', [1536, L]); S['hz1'] = dscr('s_hz1', [512, L]); S['hY'] = dscr('s_hY', [L // 128, 2, 128, 512], BF16)
    I['s5_glu_w'] = din('s5_glu_w', [DEPTH, 512, 512]); I['s5_glu_b4'] = din('s5_glu_b4', [DEPTH, 128, 4])
    g.I, g.S, g.out = I, S, out

    with ExitStack() as es:
        k = K(nc, es)
        c = Ctx(nc, k)
        g.k, g.c = k, c
        for layer in range(nlayers):
            if not ext_p:
                phase_mod(g, layer)
                phase_wcast(g, layer)
                phase_inproj(g, layer)
            if stop_after == 'inproj':
                break
            if 's5' not in skip:
                phase_s5(g, layer)
            if stop_after == 's5':
                break
            if only in (None, 'ret') and 'attn' not in skip:
                phase_ret(g, layer)
            if only in (None, 'gla') and 'attn' not in skip:
                phase_gla(g, layer)
            if stop_after == 'attn':
                break
            if 'hy' not in skip:
                if layer < DEPTH - 1:
                    phase_hyena(g, layer, CTX, 0, 'C')
                phase_hyena(g, layer, L, CTX, 'L')
            if stop_after == 'hy':
                break
            phase_outproj(g, layer, layer == nlayers - 1)
        k.barrier()
    nc._in_names = in_names
    return nc


def phase_mod(g, i):
    k, I, S = g.k, g.I, g.S
    with g.c.phase() as ph:
        cc = ph.sb([128, NK, 2], F32, "cc")
        s2 = ph.sb([128, NK, 2], F32, "s2")
        k.dma(cc[:], I['cc'][:, :, :], w=[cc])
        k.op("act", lambda e: e.activation(out=s2[:], in_=cc[:], func=AF.Silu), r=[cc], w=[s2])
        ab = ph.sb([2, 3 * D], F32, "ab")
        k.dma(ab[0:1, :], I['ada_b'][i:i + 1, :], w=[ab])
        k.dma(ab[1:2, :], I['ada_b'][i:i + 1, :], w=[ab])
        mo = ph.sb([2, 3 * D], F32, "mo")
        HALF = 3 * D // 2
        wts = [ph.sb([128, HALF], F32, "aw%d" % j) for j in range(2)]
        pss = [ph.ps([2, 512], F32, "mps%d" % j) for j in range(HALF // 512)]
        aw = I['ada_w'][i].rearrange("(k p) c -> k p c", p=128)
        n = 0
        for half in range(2):
            for kk in range(NK):
                wt = wts[n % 2]
                n += 1
                k.dma(wt[:], aw[kk, :, half * HALF:(half + 1) * HALF], w=[wt], q=("sp", "sp")[n % 2])
                for j, ps in enumerate(pss):
                    k.op("pe", lambda e, ps=ps, wt=wt, j=j, kk=kk: e.matmul(
                        ps[:], lhsT=s2[:, kk, :], rhs=wt[:, j * 512:(j + 1) * 512], start=(kk == 0), stop=(kk == NK - 1)),
                        r=[s2, wt], w=[ps])
            for j, ps in enumerate(pss):
                c0 = half * HALF + j * 512
                k.op("dve", lambda e, ps=ps, c0=c0: e.tensor_tensor(out=mo[:, c0:c0 + 512], in0=ps[:], in1=ab[:, c0:c0 + 512], op=ALU.add),
                     r=[ps, ab], w=[mo])
        k.dma(S['mod'][:, :], mo[:], r=[mo])


def phase_wcast(g, i):
    k, I, S = g.k, g.I, g.S
    with g.c.phase() as ph:
        for (src, dst, ncol) in ((I['w_in'][i], S['wb'], IN_W), (I['w_out'][i], S['wob'], D)):
            fs = [ph.sb([128, ncol], F32, "wf%d" % j) for j in range(2)]
            bs = [ph.sb([128, ncol], BF16, "wbf%d" % j) for j in range(2)]
            sv = src.rearrange("(k p) c -> k p c", p=128)
            dv = dst.rearrange("(k p) c -> k p c", p=128)
            for kk in range(NK):
                f, b = fs[kk % 2], bs[kk % 2]
                k.dma(f[:], sv[kk], w=[f], q=("sp", "sp")[kk % 2])
                e_ = ("dve", "pool", "act")[kk % 3]
                if e_ == "act":
                    k.op("act", lambda e, f=f, b=b: e.copy(out=b[:], in_=f[:]), r=[f], w=[b])
                else:
                    k.op(e_, lambda e, f=f, b=b: e.tensor_copy(out=b[:], in_=f[:]), r=[f], w=[b])
                k.dma(dv[kk], b[:], r=[b])


def load_bc(g, ph, name, src_row_ap, n):
    t = ph.sb([128, n], F32, name)
    g.k.dma(t[:], src_row_ap.partition_broadcast(128), w=[t])
    return t


def phase_inproj(g, i):
    k, I, S, L, CTX, T = g.k, g.I, g.S, g.L, g.CTX, g.T
    with g.c.phase() as ph:
        ident = ph.sb([128, 128], BF16, "ident")
        k.dma(ident[:], I['ident_bf'][:, :], w=[ident])
        nw = load_bc(g, ph, "nw", I['norm_w'][i:i + 1, :], D)
        wmod, sh = [], []
        for r in range(2):
            sc = load_bc(g, ph, "sc%d" % r, S['mod'][r:r + 1, D:2 * D], D)
            k.op("dve", lambda e, sc=sc: e.scalar_tensor_tensor(out=sc[:], in0=sc[:], scalar=1.0, in1=nw[:], op0=ALU.add, op1=ALU.mult),
                 r=[sc, nw], w=[sc])
            wmod.append(sc)
            sh.append(load_bc(g, ph, "sh%d" % r, S['mod'][r:r + 1, 0:D], D))
        TG = min(1024, T)
        hT = ph.sb([128, NK, TG], BF16, "hT")
        xs = [ph.sb([128, D], F32, "xt%d" % j) for j in range(2)]
        junk = ph.sb([128, D], BF16, "junk")
        h1 = ph.sb([128, D], F32, "h1")
        hb = ph.sb([128, D], BF16, "hb")
        ss = ph.sb([128, 1], F32, "ss")
        epsb = ph.sb([128, 1], F32, "epsb")
        k.op("pool", lambda e: e.memset(epsb[:], EPS), w=[epsb])
        pT = [ph.ps([128, 1024], BF16, "pT%d" % j) for j in range(2)]
        pso = [ph.ps([128, 512], F32, "pso%d" % j) for j in range(4)]
        wts = [ph.sb([128, NK, 128], BF16, "wt%d" % j) for j in range(3)]
        wvs = ph.sb([128, NK, 512], BF16, "wv")
        ots = [ph.sb([128, 512], F32, "ot%d" % j) for j in range(4)]
        wbv = S['wb'].rearrange("(k p) c -> p k c", p=128)
        tiles = fm_tiles()
        nx = 0
        no = 0
        for t0 in range(0, T, TG):
            tg = min(TG, T - t0)
            for tb in range(tg // 128):
                tok = t0 + tb * 128
                isctx = tok < CTX
                src = I['ctx'][tok:tok + 128, :] if isctx else I['x'][tok - CTX:tok - CTX + 128, :]
                if i > 0:
                    src = S['xc1'][tok:tok + 128, :] if isctx else S['x1'][tok - CTX:tok - CTX + 128, :]
                x_ = xs[nx % 2]
                nx += 1
                k.dma(x_[:], src, w=[x_], q=("sp", "sp")[nx % 2])
                k.op("act", lambda e, x_=x_: e.activation(out=junk[:], in_=x_[:], func=AF.Square, accum_out=ss[:]), r=[x_], w=[junk, ss])
                rsqrt_inplace(k, ss[:], ss, epsb[:, 0:1], epsb, scale=1.0 / D)
                r_ = 1 if isctx else 0
                k.op("dve", lambda e, x_=x_, r_=r_: e.scalar_tensor_tensor(out=h1[:], in0=x_[:], scalar=ss[:, 0:1], in1=wmod[r_][:], op0=ALU.mult, op1=ALU.mult),
                     r=[x_, ss, wmod[r_]], w=[h1])
                k.op("pool", lambda e, r_=r_: e.tensor_tensor(out=hb[:], in0=h1[:], in1=sh[r_][:], op=ALU.add), r=[h1, sh[r_]], w=[hb])
                for half in range(2):
                    p_ = pT[half]
                    for j in range(8):
                        kk = half * 8 + j
                        k.op("pe", lambda e, p_=p_, j=j, kk=kk: e.transpose(out=p_[:, j * 128:(j + 1) * 128], in_=hb[:, kk * 128:(kk + 1) * 128], identity=ident[:]),
                             r=[hb, ident], w=[p_])
                    ee = k.ev()
                    dst = hT[:, half * 8:half * 8 + 8, tb * 128:(tb + 1) * 128]
                    srcp = p_[:].rearrange("p (a b) -> p a b", a=8)
                    if ee == "act":
                        k.op("act", lambda e, dst=dst, srcp=srcp: e.copy(out=dst, in_=srcp), r=[p_], w=[hT])
                    else:
                        k.op("dve", lambda e, dst=dst, srcp=srcp: e.tensor_copy(out=dst, in_=srcp), r=[p_], w=[hT])
            for ti, (r0, nr, pieces) in enumerate(tiles):
                wt = wts[ti % 3]
                c_ = 0
                for (s0, n_) in pieces:
                    k.dma(wt[:, :, c_:c_ + n_], wbv[:, :, s0:s0 + n_], w=[wt], q=("sp", "sp")[ti % 2])
                    c_ += n_
                for n0 in range(0, tg, 512):
                    nn = min(512, tg - n0)
                    ps = pso[no % 4]
                    ot = ots[no % 4]
                    no += 1
                    for kk in range(NK):
                        k.op("pe", lambda e, ps=ps, wt=wt, kk=kk, nr=nr, n0=n0, nn=nn: e.matmul(
                            ps[0:nr, 0:nn], lhsT=wt[:, kk, 0:nr], rhs=hT[:, kk, n0:n0 + nn], start=(kk == 0), stop=(kk == NK - 1)),
                            r=[wt, hT], w=[ps])
                    ee = k.ev()
                    if ee == "act":
                        k.op("act", lambda e, ps=ps, ot=ot, nr=nr, nn=nn: e.copy(out=ot[0:nr, 0:nn], in_=ps[0:nr, 0:nn]), r=[ps], w=[ot])
                    else:
                        k.op("dve", lambda e, ps=ps, ot=ot, nr=nr, nn=nn: e.tensor_copy(out=ot[0:nr, 0:nn], in_=ps[0:nr, 0:nn]), r=[ps], w=[ot])
                    k.dma(S['pfm'][r0:r0 + nr, t0 + n0:t0 + n0 + nn], ot[0:nr, 0:nn], r=[ot], q=("sp", "sp")[no % 2])
            for vi, nm in enumerate(('ret_v', 'gla_v')):
                s0 = SEG[nm][0]
                k.dma(wvs[:], wbv[:, :, s0:s0 + 512], w=[wvs])
                for tb in range(tg // 128):
                    ps = pso[no % 4]
                    ot = ots[no % 4]
                    no += 1
                    for kk in range(NK):
                        k.op("pe", lambda e, ps=ps, kk=kk, tb=tb: e.matmul(
                            ps[:, :], lhsT=hT[:, kk, tb * 128:(tb + 1) * 128], rhs=wvs[:, kk, :], start=(kk == 0), stop=(kk == NK - 1)),
                            r=[wvs, hT], w=[ps])
                    ee = k.ev()
                    if ee == "act":
                        k.op("act", lambda e, ps=ps, ot=ot: e.copy(out=ot[:], in_=ps[:]), r=[ps], w=[ot])
                    else:
                        k.op("dve", lambda e, ps=ps, ot=ot: e.tensor_copy(out=ot[:], in_=ps[:]), r=[ps], w=[ot])
                    tok = t0 + tb * 128
                    k.dma(S['ptm'][tok:tok + 128, vi * 512:(vi + 1) * 512], ot[:], r=[ot], q=("sp", "sp")[no % 2])


TWO_PI = 2.0 * math.pi


def tt(k, e_, out, a, b, op, r, w):
    return k.op(e_, lambda e: e.tensor_tensor(out=out, in0=a, in1=b, op=op), r=r, w=w)


def ts(k, e_, out, a, s1, s2, op0, op1, r, w):
    if op1 is None:
        return k.op(e_, lambda e: e.tensor_scalar(out=out, in0=a, scalar1=s1, scalar2=None, op0=op0), r=r, w=w)
    return k.op(e_, lambda e: e.tensor_scalar(out=out, in0=a, scalar1=s1, scalar2=s2, op0=op0, op1=op1), r=r, w=w)


def act(k, out, a, func, r, w, bias=None, scale=None):
    kw = {}
    if bias is not None:
        kw['bias'] = bias
    if scale is not None:
        kw['scale'] = scale
    return k.op("act", lambda e: e.activation(out=out, in_=a, func=func, **kw), r=r, w=w)


def frac_inplace(k, X, Ti, r, eng_cast="pool"):
    cp(k, eng_cast, Ti, X, [r[0]], [r[1]])
    tt(k, "pool", X, X, Ti, ALU.subtract, [r[0], r[1]], [r[0]])
    k.op("dve", lambda e: e.scalar_tensor_tensor(out=X, in0=X, scalar=0.0, in1=X, op0=ALU.is_lt, op1=ALU.add), r=[r[0]], w=[r[0]])


def rsqrt_inplace(k, X, r, bias_ap, rb, scale=1.0):
    k.op("act", lambda e: e.activation(out=X, in_=X, func=AF.Sqrt, bias=bias_ap, scale=scale), r=[r, rb], w=[r])
    k.op("dve", lambda e: e.reciprocal(out=X, in_=X), r=[r], w=[r])


def frac_pos_OLD(k, ph, x, name):
    n = x.t.shape[1]
    m = ph.sb([128, n], F32, name + "_m")
    ts(k, "pool", x[:], x[:], 1.0, None, ALU.mod, None, [x], [x])
    ts(k, "dve", m[:], x[:], 0.0, None, ALU.is_lt, None, [x], [m])
    tt(k, "dve", x[:], x[:], m[:], ALU.add, [x, m], [x])


def phase_s5(g, i):
    k, I, S, L, CTX, T = g.k, g.I, g.S, g.L, g.CTX, g.T
    u0 = FM['s5_u'][0]
    with g.c.phase() as ph:
        NS = 32
        are = ph.sb([128, NS], F32, "are"); aim = ph.sb([128, NS], F32, "aim"); dtt = ph.sb([128, NS], F32, "dtt")
        k.dma(are[:], I['s5_are'][i], w=[are]); k.dma(aim[:], I['s5_aim'][i], w=[aim]); k.dma(dtt[:], I['s5_ldt'][i], w=[dtt])
        negpi = ph.sb([128, 1], F32, "negpi")
        k.op("pool", lambda e: e.memset(negpi[:], -math.pi), w=[negpi])
        neghalf = ph.sb([128, 1], F32, "neghalf"); halfpi = ph.sb([128, 1], F32, "halfpi")
        k.op("pool", lambda e: e.memset(neghalf[:], -0.5), w=[neghalf])
        k.op("pool", lambda e: e.memset(halfpi[:], 0.5 * math.pi), w=[halfpi])
        act(k, dtt[:], dtt[:], AF.Exp, [dtt], [dtt])
        rho = ph.sb([128, NS], F32, "rho")
        tt(k, "dve", rho[:], are[:], dtt[:], ALU.mult, [are, dtt], [rho])
        act(k, rho[:], rho[:], AF.Exp, [rho], [rho])
        phi = ph.sb([128, NS], F32, "phi")
        tt(k, "dve", phi[:], aim[:], dtt[:], ALU.mult, [aim, dtt], [phi])
        ts(k, "dve", phi[:], phi[:], 1.0 / TWO_PI, None, ALU.mult, None, [phi], [phi])
        phii = ph.sb([128, NS], mybir.dt.int32, "phii")
        frac_inplace(k, phi[:], phii[:], [phi, phii])
        ph2 = ph.sb([128, NS], F32, "ph2")
        nsin = ph.sb([128, NS], F32, "nsin"); ncos = ph.sb([128, NS], F32, "ncos")
        act(k, nsin[:], phi[:], AF.Sin, [phi, negpi], [nsin], bias=negpi[:, 0:1], scale=TWO_PI)
        act(k, ph2[:], phi[:], AF.Abs, [phi, neghalf], [ph2], bias=neghalf[:, 0:1])
        act(k, ncos[:], ph2[:], AF.Sin, [ph2, halfpi], [ncos], bias=halfpi[:, 0:1], scale=-TWO_PI)
        abre = ph.sb([128, NS], F32, "abre"); abim = ph.sb([128, NS], F32, "abim")
        k.op("dve", lambda e: e.scalar_tensor_tensor(out=abre[:], in0=ncos[:], scalar=-1.0, in1=rho[:], op0=ALU.mult, op1=ALU.mult), r=[ncos, rho], w=[abre])
        k.op("dve", lambda e: e.scalar_tensor_tensor(out=abim[:], in0=nsin[:], scalar=-1.0, in1=rho[:], op0=ALU.mult, op1=ALU.mult), r=[nsin, rho], w=[abim])
        nr = ph.sb([128, NS], F32, "nr"); den = ph.sb([128, NS], F32, "den"); t1 = ph.sb([128, NS], F32, "t1"); t2 = ph.sb([128, NS], F32, "t2")
        core = ph.sb([128, NS], F32, "core"); coim = ph.sb([128, NS], F32, "coim")
        ts(k, "dve", nr[:], abre[:], -1.0, None, ALU.add, None, [abre], [nr])
        tt(k, "dve", den[:], are[:], are[:], ALU.mult, [are], [den])
        tt(k, "dve", t1[:], aim[:], aim[:], ALU.mult, [aim], [t1])
        tt(k, "dve", den[:], den[:], t1[:], ALU.add, [den, t1], [den])
        k.op("dve", lambda e: e.reciprocal(out=den[:], in_=den[:]), r=[den], w=[den])
        tt(k, "dve", t1[:], nr[:], are[:], ALU.mult, [nr, are], [t1])
        tt(k, "dve", t2[:], abim[:], aim[:], ALU.mult, [abim, aim], [t2])
        tt(k, "dve", t1[:], t1[:], t2[:], ALU.add, [t1, t2], [t1])
        tt(k, "dve", core[:], t1[:], den[:], ALU.mult, [t1, den], [core])
        tt(k, "dve", t1[:], abim[:], are[:], ALU.mult, [abim, are], [t1])
        tt(k, "dve", t2[:], nr[:], aim[:], ALU.mult, [nr, aim], [t2])
        tt(k, "dve", t1[:], t1[:], t2[:], ALU.subtract, [t1, t2], [t1])
        tt(k, "dve", coim[:], t1[:], den[:], ALU.mult, [t1, den], [coim])
        tv = ph.sb([128, T], F32, "tv")
        k.dma(tv[:], I['tvals'][:, 0:T], w=[tv])
        d32 = ph.sb([32, 16], F32, "d32")
        k.dma(d32[:], I['s5_d32'][i], w=[d32])
        cosT = ph.sb([128, T], F32, "cosT"); sinT = ph.sb([128, T], F32, "sinT")
        rhoT = ph.sb([128, T], F32, "rhoT")
        zre = ph.sb([128, T], F32, "zre"); zim = ph.sb([128, T], F32, "zim")
        gre = ph.sb([128, T], F32, "gre"); gim = ph.sb([128, T], F32, "gim")
        pht = gre
        hre = ph.sb([128, T], BF16, "hre"); him = ph.sb([128, T], BF16, "him")
        uf = gim; ub = ph.sb([32, T], BF16, "ub")
        yF = ph.sb([32, T], F32, "yF")
        zr = ph.sb([128, 512], F32, "zr"); zi = ph.sb([128, 512], F32, "zi")
        ta = ph.sb([128, 512], F32, "ta"); tb_ = ph.sb([128, 512], F32, "tb")
        tc_ = zre; td_ = zim
        bre_f = ph.sb([32, 128], F32, "bref"); bim_f = ph.sb([32, 128], F32, "bimf")
        bre = ph.sb([32, 128], BF16, "bre"); bim = ph.sb([32, 128], BF16, "bim")
        cre_f = ph.sb([128, 32], F32, "cref"); cim_f = ph.sb([128, 32], F32, "cimf")
        cre = ph.sb([128, 32], BF16, "cre"); cimn = ph.sb([128, 32], BF16, "cimn")
        ct1 = ph.sb([128, 32], F32, "ct1"); ct2 = ph.sb([128, 32], F32, "ct2")
        pz = [ph.ps([128, 512], F32, "pzr"), ph.ps([128, 512], F32, "pzi")]
        py = [ph.ps([32, 512], F32, "py%d" % j) for j in range(2)]
        npy = 0
        for gp in range(16):
            k.dma(uf[0:32, :], S['pfm'][u0 + gp * 32:u0 + gp * 32 + 32, :], w=[uf])
            k.op("pool", lambda e: e.tensor_copy(out=ub[:], in_=uf[0:32, :]), r=[uf], w=[ub])
            ts(k, "dve", yF[:], uf[0:32, :], d32[:, gp:gp + 1], None, ALU.mult, None, [uf, d32], [yF])
            for dr in range(2):
                col = dr * 16 + gp
                k.dma(bre_f[:], I['s5_Bre'][i, dr, gp], w=[bre_f]); k.dma(bim_f[:], I['s5_Bim'][i, dr, gp], w=[bim_f])
                k.dma(cre_f[:], I['s5_Cre'][i, dr, gp], w=[cre_f]); k.dma(cim_f[:], I['s5_Cim'][i, dr, gp], w=[cim_f])
                k.op("pool", lambda e: e.tensor_copy(out=bre[:], in_=bre_f[:]), r=[bre_f], w=[bre])
                k.op("pool", lambda e: e.tensor_copy(out=bim[:], in_=bim_f[:]), r=[bim_f], w=[bim])
                ts(k, "dve", ct1[:], cre_f[:], core[:, col:col + 1], None, ALU.mult, None, [cre_f, core], [ct1])
                ts(k, "dve", ct2[:], cim_f[:], coim[:, col:col + 1], None, ALU.mult, None, [cim_f, coim], [ct2])
                tt(k, "dve", cre[:], ct1[:], ct2[:], ALU.subtract, [ct1, ct2], [cre])
                ts(k, "dve", ct1[:], cre_f[:], coim[:, col:col + 1], None, ALU.mult, None, [cre_f, coim], [ct1])
                ts(k, "dve", ct2[:], cim_f[:], core[:, col:col + 1], None, ALU.mult, None, [cim_f, core], [ct2])
                k.op("dve", lambda e: e.scalar_tensor_tensor(out=cimn[:], in0=ct1[:], scalar=-1.0, in1=ct2[:], op0=ALU.mult, op1=ALU.subtract), r=[ct1, ct2], w=[cimn])
                ts(k, "dve", pht[:], tv[:], phi[:, col:col + 1], None, ALU.mult, None, [tv, phi], [pht])
                frac_inplace(k, pht[:], zim[:].bitcast(mybir.dt.int32), [pht, zim])
                act(k, sinT[:], pht[:], AF.Sin, [pht, negpi], [sinT], bias=negpi[:, 0:1], scale=TWO_PI)
                act(k, pht[:], pht[:], AF.Abs, [pht, neghalf], [pht], bias=neghalf[:, 0:1])
                act(k, cosT[:], pht[:], AF.Sin, [pht, halfpi], [cosT], bias=halfpi[:, 0:1], scale=-TWO_PI)
                ts(k, "dve", rhoT[:], tv[:], 0.0, rho[:, col:col + 1], ALU.mult, ALU.add, [tv, rho], [rhoT])
                blocks = []
                if dr == 0:
                    for n0 in range(0, T, 512):
                        nn = min(512, T - n0)
                        blocks.append((n0, nn, slice(n0, n0 + nn)))
                else:
                    def rs(hi, nn):
                        lo = hi - nn
                        return slice(hi - 1, lo - 1 if lo > 0 else None, -1)
                    n0 = 0
                    for c0 in range(CTX, 0, -512):
                        nn = min(512, c0)
                        blocks.append((n0, nn, rs(c0, nn))); n0 += nn
                    for c0 in range(T, CTX, -512):
                        nn = min(512, c0 - CTX)
                        blocks.append((n0, nn, rs(c0, nn))); n0 += nn
                for (n0, nn, cs) in blocks:
                    k.op("pe", lambda e, cs=cs, nn=nn: e.matmul(pz[0][:, 0:nn], lhsT=bre[:], rhs=ub[:, cs], start=True, stop=True), r=[bre, ub], w=[pz[0]])
                    k.op("pe", lambda e, cs=cs, nn=nn: e.matmul(pz[1][:, 0:nn], lhsT=bim[:], rhs=ub[:, cs], start=True, stop=True), r=[bim, ub], w=[pz[1]])
                    k.op("act", lambda e, nn=nn: e.copy(out=zr[:, 0:nn], in_=pz[0][:, 0:nn]), r=[pz[0]], w=[zr])
                    k.op("act", lambda e, nn=nn: e.copy(out=zi[:, 0:nn], in_=pz[1][:, 0:nn]), r=[pz[1]], w=[zi])
                    ns = slice(n0, n0 + nn)
                    tt(k, "dve", ta[:, 0:nn], zr[:, 0:nn], cosT[:, ns], ALU.mult, [zr, cosT], [ta])
                    tt(k, "pool", tb_[:, 0:nn], zi[:, 0:nn], sinT[:, ns], ALU.mult, [zi, sinT], [tb_])
                    tt(k, "dve", zre[:, ns], ta[:, 0:nn], tb_[:, 0:nn], ALU.add, [ta, tb_], [zre])
                    tt(k, "pool", ta[:, 0:nn], zi[:, 0:nn], cosT[:, ns], ALU.mult, [zi, cosT], [ta])
                    tt(k, "dve", tb_[:, 0:nn], zr[:, 0:nn], sinT[:, ns], ALU.mult, [zr, sinT], [tb_])
                    tt(k, "pool", zim[:, ns], ta[:, 0:nn], tb_[:, 0:nn], ALU.subtract, [ta, tb_], [zim])
                k.op("dve", lambda e: e.tensor_tensor_scan(out=gre[:], data0=rhoT[:], data1=zre[:], initial=0.0, op0=ALU.mult, op1=ALU.add), r=[rhoT, zre], w=[gre])
                k.op("dve", lambda e: e.tensor_tensor_scan(out=gim[:], data0=rhoT[:], data1=zim[:], initial=0.0, op0=ALU.mult, op1=ALU.add), r=[rhoT, zim], w=[gim])
                tt(k, "pool", tc_[:], gre[:], cosT[:], ALU.mult, [gre, cosT], [tc_])
                tt(k, "dve", td_[:], gim[:], sinT[:], ALU.mult, [gim, sinT], [td_])
                tt(k, "pool", hre[:], tc_[:], td_[:], ALU.subtract, [tc_, td_], [hre])
                tt(k, "dve", tc_[:], gre[:], sinT[:], ALU.mult, [gre, sinT], [tc_])
                tt(k, "pool", td_[:], gim[:], cosT[:], ALU.mult, [gim, cosT], [td_])
                tt(k, "dve", him[:], tc_[:], td_[:], ALU.add, [tc_, td_], [him])
                for (n0, nn, cs) in blocks:
                    p_ = py[npy % 2]; npy += 1
                    ns = slice(n0, n0 + nn)
                    k.op("pe", lambda e, p_=p_, ns=ns, nn=nn: e.matmul(p_[:, 0:nn], lhsT=cre[:], rhs=hre[:, ns], start=True, stop=False), r=[cre, hre], w=[p_])
                    k.op("pe", lambda e, p_=p_, ns=ns, nn=nn: e.matmul(p_[:, 0:nn], lhsT=cimn[:], rhs=him[:, ns], start=False, stop=True), r=[cimn, him], w=[p_])
                    tt(k, "dve", yF[:, cs], yF[:, cs], p_[:, 0:nn], ALU.add, [yF, p_], [yF])
            k.dma(S['ys5'][gp * 32:gp * 32 + 32, :], yF[:], r=[yF])
    with g.c.phase() as ph:
        gw_f = ph.sb([128, 4, 512], F32, "gwf"); gw = ph.sb([128, 4, 512], BF16, "gw")
        k.dma(gw_f[:], I['s5_glu_w'][i].rearrange("(k p) c -> p k c", p=128), w=[gw_f])
        k.op("pool", lambda e: e.tensor_copy(out=gw[:], in_=gw_f[:]), r=[gw_f], w=[gw])
        gb = ph.sb([128, 4], F32, "gb")
        k.dma(gb[:], I['s5_glu_b4'][i], w=[gb])
        ys = [ph.sb([128, 512], F32, "ys%d" % j) for j in range(4)]
        Gf = [ph.sb([128, 512], F32, "Gf%d" % j) for j in range(4)]
        Gb = [ph.sb([128, 512], BF16, "Gb%d" % j) for j in range(4)]
        w1 = ph.sb([128, 512], F32, "w1"); w2 = ph.sb([128, 512], F32, "w2")
        gt_ = [ph.sb([128, 512], F32, "gt%d" % j) for j in range(2)]
        ob = [ph.sb([128, 512], BF16, "ob%d" % j) for j in range(2)]
        pg = [ph.ps([128, 512], F32, "pg%d" % j) for j in range(2)]
        g0 = FM['s5_g'][0]
        n_ = 0
        for n0 in range(0, T, 512):
            nn = min(512, T - n0)
            for j in range(4):
                k.dma(ys[j][:, 0:nn], S['ys5'][j * 128:(j + 1) * 128, n0:n0 + nn], w=[ys[j]])
                gelu_tanh(k, Gf[j], Gb[j], ys[j], w1, w2, nn)
            for j in range(4):
                p_ = pg[n_ % 2]; g_ = gt_[n_ % 2]; o_ = ob[n_ % 2]; n_ += 1
                for kt in range(4):
                    k.op("pe", lambda e, p_=p_, kt=kt, j=j: e.matmul(p_[:, 0:nn], lhsT=gw[:, kt, j * 128:(j + 1) * 128], rhs=Gb[kt][:, 0:nn], start=(kt == 0), stop=(kt == 3)),
                         r=[gw, Gb[kt]], w=[p_])
                act(k, w1[:, 0:nn], p_[:, 0:nn], AF.Sigmoid, [p_, gb], [w1], bias=gb[:, j:j + 1])
                tt(k, "dve", w1[:, 0:nn], w1[:, 0:nn], Gf[j][:, 0:nn], ALU.mult, [w1, Gf[j]], [w1])
                k.dma(g_[:, 0:nn], S['pfm'][g0 + j * 128:g0 + (j + 1) * 128, n0:n0 + nn], w=[g_])
                act(k, g_[:, 0:nn], g_[:, 0:nn], AF.Silu, [g_], [g_])
                tt(k, "dve", o_[:, 0:nn], w1[:, 0:nn], g_[:, 0:nn], ALU.mult, [w1, g_], [o_])
                k.dma(S['y'][j * 128:(j + 1) * 128, n0:n0 + nn], o_[:, 0:nn], r=[o_])


def gelu_tanh(k, outf, outb, x, w1, w2, nn):
    tt(k, "pool", w1[:, 0:nn], x[:, 0:nn], x[:, 0:nn], ALU.mult, [x], [w1])
    ts(k, "dve", w1[:, 0:nn], w1[:, 0:nn], 0.044715, 1.0, ALU.mult, ALU.add, [w1], [w1])
    tt(k, "dve", w2[:, 0:nn], w1[:, 0:nn], x[:, 0:nn], ALU.mult, [w1, x], [w2])
    act(k, w2[:, 0:nn], w2[:, 0:nn], AF.Sigmoid, [w2], [w2], scale=2.0 * math.sqrt(2.0 / math.pi))
    tt(k, "dve", outf[:, 0:nn], w2[:, 0:nn], x[:, 0:nn], ALU.mult, [w2, x], [outf])
    k.op("pool", lambda e: e.tensor_copy(out=outb[:, 0:nn], in_=outf[:, 0:nn]), r=[outf], w=[outb])


def lin_attn_tile(g, ph, B_, q32, k32, laF, laB, vts, C, dk, yrow0, grow0, nw_cols, layer, nwoff=0):
    k, I, S, L, CTX, T = g.k, g.I, g.S, g.L, g.CTX, g.T
    HPT = 128 // dk
    NCH = T // C
    ncc = CTX // C
    for dr in range(2):
        la = laF if dr == 0 else laB
        b = B_['b']
        if dr == 0:
            k.op("dve", lambda e: e.tensor_tensor_scan(out=b[:], data0=B_['m01'][:, 0:T], data1=la[:], initial=0.0, op0=ALU.mult, op1=ALU.add), r=[B_['m01'], la], w=[b])
        else:
            k.op("dve", lambda e: e.tensor_tensor_scan(out=b[:, ::-1], data0=B_['m01'][:, T:0:-1], data1=la[:, ::-1], initial=0.0, op0=ALU.mult, op1=ALU.add), r=[B_['m01'], la], w=[b])
        b3 = b[:].rearrange("p (n c) -> p n c", c=C)
        rc = C // 2 - 1 if dr == 0 else C // 2
        lc = C - 1 if dr == 0 else 0
        ref = b3[:, :, rc:rc + 1].to_broadcast([128, NCH, C])
        bl = b3[:, :, lc:lc + 1].to_broadcast([128, NCH, C])
        tmp = la
        t3 = tmp[:].rearrange("p (n c) -> p n c", c=C)
        tt(k, "pool", t3, b3, ref, ALU.subtract, [b], [tmp])
        act(k, tmp[:], tmp[:], AF.Exp, [tmp], [tmp])
        tt(k, "dve", B_['qt'][:], q32[:], tmp[:], ALU.mult, [q32, tmp], [B_['qt']])
        tt(k, "pool", t3, ref, b3, ALU.subtract, [b], [tmp])
        act(k, tmp[:], tmp[:], AF.Exp, [tmp], [tmp])
        tt(k, "dve", B_['kt'][:], k32[:], tmp[:], ALU.mult, [k32, tmp], [B_['kt']])
        tt(k, "pool", t3, bl, b3, ALU.subtract, [b], [tmp])
        act(k, tmp[:], tmp[:], AF.Exp, [tmp], [tmp])
        tt(k, "dve", B_['ks'][:], k32[:], tmp[:], ALU.mult, [k32, tmp], [B_['ks']])
        dec = B_['dec']
        k.op("act", lambda e: e.activation(out=dec[:], in_=b3[:, :, lc], func=AF.Exp), r=[b], w=[dec])
        if g.cut == 'prep':
            continue
        kT = B_['kT']
        per = 1024 // 128
        for j0 in range(0, NCH, per):
            nj = min(per, NCH - j0)
            p_ = B_['pT'][(j0 // per) % 2]
            for jj in range(nj):
                j = j0 + jj
                k.op("pe", lambda e, p_=p_, jj=jj, j=j: e.transpose(out=p_[0:C, jj * 128:(jj + 1) * 128], in_=B_['ks'][:, j * C:(j + 1) * C], identity=B_['ident'][:]),
                     r=[B_['ks'], B_['ident']], w=[p_])
            src = p_[0:C, 0:nj * 128].rearrange("p (a b) -> p a b", b=128)
            k.op("dve", lambda e, src=src, j0=j0, nj=nj: e.tensor_copy(out=kT[0:C, j0:j0 + nj, :], in_=src), r=[p_], w=[kT])
        act(k, tmp[:], b[:], AF.Exp, [b], [tmp])
        tt(k, "dve", B_['qs'][:], q32[:], tmp[:], ALU.mult, [q32, tmp], [B_['qs']])
        if g.cut == 'tr':
            continue
        if dr == 0:
            order = list(range(NCH))
        else:
            order = list(range(ncc - 1, -1, -1)) + list(range(NCH - 1, ncc - 1, -1))
        St = B_['St']
        Sall = B_['Sall']
        k.op("pool", lambda e: e.memset(St[:], 0.0), w=[St])
        for j in order:
            k.op("pool", lambda e, j=j: e.tensor_copy(out=Sall[:, j, :], in_=St[:]), r=[St], w=[Sall])
            for hh in range(HPT):
                p_ = B_['pkv'][hh]
                k.op("pe", lambda e, p_=p_, j=j, hh=hh: e.matmul(p_[:, 0:128], lhsT=kT[0:C, j, :], rhs=vts[hh][0:C, j, :], start=True, stop=True), r=[kT, vts[hh]], w=[p_])
                rs_ = slice(hh * dk, (hh + 1) * dk)
                k.op("dve", lambda e, p_=p_, j=j, rs_=rs_: e.scalar_tensor_tensor(out=St[rs_, :], in0=St[rs_, :], scalar=dec[rs_, j:j + 1], in1=p_[rs_, 0:128], op0=ALU.mult, op1=ALU.add),
                     r=[St, dec, p_], w=[St])
        if g.cut == 'A':
            continue
        mk = B_['maskF'] if dr == 0 else B_['maskB']
        na = 0
        for j in range(NCH):
            cs = slice(j * C, (j + 1) * C)
            for hh in range(HPT):
                rs_ = slice(hh * dk, (hh + 1) * dk)
                pa = B_['patt'][na % 2]; po = B_['po'][na % 2]; am = B_['am'][na % 2]; na += 1
                k.op("pe", lambda e, pa=pa, rs_=rs_, cs=cs: e.matmul(pa[0:C, 0:C], lhsT=B_['kt'][rs_, cs], rhs=B_['qt'][rs_, cs], start=True, stop=True), r=[B_['kt'], B_['qt']], w=[pa])
                tt(k, "dve", am[0:C, 0:C], pa[0:C, 0:C], mk[0:C, 0:C], ALU.mult, [pa, mk], [am])
                o_ = B_['o'][hh]
                if hh == 0:
                    k.op("pe", lambda e, po=po, j=j, hh=hh, am=am: e.matmul(po[:, 0:C], lhsT=vts[hh][0:C, j, :], rhs=am[0:C, 0:C], start=True, stop=False), r=[vts[hh], am], w=[po])
                    k.op("pe", lambda e, po=po, j=j, rs_=rs_, cs=cs: e.matmul(po[:, 0:C], lhsT=Sall[rs_, j, :], rhs=B_['qs'][rs_, cs], start=False, stop=True), r=[Sall, B_['qs']], w=[po])
                    po2 = None
                else:
                    po2 = B_['pkv'][na % 2]
                    k.op("pe", lambda e, po=po, j=j, hh=hh, am=am: e.matmul(po[:, 0:C], lhsT=vts[hh][0:C, j, :], rhs=am[0:C, 0:C], start=True, stop=True), r=[vts[hh], am], w=[po])
                    k.op("pe", lambda e, po2=po2, j=j, rs_=rs_, cs=cs: e.matmul(po2[:, 0:C], lhsT=Sall[rs_, j, :], rhs=B_['qs'][rs_, cs], start=True, stop=True), r=[Sall, B_['qs']], w=[po2])
                if dr == 0:
                    k.op("act", lambda e, o_=o_, po=po, cs=cs: e.copy(out=o_[:, cs], in_=po[:, 0:C]), r=[po], w=[o_])
                else:
                    tt(k, "dve", o_[:, cs], o_[:, cs], po[:, 0:C], ALU.add, [o_, po], [o_])
                if po2 is not None:
                    tt(k, "dve", o_[:, cs], o_[:, cs], po2[:, 0:C], ALU.add, [o_, po2], [o_])
    if g.cut in ('prep', 'tr', 'A', 'B'):
        return
    for hh in range(HPT):
        o_ = B_['o'][hh]
        r0 = yrow0 + hh * 128
        for n0 in range(0, T, 512):
            nn = min(512, T - n0)
            ns = slice(n0, n0 + nn)
            sq = B_['w512'][0]; gt_ = B_['w512'][1]; ob = B_['ob']
            tt(k, "pool", sq[:, 0:nn], o_[:, ns], o_[:, ns], ALU.mult, [o_], [sq])
            pm = B_['patt'][0]
            k.op("pe", lambda e, pm=pm, sq=sq, nn=nn: e.matmul(pm[:, 0:nn], lhsT=B_['onesm'][:], rhs=sq[:, 0:nn], start=True, stop=True), r=[B_['onesm'], sq], w=[pm])
            k.op("act", lambda e, sq=sq, pm=pm, nn=nn: e.activation(out=sq[:, 0:nn], in_=pm[:, 0:nn], func=AF.Sqrt, bias=B_['epsb'][:, 0:1]), r=[pm, B_['epsb']], w=[sq])
            k.op("dve", lambda e, sq=sq, nn=nn: e.reciprocal(out=sq[:, 0:nn], in_=sq[:, 0:nn]), r=[sq], w=[sq])
            tt(k, "dve", sq[:, 0:nn], sq[:, 0:nn], o_[:, ns], ALU.mult, [sq, o_], [sq])
            k.dma(gt_[:, 0:nn], S['pfm'][grow0 + hh * 128:grow0 + (hh + 1) * 128, n0:n0 + nn], w=[gt_])
            act(k, gt_[:, 0:nn], gt_[:, 0:nn], AF.Silu, [gt_], [gt_])
            k.op("dve", lambda e, sq=sq, gt_=gt_, nn=nn, hh=hh: e.scalar_tensor_tensor(out=ob[:, 0:nn], in0=sq[:, 0:nn], scalar=nw_cols[:, nwoff + hh:nwoff + hh + 1], in1=gt_[:, 0:nn], op0=ALU.mult, op1=ALU.mult),
                 r=[sq, gt_, nw_cols], w=[ob])
            k.dma(S['y'][r0:r0 + 128, n0:n0 + nn], ob[:, 0:nn], r=[ob])


def la_bufs(g, ph, C):
    k, I, T = g.k, g.I, g.T
    NCH = T // C
    B_ = {}
    for nm in ('b',):
        B_[nm] = ph.sb([128, T], F32, nm)
    for nm in ('qt', 'kt', 'ks'):
        B_[nm] = ph.sb([128, T], BF16, nm)
    B_['qs'] = B_['ks']
    B_['m01'] = ph.sb([128, T + 1], F32, "m01")
    k.dma(B_['m01'][:], I['m01_%d' % C][:, :], w=[B_['m01']])
    B_['dec'] = ph.sb([128, NCH], F32, "dec")
    B_['kT'] = ph.sb([128, NCH, 128], BF16, "kT")
    B_['Sall'] = ph.sb([128, NCH, 128], BF16, "Sall")
    B_['St'] = ph.sb([128, 128], F32, "St")
    B_['ident'] = ph.sb([128, 128], BF16, "identb")
    k.dma(B_['ident'][:], I['ident_bf'][:, :], w=[B_['ident']])
    B_['maskF'] = ph.sb([128, 128], F32, "maskF"); B_['maskB'] = ph.sb([128, 128], F32, "maskB")
    k.dma(B_['maskF'][:], I['maskF'][:, :], w=[B_['maskF']]); k.dma(B_['maskB'][:], I['maskB'][:, :], w=[B_['maskB']])
    B_['epsb'] = ph.sb([128, 1], F32, "lepsb")
    k.op("pool", lambda e: e.memset(B_['epsb'][:], EPS), w=[B_['epsb']])
    B_['onesm'] = ph.sb([128, 128], F32, "onesm")
    k.op("pool", lambda e: e.memset(B_['onesm'][:], 1.0 / 128.0), w=[B_['onesm']])
    B_['pT'] = [ph.ps([128, 1024], BF16, "lpT%d" % j) for j in range(2)]
    B_['pkv'] = [ph.ps([128, 512], F32, "pkv%d" % j) for j in range(2)]
    B_['patt'] = [ph.ps([128, 512], F32, "patt%d" % j) for j in range(2)]
    B_['po'] = [ph.ps([128, 512], F32, "po%d" % j) for j in range(2)]
    B_['am'] = [ph.sb([128, 128], BF16, "am%d" % j) for j in range(2)]
    B_['w512'] = [ph.sb([128, 512], F32, "w512_%d" % j) for j in range(2)]
    B_['ob'] = ph.sb([128, 512], BF16, "ob")
    return B_


def load_v(g, ph, C, col0, name, B_):
    k, S, T = g.k, g.S, g.T
    NCH = T // C
    halves = (NCH * 128) // T
    nph = NCH // halves
    b = B_['b']
    bv = b[0:C, :].rearrange("p (n e) -> p n e", e=128)
    vb = ph.sb([C, NCH, 128], BF16, name)
    for hf in range(halves):
        r0 = hf * nph * C
        k.dma(bv, S['ptm'][r0:r0 + nph * C, col0:col0 + 128].rearrange("(n c) e -> c n e", c=C), w=[b])
        k.op("pool", lambda e, hf=hf: e.tensor_copy(out=vb[:, hf * nph:(hf + 1) * nph, :], in_=bv), r=[b], w=[vb])
    return vb


def phase_ret(g, i):
    k, I, S, L, CTX, T = g.k, g.I, g.S, g.L, g.CTX, g.T
    for h in range(4):
        with g.c.phase() as ph:
            B_ = la_bufs(g, ph, 128)
            B_['o'] = [ph.sb([128, T], F32, "o0")]
            rc_ = ph.sb([128, T], F32, "ropeC"); rs_ = ph.sb([128, T], F32, "ropeS")
            k.dma(rc_[:], I['ropeC'][:, 0:T], w=[rc_]); k.dma(rs_[:], I['ropeS'][:, 0:T], w=[rs_])
            q32 = ph.sb([128, T], F32, "q32"); k32 = ph.sb([128, T], F32, "k32")
            x2 = B_['b']
            for (dst, a, b_, scale) in ((q32, 'ret_q', 'ret_qs', 1.0), (k32, 'ret_k', 'ret_ks', 128.0 ** -0.5)):
                k.dma(dst[:], S['pfm'][FM[a][0] + h * 128:FM[a][0] + (h + 1) * 128, :], w=[dst])
                k.dma(x2[:], S['pfm'][FM[b_][0] + h * 128:FM[b_][0] + (h + 1) * 128, :], w=[x2])
                tt(k, "dve", dst[:], dst[:], rc_[:], ALU.mult, [dst, rc_], [dst])
                tt(k, "pool", x2[:], x2[:], rs_[:], ALU.mult, [x2, rs_], [x2])
                if scale == 1.0:
                    tt(k, "dve", dst[:], dst[:], x2[:], ALU.add, [dst, x2], [dst])
                else:
                    tt(k, "dve", dst[:], dst[:], x2[:], ALU.add, [dst, x2], [dst])
                    ts(k, "dve", dst[:], dst[:], scale, None, ALU.mult, None, [dst], [dst])
            lg = math.log(1.0 - 2.0 ** (-5.0 - h))
            laF = rc_; laB = rs_
            k.op("pool", lambda e: e.memset(laF[:], lg), w=[laF])
            k.op("pool", lambda e: e.memset(laB[:], lg), w=[laB])
            vts = [load_v(g, ph, 128, h * 128, "vr", B_)]
            nwc = ph.sb([128, 4], F32, "nwc")
            k.dma(nwc[:], I['ret_nw'][i], w=[nwc])
            lin_attn_tile(g, ph, B_, q32, k32, laF, laB, vts, 128, 128, 1024 + h * 128, FM['ret_g'][0] + h * 128, nwc, i, nwoff=h)


def phase_gla(g, i):
    k, I, S, L, CTX, T = g.k, g.I, g.S, g.L, g.CTX, g.T
    for tl in range(2):
        with g.c.phase() as ph:
            B_ = la_bufs(g, ph, 64)
            B_['o'] = [ph.sb([128, T], BF16, "o%d" % j) for j in range(2)]
            las = [ph.sb([128, T], F32, "laF"), ph.sb([128, T], F32, "laB")]
            q32 = ph.sb([128, T], BF16, "q16"); k32 = ph.sb([128, T], BF16, "k16")
            k.dma(las[0][:], S['pfm'][FM['gla_q'][0] + tl * 128:FM['gla_q'][0] + (tl + 1) * 128, :], w=[las[0]])
            k.dma(las[1][:], S['pfm'][FM['gla_k'][0] + tl * 128:FM['gla_k'][0] + (tl + 1) * 128, :], w=[las[1]])
            ts(k, "dve", q32[:], las[0][:], 64.0 ** -0.5, None, ALU.mult, None, [las[0]], [q32])
            cp(k, "dve", k32[:], las[1][:], [las[1]], [k32])
            lrb = B_['b']
            lr = lrb[0:32, :]
            k.dma(lr, S['pfm'][FM['gla_lr'][0]:FM['gla_lr'][0] + 32, :], w=[lrb])
            gwt = ph.sb([32, 2, 128], F32, "gwt")
            k.dma(gwt[:], I['gla_gw'][i, tl], w=[gwt])
            gbt = ph.sb([128, 2], F32, "gbt")
            k.dma(gbt[:], I['gla_gb'][i, tl], w=[gbt])
            ts(k, "dve", gbt[:], gbt[:], -1.0, None, ALU.mult, None, [gbt], [gbt])
            pl_ = B_['patt']
            for dr in range(2):
                for bi, n0 in enumerate(range(0, T, 512)):
                    nn = min(512, T - n0)
                    p_ = pl_[bi % 2]
                    k.op("pe", lambda e, p_=p_, dr=dr, n0=n0, nn=nn: e.matmul(p_[:, 0:nn], lhsT=gwt[:, dr, :], rhs=lr[:, n0:n0 + nn], start=True, stop=True), r=[gwt, lrb], w=[p_])
                    negb = B_['w512'][0]
                    act(k, negb[:, 0:nn], p_[:, 0:nn], AF.Exp, [p_, gbt], [negb], bias=gbt[:, dr:dr + 1], scale=-1.0)
                    ts(k, "dve", negb[:, 0:nn], negb[:, 0:nn], 1.0, None, ALU.add, None, [negb], [negb])
                    act(k, negb[:, 0:nn], negb[:, 0:nn], AF.Ln, [negb], [negb])
                    ts(k, "dve", las[dr][:, n0:n0 + nn], negb[:, 0:nn], -1.0 / 16.0, None, ALU.mult, None, [negb], [las[dr]])
            vts = [load_v(g, ph, 64, 512 + (tl * 2 + hh) * 128, "vg%d" % hh, B_) for hh in range(2)]
            nwc = ph.sb([128, 4], F32, "nwc")
            k.dma(nwc[:], I['gla_nw'][i], w=[nwc])
            lin_attn_tile(g, ph, B_, q32, k32, las[0], las[1], vts, 64, 64, 1536 + tl * 256, FM['gla_g'][0] + tl * 256, nwc, i, nwoff=tl * 2)


def cp(k, e_, out, in_, r, w):
    if e_ == "act":
        return k.op("act", lambda e: e.copy(out=out, in_=in_), r=r, w=w)
    return k.op(e_, lambda e: e.tensor_copy(out=out, in_=in_), r=r, w=w)


def phase_hyena(g, i, Lh, tok0, tg):
    k, I, S = g.k, g.I, g.S
    N = 2 * Lh
    NT, LT = N // 128, Lh // 128
    hx0 = FM['hy_x'][0]
    PI = math.pi
    with g.c.phase() as ph:
        w1 = ph.sb([33, 64], F32, "hw1"); w2 = ph.sb([64, 64], F32, "hw2"); w3 = ph.sb([64, 64], F32, "hw3"); w4 = ph.sb([64, 2048], F32, "hw4")
        k.dma(w1[:], I['hy_w1'][i], w=[w1]); k.dma(w2[:], I['hy_w2'][i], w=[w2]); k.dma(w3[:], I['hy_w3'][i], w=[w3]); k.dma(w4[:], I['hy_w4'][i], w=[w4])
        bf = ph.sb([64, 6], F32, "hbf")
        k.dma(bf[:], I['hy_bf'][i], w=[bf])
        fb = ph.sb([64, 3], F32, "hfb")
        fs = ph.sb([64, 3], F32, "hfs")
        for l_ in range(3):
            tt(k, "dve", fb[:, l_:l_ + 1], bf[:, 2 * l_:2 * l_ + 1], bf[:, 2 * l_ + 1:2 * l_ + 2], ALU.mult, [bf], [fb])
            ts(k, "dve", fs[:, l_:l_ + 1], bf[:, 2 * l_ + 1:2 * l_ + 2], 1.0 / (2.0 * PI), None, ALU.mult, None, [bf], [fs])
        ts(k, "dve", fb[:], fb[:], 1.0 / (2.0 * PI), 0.5, ALU.mult, ALU.add, [fb], [fb])
        negpi = ph.sb([64, 1], F32, "hnegpi")
        k.op("pool", lambda e: e.memset(negpi[:], -PI), w=[negpi])
        zT = ph.sb([33, N], F32, "hzT")
        k.dma(zT[:], I['hy_z' + tg][:, :], w=[zT])
        hA = ph.sb([64, N], F32, "hA"); hB = ph.sb([64, N], F32, "hB")
        pm = [ph.ps([64, 512], F32, "hpm%d" % j) for j in range(2)]
        a_ = ph.sb([64, 512], F32, "ha"); ai_ = ph.sb([64, 512], mybir.dt.int32, "hai")
        srcs = [(w1, zT, 33), (w2, hA, 64), (w3, hB, 64)]
        dsts = [hA, hB, hA]
        nb = 0
        for l_ in range(3):
            w_, src, kk = srcs[l_]
            dst = dsts[l_]
            BK = min(512, N)
            for n0 in range(0, N, BK):
                p_ = pm[nb % 2]; nb += 1
                k.op("pe", lambda e, p_=p_, w_=w_, src=src, kk=kk, n0=n0: e.matmul(p_[:, 0:BK], lhsT=w_[0:kk, :], rhs=src[0:kk, n0:n0 + BK], start=True, stop=True), r=[w_, src], w=[p_])
                k.op("act", lambda e, p_=p_, l_=l_: e.activation(out=a_[:, 0:BK], in_=p_[:, 0:BK], func=AF.Identity, bias=fb[:, l_:l_ + 1], scale=fs[:, l_:l_ + 1]), r=[p_, fb, fs], w=[a_])
                frac_inplace(k, a_[:, 0:BK], ai_[:, 0:BK], [a_, ai_])
                act(k, dst[:, n0:n0 + BK], a_[:, 0:BK], AF.Sin, [a_, negpi], [dst], bias=negpi[:, 0:1], scale=2.0 * PI)
        h3 = hA
        ones = ph.sb([128, 128], F32, "hones")
        k.op("pool", lambda e: e.memset(ones[:], 1.0), w=[ones])
        pk = [ph.ps([128, 512], F32, "hpk%d" % j) for j in range(2)]
        pss = [ph.ps([128, 512], F32, "hpss%d" % j) for j in range(2)]
        dct = [ph.sb([128, 512], F32, "hdc%d" % j) for j in range(2)]
        kt_ = [ph.sb([128, 1024], F32, "hkt%d" % j) for j in range(2)]
        sq = ph.sb([128, 1024], F32, "hsq")
        kb = [ph.sb([128, 1024], BF16, "hkb%d" % j) for j in range(2)]
        for nt in range(NT):
            dr = 0 if nt * 128 < Lh else 1
            dc = dct[nt % 2]; ktile = kt_[nt % 2]; kbt = kb[nt % 2]
            k.dma(dc[:], I['hy_dec' + tg][nt * 128:(nt + 1) * 128, :], w=[dc])
            for o in range(2):
                k.op("pe", lambda e, o=o, nt=nt, dr=dr: e.matmul(pk[o][:, :], lhsT=h3[:, nt * 128:(nt + 1) * 128], rhs=w4[:, dr * 1024 + o * 512:dr * 1024 + (o + 1) * 512], start=True, stop=True), r=[h3, w4], w=[pk[o]])
                tt(k, "dve", ktile[:, o * 512:(o + 1) * 512], pk[o][:, :], dc[:], ALU.mult, [pk[o], dc], [ktile])
            tt(k, "pool", sq[:], ktile[:], ktile[:], ALU.mult, [ktile], [sq])
            for o in range(2):
                k.op("pe", lambda e, o=o, nt=nt: e.matmul(pss[o][:, :], lhsT=ones[:], rhs=sq[:, o * 512:(o + 1) * 512], start=(nt == 0), stop=(nt == NT - 1)), r=[ones, sq], w=[pss[o]])
            cp(k, "act", kbt[:], ktile[:], [ktile], [kbt])
            k.dma(S['hk'][nt * 128:(nt + 1) * 128, :], kbt[:], r=[kbt])
        rs = ph.sb([128, 1024], F32, "hrs")
        hepsb = ph.sb([128, 1], F32, "hepsb")
        k.op("pool", lambda e: e.memset(hepsb[:], EPS), w=[hepsb])
        for o in range(2):
            k.op("act", lambda e, o=o: e.activation(out=rs[:, o * 512:(o + 1) * 512], in_=pss[o][:, :], func=AF.Sqrt, bias=hepsb[:, 0:1]), r=[pss[o], hepsb], w=[rs])
        k.op("dve", lambda e: e.reciprocal(out=rs[:], in_=rs[:]), r=[rs], w=[rs])
        ts(k, "dve", rs[:], rs[:], 2.0 / N, None, ALU.mult, None, [rs], [rs])
        k.dma(S['hrs'][:, :], rs[:], r=[rs])
    if g.cut == 'h_filt':
        return
    with g.c.phase() as ph:
        rs = ph.sb([128, 1024], F32, "hrs2")
        k.dma(rs[:], S['hrs'][:, :], w=[rs])
        kbo = ph.sb([128, NT, 512], BF16, "hkbo")
        tks = [ph.sb([128, NT, 128], BF16, "htk%d" % j) for j in range(2)]
        pf = [ph.ps([128, 512], F32, "hpf%d" % j) for j in range(2)]
        ko = [ph.sb([128, 512], F32, "hko%d" % j) for j in range(2)]
        nn_ = 0
        for o in range(2):
            k.dma(kbo[:], S['hk'][0:N, o * 512:(o + 1) * 512].rearrange("(n p) c -> p n c", p=128), w=[kbo])
            for ft in range(LT):
                for ri in range(2):
                    tk = tks[nn_ % 2]; p_ = pf[nn_ % 2]; ko_ = ko[nn_ % 2]; nn_ += 1
                    k.dma(tk[:], I['hy_K%s%s' % ("re" if ri == 0 else "im", tg)][ft], w=[tk])
                    for nt in range(NT):
                        k.op("pe", lambda e, p_=p_, tk=tk, nt=nt: e.matmul(p_[:, :], lhsT=tk[:, nt, :], rhs=kbo[:, nt, :], start=(nt == 0), stop=(nt == NT - 1)), r=[tk, kbo], w=[p_])
                    tt(k, "dve", ko_[:], p_[:, :], rs[:, o * 512:(o + 1) * 512], ALU.mult, [p_, rs], [ko_])
                    if ft == 0:
                        ts(k, "dve", ko_[0:1, :], ko_[0:1, :], 0.5, None, ALU.mult, None, [ko_], [ko_])
                    k.dma(S['hKf'][o, ri, ft * 128:(ft + 1) * 128, :], ko_[:], r=[ko_])
    if g.cut == 'h_kf':
        return
    with g.c.phase() as ph:
        cw = ph.sb([128, 12, 4], F32, "hcw")
        k.dma(cw[:], I['hy_cw'][i], w=[cw])
        pt_ = [ph.sb([128, Lh + 2], F32, "hp%d" % j) for j in range(2)]
        xo = [ph.sb([128, Lh], F32, "hxo%d" % j) for j in range(2)]
        for c_ in range(12):
            p_ = pt_[c_ % 2]; x_ = xo[c_ % 2]
            k.op("pool", lambda e, p_=p_: e.memset(p_[:, 0:1], 0.0), w=[p_])
            k.op("pool", lambda e, p_=p_: e.memset(p_[:, Lh + 1:Lh + 2], 0.0), w=[p_])
            k.dma(p_[:, 1:Lh + 1], S['pfm'][hx0 + c_ * 128:hx0 + (c_ + 1) * 128, tok0:tok0 + Lh], w=[p_])
            k.op("act", lambda e, p_=p_, x_=x_, c_=c_: e.activation(out=x_[:], in_=p_[:, 1:Lh + 1], func=AF.Identity, bias=cw[:, c_, 3:4], scale=cw[:, c_, 1:2]), r=[p_, cw], w=[x_])
            k.op("dve", lambda e, p_=p_, x_=x_, c_=c_: e.scalar_tensor_tensor(out=x_[:], in0=p_[:, 0:Lh], scalar=cw[:, c_, 0:1], in1=x_[:], op0=ALU.mult, op1=ALU.add), r=[p_, cw, x_], w=[x_])
            k.op("dve", lambda e, p_=p_, x_=x_, c_=c_: e.scalar_tensor_tensor(out=x_[:], in0=p_[:, 2:Lh + 2], scalar=cw[:, c_, 2:3], in1=x_[:], op0=ALU.mult, op1=ALU.add), r=[p_, cw, x_], w=[x_])
            k.dma(S['hxs'][c_ * 128:(c_ + 1) * 128, 0:Lh], x_[:], r=[x_])
    if g.cut == 'h_sc':
        return
    for o in range(2):
        zsrc = S['hxs'][0:512, 0:Lh] if o == 0 else S['hz1'][:, 0:Lh]
        with g.c.phase() as ph:
            ident = ph.sb([128, 128], BF16, "hident")
            k.dma(ident[:], I['ident_bf'][:, :], w=[ident])
            zTt = ph.sb([128, LT, 512], BF16, "hzTt")
            zf = ph.sb([128, Lh], F32, "hzf"); zb = ph.sb([128, Lh], BF16, "hzb")
            pT = [ph.ps([128, 1024], BF16, "hpT%d" % j) for j in range(2)]
            nq = 0
            for c4 in range(4):
                k.dma(zf[:], zsrc[c4 * 128:(c4 + 1) * 128, :], w=[zf])
                cp(k, "pool", zb[:], zf[:], [zf], [zb])
                for t0 in range(0, LT, 8):
                    nj = min(8, LT - t0)
                    p_ = pT[nq % 2]; nq += 1
                    for jj in range(nj):
                        k.op("pe", lambda e, p_=p_, jj=jj, t0=t0: e.transpose(out=p_[:, jj * 128:(jj + 1) * 128], in_=zb[:, (t0 + jj) * 128:(t0 + jj + 1) * 128], identity=ident[:]), r=[zb, ident], w=[p_])
                    cp(k, k.ev(), zTt[:, t0:t0 + nj, c4 * 128:(c4 + 1) * 128], p_[:, 0:nj * 128].rearrange("p (a b) -> p a b", b=128), [p_], [zTt])
            tks = [ph.sb([128, LT, 128], BF16, "htkf%d" % j) for j in range(4)]
            pz = [ph.ps([128, 512], F32, "hpz%d" % j) for j in range(4)]
            kf = [ph.sb([128, 512], F32, "hkf%d" % j) for j in range(4)]
            wa = ph.sb([128, 512], F32, "hwa"); wb_ = ph.sb([128, 512], F32, "hwb")
            yo = [ph.sb([128, 2, 512], BF16, "hyo%d" % j) for j in range(2)]
            r0t = ph.sb([1, 2, 512], F32, "hr0t")
            for ft in range(LT):
                s_ = (ft % 2) * 2
                for ri in range(2):
                    tk = tks[s_ + ri]
                    k.dma(tk[:], I['hy_K%s%s' % ("re" if ri == 0 else "im", tg)][ft][:, 0:LT, :], w=[tk])
                    k.dma(kf[s_ + ri][:], S['hKf'][o, ri, ft * 128:(ft + 1) * 128, :], w=[kf[s_ + ri]])
                    for tt_ in range(LT):
                        k.op("pe", lambda e, ri=ri, tk=tk, tt_=tt_, s_=s_: e.matmul(pz[s_ + ri][:, :], lhsT=tk[:, tt_, :], rhs=zTt[:, tt_, :], start=(tt_ == 0), stop=(tt_ == LT - 1)), r=[tk, zTt], w=[pz[s_ + ri]])
                zr_, zi_, kr_, ki_ = pz[s_], pz[s_ + 1], kf[s_], kf[s_ + 1]
                y_ = yo[ft % 2]
                tt(k, "dve", wa[:], zr_[:, :], kr_[:], ALU.mult, [zr_, kr_], [wa])
                tt(k, "dve", wb_[:], zi_[:, :], ki_[:], ALU.mult, [zi_, ki_], [wb_])
                tt(k, "pool", y_[:, 0, :], wa[:], wb_[:], ALU.subtract, [wa, wb_], [y_])
                if ft == 0:
                    cp(k, "pool", r0t[0:1, 0, :], wa[0:1, :], [wa], [r0t])
                    cp(k, "pool", r0t[0:1, 1, :], wb_[0:1, :], [wb_], [r0t])
                tt(k, "dve", wa[:], zr_[:, :], ki_[:], ALU.mult, [zr_, ki_], [wa])
                tt(k, "dve", wb_[:], zi_[:, :], kr_[:], ALU.mult, [zi_, kr_], [wb_])
                tt(k, "pool", y_[:, 1, :], wa[:], wb_[:], ALU.add, [wa, wb_], [y_])
                if ft == 0:
                    cp(k, "pool", y_[0:1, :, :], r0t[0:1, :, :], [r0t, y_], [y_])
                k.dma(S['hY'][ft].rearrange("r p c -> p r c"), y_[:], r=[y_])
        if g.cut == 'h_fwd':
            return
        with g.c.phase() as ph:
            identf = ph.sb([128, 128], F32, "hidf")
            k.dma(identf[:], I['ident_f'][:, :], w=[identf])
            Y = ph.sb([128, LT, 2, 512], BF16, "hYs")
            for ft in range(LT):
                k.dma(Y[:, ft], S['hY'][ft].rearrange("r p c -> p r c"), w=[Y])
            cv = [ph.sb([128, Lh], F32, "hcv%d" % j) for j in range(4)]
            tis = [ph.sb([128, LT, 128], BF16, "hti%d" % j) for j in range(4)]
            py = [ph.ps([128, 512], F32, "hpy%d" % j) for j in range(2)]
            ptr = [ph.ps([128, 512], F32, "hptr%d" % j) for j in range(2)]
            yt = [ph.sb([128, 512], F32, "hyt%d" % j) for j in range(2)]
            qs4 = py + ptr
            for tt_ in range(LT):
                s_ = (tt_ % 2) * 2
                for ri in range(2):
                    ti = tis[s_ + ri]
                    k.dma(ti[:], I['hy_I%s%s' % ("re" if ri == 0 else "im", tg)][tt_], w=[ti])
                for c4 in range(4):
                    for ri in range(2):
                        ti = tis[s_ + ri]
                        for ft in range(LT):
                            q_ = qs4[c4]
                            k.op("pe", lambda e, q_=q_, ti=ti, ft=ft, ri=ri, c4=c4: e.matmul(q_[:, 0:128], lhsT=Y[:, ft, ri, c4 * 128:(c4 + 1) * 128], rhs=ti[:, ft, :],
                                                                                     start=(ri == 0 and ft == 0), stop=(ri == 1 and ft == LT - 1)), r=[ti, Y], w=[q_])
                    cp(k, k.ev(), cv[c4][:, tt_ * 128:(tt_ + 1) * 128], qs4[c4][:, 0:128], [qs4[c4]], [cv[c4]])
            if g.cut in ('h_inv1', 'h_inv2'):
                return
            hb_ = ph.sb([128, 4, 2], F32, "hbias")
            k.dma(hb_[:], I['hy_bias4'][i], w=[hb_])
            zf = ph.sb([128, Lh], F32, "hzf2"); gf = ph.sb([128, Lh], F32, "hgf"); ob = ph.sb([128, Lh], BF16, "hob")
            for c4 in range(4):
                k.dma(zf[:], zsrc[c4 * 128:(c4 + 1) * 128, :], w=[zf])
                k.dma(gf[:], S['hxs'][(o + 1) * 512 + c4 * 128:(o + 1) * 512 + (c4 + 1) * 128, 0:Lh], w=[gf])
                k.op("dve", lambda e, c4=c4: e.scalar_tensor_tensor(out=zf[:], in0=zf[:], scalar=hb_[:, c4, o:o + 1], in1=cv[c4][:], op0=ALU.mult, op1=ALU.add), r=[zf, hb_, cv[c4]], w=[zf])
                tt(k, "pool", zf[:], zf[:], gf[:], ALU.mult, [zf, gf], [zf])
                if o == 0:
                    k.dma(S['hz1'][c4 * 128:(c4 + 1) * 128, 0:Lh], zf[:], r=[zf])
                else:
                    g0 = FM['hy_g'][0]
                    k.dma(gf[:], S['pfm'][g0 + c4 * 128:g0 + (c4 + 1) * 128, tok0:tok0 + Lh], w=[gf])
                    act(k, gf[:], gf[:], AF.Silu, [gf], [gf])
                    tt(k, "dve", ob[:], zf[:], gf[:], ALU.mult, [zf, gf], [ob])
                    k.dma(S['y'][512 + c4 * 128:512 + (c4 + 1) * 128, tok0:tok0 + Lh], ob[:], r=[ob])


def phase_outproj(g, i, last):
    k, I, S, L, CTX, T = g.k, g.I, g.S, g.L, g.CTX, g.T
    with g.c.phase() as ph:
        wo = ph.sb([128, NK, D], BF16, "wo")
        k.dma(wo[:], S['wob'].rearrange("(k p) c -> p k c", p=128), w=[wo])
        gts = [load_bc(g, ph, "gt%d" % r, S['mod'][r:r + 1, 2 * D:3 * D], D) for r in range(2)]
        fnw = load_bc(g, ph, "fnw", I['final_norm_w'][0:1, :], D) if last else None
        yT = [ph.sb([128, NK, 128], BF16, "yT%d" % j) for j in range(2)]
        xr = [ph.sb([128, D], F32, "xr%d" % j) for j in range(2)]
        xn = [ph.sb([128, D], F32, "xn%d" % j) for j in range(2)]
        tmp = ph.sb([128, 512], F32, "otmp")
        junk = ph.sb([128, D], BF16, "ojunk")
        ss = ph.sb([128, 1], F32, "oss")
        epsb = ph.sb([128, 1], F32, "oepsb")
        k.op("pool", lambda e: e.memset(epsb[:], EPS), w=[epsb])
        po = [ph.ps([128, 512], F32, "opo%d" % j) for j in range(4)]
        yv = S['y'].rearrange("(k p) t -> p k t", p=128)
        nb = 0
        npo = 0
        for tok in range(0 if not last else CTX, T, 128):
            isctx = tok < CTX
            y_ = yT[nb % 2]; x_ = xr[nb % 2]; o_ = xn[nb % 2]; nb += 1
            k.dma(y_[:], yv[:, :, tok:tok + 128], w=[y_])
            if i == 0:
                src = I['ctx'][tok:tok + 128, :] if isctx else I['x'][tok - CTX:tok - CTX + 128, :]
            else:
                src = S['xc1'][tok:tok + 128, :] if isctx else S['x1'][tok - CTX:tok - CTX + 128, :]
            k.dma(x_[:], src, w=[x_])
            gt = gts[1 if isctx else 0]
            for n in range(4):
                p_ = po[npo % 4]; npo += 1
                for kk in range(NK):
                    k.op("pe", lambda e, p_=p_, kk=kk, n=n, y_=y_: e.matmul(p_[:, :], lhsT=y_[:, kk, :], rhs=wo[:, kk, n * 512:(n + 1) * 512], start=(kk == 0), stop=(kk == NK - 1)), r=[y_, wo], w=[p_])
                cs = slice(n * 512, (n + 1) * 512)
                tt(k, "dve", tmp[:], p_[:, :], gt[:, cs], ALU.mult, [p_, gt], [tmp])
                tt(k, "pool", o_[:, cs], tmp[:], x_[:, cs], ALU.add, [tmp, x_], [o_])
            if last:
                k.op("act", lambda e, o_=o_: e.activation(out=junk[:], in_=o_[:], func=AF.Square, accum_out=ss[:]), r=[o_], w=[junk, ss])
                rsqrt_inplace(k, ss[:], ss, epsb[:, 0:1], epsb, scale=1.0 / D)
                k.op("dve", lambda e, o_=o_: e.scalar_tensor_tensor(out=o_[:], in0=o_[:], scalar=ss[:, 0:1], in1=fnw[:], op0=ALU.mult, op1=ALU.mult), r=[o_, ss, fnw], w=[o_])
                k.dma(g.out[tok - CTX:tok - CTX + 128, :], o_[:], r=[o_])
            elif isctx:
                k.dma(S['xc1'][tok:tok + 128, :], o_[:], r=[o_])
            else:
                k.dma(S['x1'][tok - CTX:tok - CTX + 128, :], o_[:], r=[o_])

def s5_prep(inp, T):
    m = {}
    DEPTH = inp['s5_a_re'].shape[0]
    def st(a):
        return np.ascontiguousarray(a.reshape(DEPTH, 2, 16, 2, 64).transpose(0, 3, 4, 1, 2).reshape(DEPTH, 128, 32))
    m['s5_are'] = st(inp['s5_a_re']); m['s5_aim'] = st(inp['s5_a_im'])
    ldt = np.broadcast_to(inp['s5_log_dt'][..., None], (DEPTH, 2, 32, 64))
    m['s5_ldt'] = st(np.ascontiguousarray(ldt))
    m['s5_d32'] = np.ascontiguousarray(inp['s5_d'].reshape(DEPTH, 16, 32).transpose(0, 2, 1))
    def Bl(B):
        o = np.zeros((DEPTH, 2, 16, 32, 128), np.float32)
        Br = B.reshape(DEPTH, 2, 16, 2, 64, 16)
        for g2 in range(2):
            o[:, :, :, g2 * 16:(g2 + 1) * 16, g2 * 64:(g2 + 1) * 64] = Br[:, :, :, g2].transpose(0, 1, 2, 4, 3)
        return o
    m['s5_Bre'] = Bl(inp['s5_b_re']); m['s5_Bim'] = Bl(inp['s5_b_im'])
    def Cl(C):
        o = np.zeros((DEPTH, 2, 16, 128, 32), np.float32)
        Cr = C.reshape(DEPTH, 2, 16, 2, 16, 64)
        for g2 in range(2):
            o[:, :, :, g2 * 64:(g2 + 1) * 64, g2 * 16:(g2 + 1) * 16] = Cr[:, :, :, g2].transpose(0, 1, 2, 4, 3)
        return o
    m['s5_Cre'] = Cl(inp['s5_c_re']); m['s5_Cim'] = Cl(inp['s5_c_im'])
    m['s5_glu_w'] = inp['s5_glu_w']
    m['s5_glu_b4'] = np.ascontiguousarray(inp['s5_glu_b'].reshape(DEPTH, 4, 128).transpose(0, 2, 1))
    m['tvals'] = np.ascontiguousarray(np.broadcast_to(np.arange(T, dtype=np.float32)[None], (128, T)))
    return m


def attn_prep(inp, L, CTX, GW):
    T = L + CTX
    m = {}
    DEPTH = inp['ret_norm_w'].shape[0]
    for C in (128, 64):
        a = np.ones((128, T + 1), np.float32); a[:, ::C] = 0.0
        m['m01_%d' % C] = a
    s = np.arange(128)
    m['maskF'] = (s[:, None] <= s[None, :]).astype(np.float32)
    m['maskB'] = (s[:, None] >= s[None, :]).astype(np.float32)
    BASE = 10000.0
    inv_c = BASE ** (-np.arange(64, dtype=np.float32) / 64)
    ang_c = np.arange(CTX, dtype=np.float32)[:, None] * inv_c
    inv = BASE ** (-np.arange(32, dtype=np.float32) / 32)
    j = np.arange(L)
    ang_l = np.concatenate([(j // GW).astype(np.float32)[:, None] * inv, (j % GW).astype(np.float32)[:, None] * inv], -1)
    ang = np.concatenate([ang_c, ang_l], 0).astype(np.float32)
    cos, sin = np.cos(ang).T, np.sin(ang).T
    m['ropeC'] = np.ascontiguousarray(np.concatenate([cos, cos], 0)).astype(np.float32)
    m['ropeS'] = np.ascontiguousarray(np.concatenate([-sin, sin], 0)).astype(np.float32)
    m['ret_nw'] = np.ascontiguousarray(inp['ret_norm_w'].reshape(DEPTH, 4, 128).transpose(0, 2, 1))
    m['gla_nw'] = np.ascontiguousarray(inp['gla_norm_w'].reshape(DEPTH, 4, 128).transpose(0, 2, 1))
    gw = np.zeros((DEPTH, 2, 32, 2, 128), np.float32)
    for tl in range(2):
        for dr in range(2):
            gw[:, tl, 16 * dr:16 * dr + 16, dr, :] = inp['gla_gate_w'][:, dr, :, tl * 128:(tl + 1) * 128]
    m['gla_gw'] = gw
    m['gla_gb'] = np.ascontiguousarray(inp['gla_gate_b'].reshape(DEPTH, 2, 2, 128).transpose(0, 2, 3, 1))
    return m


def hy_tables(Lh):
    N = 2 * Lh
    t = np.linspace(0.0, 1.0, Lh, dtype=np.float32)[:, None]
    wpos = 2.0 * math.pi * np.arange(Lh, dtype=np.float32)[:, None] / Lh
    f = np.linspace(1e-4, 15, 16, dtype=np.float32)[None, :]
    z = np.concatenate([t, np.cos(f * wpos), -np.sin(f * wpos)], -1).astype(np.float32)
    zfull = np.zeros((N, 33), np.float32)
    zfull[:Lh] = z
    zfull[Lh + 1:] = z[1:][::-1]
    deltas = np.linspace(math.log(1e-2) / 1.5, math.log(1e-2) / 0.3, 512, dtype=np.float32)
    dec = np.exp(-t * np.abs(deltas)).astype(np.float32)
    dfull = np.zeros((N, 512), np.float32)
    dfull[:Lh] = dec
    dfull[Lh + 1:] = dec[1:][::-1]
    n = np.arange(N, dtype=np.float64)[:, None]
    fr = np.arange(Lh, dtype=np.float64)[None, :]
    ang = 2.0 * math.pi * ((n * fr) % N) / N
    Kre = np.cos(ang); Kim = -np.sin(ang)
    Kim[:, 0] = np.where(np.arange(N) % 2 == 0, 1.0, -1.0)
    def tile(A, KT, MT):
        return np.ascontiguousarray(A.reshape(KT, 128, MT, 128).transpose(2, 1, 0, 3)).astype(ml_dtypes.bfloat16)
    m = {}
    m['z'] = np.ascontiguousarray(zfull.T)
    m['dec'] = dfull
    m['Kre'] = tile(Kre, N // 128, Lh // 128); m['Kim'] = tile(Kim, N // 128, Lh // 128)
    ff = np.arange(Lh, dtype=np.float64)[:, None]; tt = np.arange(Lh, dtype=np.float64)[None, :]
    ang2 = 2.0 * math.pi * ((ff * tt) % N) / N
    Ire = np.cos(ang2); Iim = -np.sin(ang2)
    Iim[0, :] = np.where(np.arange(Lh) % 2 == 0, 1.0, -1.0)
    m['Ire'] = tile(Ire, Lh // 128, Lh // 128); m['Iim'] = tile(Iim, Lh // 128, Lh // 128)
    return m


def hy_prep(inp, L, CTX):
    m = {}
    DEPTH = inp['hy_w1'].shape[0]
    for tg, Lh in (('C', CTX), ('L', L)):
        tb = hy_tables(Lh)
        for kk, v in tb.items():
            m['hy_' + kk + tg] = v
    for nm in ('hy_w1', 'hy_w2', 'hy_w3', 'hy_w4'):
        m[nm] = inp[nm]
    m['hy_bf'] = np.ascontiguousarray(np.stack([inp['hy_b1'], inp['hy_f1'], inp['hy_b2'], inp['hy_f2'], inp['hy_b3'], inp['hy_f3']], -1))
    cw = np.concatenate([inp['hy_conv_w'], inp['hy_conv_b'][:, None, :]], 1)
    m['hy_cw'] = np.ascontiguousarray(cw.reshape(DEPTH, 4, 12, 128).transpose(0, 3, 2, 1))
    m['hy_bias4'] = np.ascontiguousarray(inp['hy_bias'].reshape(DEPTH, 2, 4, 128).transpose(0, 3, 2, 1))
    return m


_NC_CACHE = {}


def kernel(**inputs):
    inp = {k_: np.asarray(v) for k_, v in inputs.items()}
    L, CTX, GW = 4096, 256, 64
    T = L + CTX
    shared = {}
    for nm in ('norm_w', 'ada_w', 'ada_b', 'w_in', 'w_out'):
        shared[nm] = np.ascontiguousarray(inp[nm], dtype=np.float32)
    shared['final_norm_w'] = np.ascontiguousarray(inp['final_norm_w'][None], dtype=np.float32)
    shared['ident_bf'] = np.eye(128, dtype=np.float32).astype(ml_dtypes.bfloat16)
    shared['ident_f'] = np.eye(128, dtype=np.float32)
    shared.update(s5_prep(inp, T))
    shared.update(attn_prep(inp, L, CTX, GW))
    shared.update(hy_prep(inp, L, CTX))
    ncores = 4
    in_maps = []
    for core in range(ncores):
        b = core % 4
        m = dict(shared)
        m['x'] = np.ascontiguousarray(inp['x'][b], dtype=np.float32)
        m['ctx'] = np.ascontiguousarray(inp['ctx'][b], dtype=np.float32)
        cc = np.stack([inp['c'][b].reshape(16, 128).T, inp['c_ctx'].reshape(16, 128).T], axis=-1)
        m['cc'] = np.ascontiguousarray(cc, dtype=np.float32)
        in_maps.append(m)
    if 'nc' not in _NC_CACHE:
        _NC_CACHE['nc'] = build(L=L, CTX=CTX)
    nc = _NC_CACHE['nc']
    res = run_bass_kernel_spmd(nc, in_maps, core_ids=list(range(ncores)))
    out = np.stack([np.asarray(res.results[b]['out'], dtype=np.float32) for b in range(4)], axis=0)
    return out
```

```python
import math
import numpy as np
import ml_dtypes
import concourse.bass as bass
import concourse.mybir as mybir
from concourse.bass_utils import run_bass_kernel_spmd
from contextlib import ExitStack

F32 = mybir.dt.float32
BF16 = mybir.dt.bfloat16
ALU = mybir.AluOpType
AF = mybir.ActivationFunctionType

D = 2048
BR = 512
IN_W = 6688
DEPTH = 2
EPS = 1e-6
NK = D // 128
SAME_ENGINE_SYNC = True

SEG = {}
_off = 0
for _n, _s in (('s5_u', 512), ('s5_g', 512), ('hy_x', 1536), ('hy_g', 512), ('ret_q', 512), ('ret_k', 512),
               ('ret_v', 512), ('ret_g', 512), ('gla_q', 256), ('gla_k', 256), ('gla_v', 512), ('gla_lr', 32),
               ('gla_g', 512)):
    SEG[_n] = (_off, _s)
    _off += _s


class Buf:
    __slots__ = ("t", "w", "r", "name")

    def __init__(self, t, name=""):
        self.t = t
        self.w = None
        self.r = []
        self.name = name

    def __getitem__(self, idx):
        return self.t[idx]


class K:
    def __init__(self, nc, es, n_dma_sems=32):
        self.nc = nc
        self.eng = {"pe": nc.tensor, "act": nc.scalar, "dve": nc.vector, "pool": nc.gpsimd, "sp": nc.sync}
        self.sem, self.cnt, self.seen = {}, {}, {}
        for e in self.eng:
            self.sem[e] = es.enter_context(nc.semaphore("s_" + e))
            self.cnt[e] = 0
            self.seen[e] = {}
        self.dsem = [es.enter_context(nc.semaphore("d%d" % i)) for i in range(n_dma_sems)]
        self.dval = [0] * n_dma_sems
        self.dnext = 0
        self.pending_dma = []
        self.rr = 0

    def _wait(self, e, tok):
        if tok is None:
            return
        key, sh, val = tok
        if key == ("e", e) and (not SAME_ENGINE_SYNC or e in ("pe", "sp")):
            return
        if self.seen[e].get(key, 0) >= val:
            return
        self.eng[e].wait_ge(sh, val)
        self.seen[e][key] = val

    def _deps(self, e, r, w):
        for b in r:
            self._wait(e, b.w)
        for b in w:
            self._wait(e, b.w)
            for t in b.r:
                self._wait(e, t)

    def _mark(self, tok, r, w):
        for b in r:
            b.r.append(tok)
            if len(b.r) > 64:
                b.r = b.r[-48:]
        for b in w:
            b.w = tok
            b.r = []

    def op(self, e, fn, r=(), w=()):
        self._deps(e, r, w)
        inst = fn(self.eng[e])
        self.cnt[e] += 1
        inst.then_inc(self.sem[e], 1)
        tok = (("e", e), self.sem[e], self.cnt[e])
        self._mark(tok, r, w)
        return tok

    def dma(self, out, in_, r=(), w=(), q="sp"):
        if len(r) > 0 and len(w) == 0:
            q = "act"
        i = self.dnext
        self.dnext = (self.dnext + 1) % len(self.dsem)
        key = ("d", i)
        if self.dval[i] > 0:
            self._wait(q, (key, self.dsem[i], self.dval[i]))
        self._deps(q, r, w)
        self.dval[i] += 16
        inst = self.eng[q].dma_start(out=out, in_=in_)
        inst.then_inc(self.dsem[i], 16)
        tok = (key, self.dsem[i], self.dval[i])
        self._mark(tok, r, w)
        self.pending_dma.append(tok)
        return tok

    def barrier(self):
        toks = [(("e", e), self.sem[e], self.cnt[e]) for e in self.eng if self.cnt[e] > 0]
        last = {}
        for t in self.pending_dma:
            last[t[0]] = t
        toks += list(last.values())
        for e in self.eng:
            for t in toks:
                if t[0] == ("e", e):
                    continue
                self._wait(e, t)
        self.pending_dma = []

    def ev(self):
        self.rr += 1
        return ("dve", "act")[self.rr % 2]


class Ctx:
    def __init__(self, nc, k):
        self.nc, self.k = nc, k
        self.n = 0

    def phase(self):
        return Phase(self)


class Phase:
    def __init__(self, c):
        self.c = c
        self.es = ExitStack()

    def __enter__(self):
        self.es.__enter__()
        return self

    def sb(self, shape, dt=F32, name=None):
        self.c.n += 1
        return Buf(self.es.enter_context(self.c.nc.sbuf_tensor("sb%d_%s" % (self.c.n, name or "t"), list(shape), dt)), name)

    def ps(self, shape, dt=F32, name=None):
        self.c.n += 1
        return Buf(self.es.enter_context(self.c.nc.psum_tensor("ps%d_%s" % (self.c.n, name or "p"), list(shape), dt)), name)

    def __exit__(self, *a):
        self.c.k.barrier()
        return self.es.__exit__(*a)


FM = {}
_o = 0
for _n, _s in (('s5_u', 512), ('s5_g', 512), ('hy_x', 1536), ('hy_g', 512), ('ret_q', 512), ('ret_qs', 512),
               ('ret_k', 512), ('ret_ks', 512), ('ret_g', 512), ('gla_q', 256), ('gla_k', 256), ('gla_lr', 32),
               ('gla_g', 512)):
    FM[_n] = (_o, _s)
    _o += _s
NFM = _o


def fm_tiles():
    out = []
    for name, (r0, sz) in FM.items():
        swap = name in ('ret_qs', 'ret_ks')
        src0 = SEG[name[:-1] if swap else name][0]
        for t in range(0, sz, 128):
            n = min(128, sz - t)
            if swap:
                pieces = [(src0 + t + 64, 64), (src0 + t, 64)]
            else:
                pieces = [(src0 + t, n)]
            out.append((r0 + t, n, pieces))
    return out


class G:
    pass


def build(L=4096, CTX=256, nlayers=DEPTH, debug=False, stop_after=None, only=None, skip=(), ext_p=False, cut=None):
    T = CTX + L
    NTB = T // 128
    nc = bass.Bass("TRN2", target_bir_lowering=False)
    g = G()
    g.nc, g.L, g.CTX, g.T = nc, L, CTX, T

    in_names = []

    def din(name, shape, dt=F32):
        in_names.append(name)
        return nc.dram_tensor(name, list(shape), dt, kind="ExternalInput").ap()

    def dscr(name, shape, dt=F32):
        return nc.dram_tensor(name, list(shape), dt, kind=("ExternalOutput" if debug else "Internal")).ap()

    I = {}
    I['x'] = din('x', [L, D])
    I['ctx'] = din('ctx', [CTX, D])
    I['cc'] = din('cc', [128, NK, 2])
    for nm, shp in (('norm_w', [DEPTH, D]), ('ada_w', [DEPTH, D, 3 * D]), ('ada_b', [DEPTH, 3 * D]),
                    ('w_in', [DEPTH, D, IN_W]), ('w_out', [DEPTH, D, D]), ('final_norm_w', [1, D])):
        if ext_p and nm in ('ada_w', 'w_in', 'w_out'):
            continue
        I[nm] = din(nm, shp)
    g.in_names = None
    I['ident_bf'] = din('ident_bf', [128, 128], BF16)
    I['ident_f'] = din('ident_f', [128, 128], F32)
    out = nc.dram_tensor("out", [L, D], F32, kind="ExternalOutput").ap()
    S = {}
    S['mod'] = dscr('s_mod', [2, 3 * D])
    S['wb'] = dscr('s_wb', [D, IN_W], BF16)
    S['wob'] = dscr('s_wob', [D, D], BF16)
    S['pfm'] = din('s_pfm', [NFM, T]) if ext_p else dscr('s_pfm', [NFM, T])
    S['ptm'] = din('s_ptm', [T, 1024]) if ext_p else dscr('s_ptm', [T, 1024])
    g.cut = cut
    S['y'] = dscr('s_y', [D, T], BF16)
    S['x1'] = dscr('s_x1', [L, D])
    S['xc1'] = dscr('s_xc1', [CTX, D])
    S['ys5'] = dscr('s_ys5', [512, T])
    I['tvals'] = din('tvals', [128, T])
    I['s5_are'] = din('s5_are', [DEPTH, 128, 32]); I['s5_aim'] = din('s5_aim', [DEPTH, 128, 32]); I['s5_ldt'] = din('s5_ldt', [DEPTH, 128, 32])
    I['s5_d32'] = din('s5_d32', [DEPTH, 32, 16])
    I['s5_Bre'] = din('s5_Bre', [DEPTH, 2, 16, 32, 128]); I['s5_Bim'] = din('s5_Bim', [DEPTH, 2, 16, 32, 128])
    I['s5_Cre'] = din('s5_Cre', [DEPTH, 2, 16, 128, 32]); I['s5_Cim'] = din('s5_Cim', [DEPTH, 2, 16, 128, 32])
    I['m01_128'] = din('m01_128', [128, T + 1]); I['m01_64'] = din('m01_64', [128, T + 1])
    I['maskF'] = din('maskF', [128, 128]); I['maskB'] = din('maskB', [128, 128])
    I['ropeC'] = din('ropeC', [128, T]); I['ropeS'] = din('ropeS', [128, T])
    I['ret_nw'] = din('ret_nw', [DEPTH, 128, 4]); I['gla_nw'] = din('gla_nw', [DEPTH, 128, 4])
    I['gla_gw'] = din('gla_gw', [DEPTH, 2, 32, 2, 128]); I['gla_gb'] = din('gla_gb', [DEPTH, 2, 128, 2])
    for tg_, Lh_ in (('C', CTX), ('L', L)):
        N_ = 2 * Lh_
        I['hy_z' + tg_] = din('hy_z' + tg_, [33, N_]); I['hy_dec' + tg_] = din('hy_dec' + tg_, [N_, 512])
        for ri_ in ('re', 'im'):
            I['hy_K' + ri_ + tg_] = din('hy_K' + ri_ + tg_, [Lh_ // 128, 128, N_ // 128, 128], BF16)
            I['hy_I' + ri_ + tg_] = din('hy_I' + ri_ + tg_, [Lh_ // 128, 128, Lh_ // 128, 128], BF16)
    I['hy_w1'] = din('hy_w1', [DEPTH, 33, 64]); I['hy_w2'] = din('hy_w2', [DEPTH, 64, 64]); I['hy_w3'] = din('hy_w3', [DEPTH, 64, 64]); I['hy_w4'] = din('hy_w4', [DEPTH, 64, 2048])
    I['hy_bf'] = din('hy_bf', [DEPTH, 64, 6]); I['hy_cw'] = din('hy_cw', [DEPTH, 128, 12, 4]); I['hy_bias4'] = din('hy_bias4', [DEPTH, 128, 4, 2])
    S['hk'] = dscr('s_hk', [2 * L, 1024], BF16); S['hrs'] = dscr('s_hrs', [128, 1024]); S['hKf'] = dscr('s_hKf', [2, 2, L, 512])
    S['hxs'] = dscr('s_hxs', [1536, L]); S['hz1'] = dscr('s_hz1', [512, L]); S['hY'] = dscr('s_hY', [L // 128, 2, 128, 512], BF16)
    I['s5_glu_w'] = din('s5_glu_w', [DEPTH, 512, 512]); I['s5_glu_b4'] = din('s5_glu_b4', [DEPTH, 128, 4])
    g.I, g.S, g.out = I, S, out

    with ExitStack() as es:
        k = K(nc, es)
        c = Ctx(nc, k)
        g.k, g.c = k, c
        for layer in range(nlayers):
            if not ext_p:
                phase_mod(g, layer)
                phase_wcast(g, layer)
                phase_inproj(g, layer)
            if stop_after == 'inproj':
                break
            if 's5' not in skip:
                phase_s5(g, layer)
            if stop_after == 's5':
                break
            if only in (None, 'ret') and 'attn' not in skip:
                phase_ret(g, layer)
            if only in (None, 'gla') and 'attn' not in skip:
                phase_gla(g, layer)
            if stop_after == 'attn':
                break
            if 'hy' not in skip:
                if layer < DEPTH - 1:
                    phase_hyena(g, layer, CTX, 0, 'C')
                phase_hyena(g, layer, L, CTX, 'L')
            if stop_after == 'hy':
                break
            phase_outproj(g, layer, layer == nlayers - 1)
        k.barrier()
    nc._in_names = in_names
    return nc


def phase_mod(g, i):
    k, I, S = g.k, g.I, g.S
    with g.c.phase() as ph:
        cc = ph.sb([128, NK, 2], F32, "cc")
        s2 = ph.sb([128, NK, 2], F32, "s2")
        k.dma(cc[:], I['cc'][:, :, :], w=[cc])
        k.op("act", lambda e: e.activation(out=s2[:], in_=cc[:], func=AF.Silu), r=[cc], w=[s2])
        ab = ph.sb([2, 3 * D], F32, "ab")
        k.dma(ab[0:1, :], I['ada_b'][i:i + 1, :], w=[ab])
        k.dma(ab[1:2, :], I['ada_b'][i:i + 1, :], w=[ab])
        mo = ph.sb([2, 3 * D], F32, "mo")
        HALF = 3 * D // 2
        wts = [ph.sb([128, HALF], F32, "aw%d" % j) for j in range(2)]
        pss = [ph.ps([2, 512], F32, "mps%d" % j) for j in range(HALF // 512)]
        aw = I['ada_w'][i].rearrange("(k p) c -> k p c", p=128)
        n = 0
        for half in range(2):
            for kk in range(NK):
                wt = wts[n % 2]
                n += 1
                k.dma(wt[:], aw[kk, :, half * HALF:(half + 1) * HALF], w=[wt], q=("sp", "sp")[n % 2])
                for j, ps in enumerate(pss):
                    k.op("pe", lambda e, ps=ps, wt=wt, j=j, kk=kk: e.matmul(
                        ps[:], lhsT=s2[:, kk, :], rhs=wt[:, j * 512:(j + 1) * 512], start=(kk == 0), stop=(kk == NK - 1)),
                        r=[s2, wt], w=[ps])
            for j, ps in enumerate(pss):
                c0 = half * HALF + j * 512
                k.op("dve", lambda e, ps=ps, c0=c0: e.tensor_tensor(out=mo[:, c0:c0 + 512], in0=ps[:], in1=ab[:, c0:c0 + 512], op=ALU.add),
                     r=[ps, ab], w=[mo])
        k.dma(S['mod'][:, :], mo[:], r=[mo])


def phase_wcast(g, i):
    k, I, S = g.k, g.I, g.S
    with g.c.phase() as ph:
        for (src, dst, ncol) in ((I['w_in'][i], S['wb'], IN_W), (I['w_out'][i], S['wob'], D)):
            fs = [ph.sb([128, ncol], F32, "wf%d" % j) for j in range(2)]
            bs = [ph.sb([128, ncol], BF16, "wbf%d" % j) for j in range(2)]
            sv = src.rearrange("(k p) c -> k p c", p=128)
            dv = dst.rearrange("(k p) c -> k p c", p=128)
            for kk in range(NK):
                f, b = fs[kk % 2], bs[kk % 2]
                k.dma(f[:], sv[kk], w=[f], q=("sp", "sp")[kk % 2])
                e_ = ("dve", "pool", "act")[kk % 3]
                if e_ == "act":
                    k.op("act", lambda e, f=f, b=b: e.copy(out=b[:], in_=f[:]), r=[f], w=[b])
                else:
                    k.op(e_, lambda e, f=f, b=b: e.tensor_copy(out=b[:], in_=f[:]), r=[f], w=[b])
                k.dma(dv[kk], b[:], r=[b])


def load_bc(g, ph, name, src_row_ap, n):
    t = ph.sb([128, n], F32, name)
    g.k.dma(t[:], src_row_ap.partition_broadcast(128), w=[t])
    return t


def phase_inproj(g, i):
    k, I, S, L, CTX, T = g.k, g.I, g.S, g.L, g.CTX, g.T
    with g.c.phase() as ph:
        ident = ph.sb([128, 128], BF16, "ident")
        k.dma(ident[:], I['ident_bf'][:, :], w=[ident])
        nw = load_bc(g, ph, "nw", I['norm_w'][i:i + 1, :], D)
        wmod, sh = [], []
        for r in range(2):
            sc = load_bc(g, ph, "sc%d" % r, S['mod'][r:r + 1, D:2 * D], D)
            k.op("dve", lambda e, sc=sc: e.scalar_tensor_tensor(out=sc[:], in0=sc[:], scalar=1.0, in1=nw[:], op0=ALU.add, op1=ALU.mult),
                 r=[sc, nw], w=[sc])
            wmod.append(sc)
            sh.append(load_bc(g, ph, "sh%d" % r, S['mod'][r:r + 1, 0:D], D))
        TG = min(1024, T)
        hT = ph.sb([128, NK, TG], BF16, "hT")
        xs = [ph.sb([128, D], F32, "xt%d" % j) for j in range(2)]
        junk = ph.sb([128, D], BF16, "junk")
        h1 = ph.sb([128, D], F32, "h1")
        hb = ph.sb([128, D], BF16, "hb")
        ss = ph.sb([128, 1], F32, "ss")
        epsb = ph.sb([128, 1], F32, "epsb")
        k.op("pool", lambda e: e.memset(epsb[:], EPS), w=[epsb])
        pT = [ph.ps([128, 1024], BF16, "pT%d" % j) for j in range(2)]
        pso = [ph.ps([128, 512], F32, "pso%d" % j) for j in range(4)]
        wts = [ph.sb([128, NK, 128], BF16, "wt%d" % j) for j in range(3)]
        wvs = ph.sb([128, NK, 512], BF16, "wv")
        ots = [ph.sb([128, 512], F32, "ot%d" % j) for j in range(4)]
        wbv = S['wb'].rearrange("(k p) c -> p k c", p=128)
        tiles = fm_tiles()
        nx = 0
        no = 0
        for t0 in range(0, T, TG):
            tg = min(TG, T - t0)
            for tb in range(tg // 128):
                tok = t0 + tb * 128
                isctx = tok < CTX
                src = I['ctx'][tok:tok + 128, :] if isctx else I['x'][tok - CTX:tok - CTX + 128, :]
                if i > 0:
                    src = S['xc1'][tok:tok + 128, :] if isctx else S['x1'][tok - CTX:tok - CTX + 128, :]
                x_ = xs[nx % 2]
                nx += 1
                k.dma(x_[:], src, w=[x_], q=("sp", "sp")[nx % 2])
                k.op("act", lambda e, x_=x_: e.activation(out=junk[:], in_=x_[:], func=AF.Square, accum_out=ss[:]), r=[x_], w=[junk, ss])
                rsqrt_inplace(k, ss[:], ss, epsb[:, 0:1], epsb, scale=1.0 / D)
                r_ = 1 if isctx else 0
                k.op("dve", lambda e, x_=x_, r_=r_: e.scalar_tensor_tensor(out=h1[:], in0=x_[:], scalar=ss[:, 0:1], in1=wmod[r_][:], op0=ALU.mult, op1=ALU.mult),
                     r=[x_, ss, wmod[r_]], w=[h1])
                k.op("pool", lambda e, r_=r_: e.tensor_tensor(out=hb[:], in0=h1[:], in1=sh[r_][:], op=ALU.add), r=[h1, sh[r_]], w=[hb])
                for half in range(2):
                    p_ = pT[half]
                    for j in range(8):
                        kk = half * 8 + j
                        k.op("pe", lambda e, p_=p_, j=j, kk=kk: e.transpose(out=p_[:, j * 128:(j + 1) * 128], in_=hb[:, kk * 128:(kk + 1) * 128], identity=ident[:]),
                             r=[hb, ident], w=[p_])
                    ee = k.ev()
                    dst = hT[:, half * 8:half * 8 + 8, tb * 128:(tb + 1) * 128]
                    srcp = p_[:].rearrange("p (a b) -> p a b", a=8)
                    if ee == "act":
                        k.op("act", lambda e, dst=dst, srcp=srcp: e.copy(out=dst, in_=srcp), r=[p_], w=[hT])
                    else:
                        k.op("dve", lambda e, dst=dst, srcp=srcp: e.tensor_copy(out=dst, in_=srcp), r=[p_], w=[hT])
            for ti, (r0, nr, pieces) in enumerate(tiles):
                wt = wts[ti % 3]
                c_ = 0
                for (s0, n_) in pieces:
                    k.dma(wt[:, :, c_:c_ + n_], wbv[:, :, s0:s0 + n_], w=[wt], q=("sp", "sp")[ti % 2])
                    c_ += n_
                for n0 in range(0, tg, 512):
                    nn = min(512, tg - n0)
                    ps = pso[no % 4]
                    ot = ots[no % 4]
                    no += 1
                    for kk in range(NK):
                        k.op("pe", lambda e, ps=ps, wt=wt, kk=kk, nr=nr, n0=n0, nn=nn: e.matmul(
                            ps[0:nr, 0:nn], lhsT=wt[:, kk, 0:nr], rhs=hT[:, kk, n0:n0 + nn], start=(kk == 0), stop=(kk == NK - 1)),
                            r=[wt, hT], w=[ps])
                    ee = k.ev()
                    if ee == "act":
                        k.op("act", lambda e, ps=ps, ot=ot, nr=nr, nn=nn: e.copy(out=ot[0:nr, 0:nn], in_=ps[0:nr, 0:nn]), r=[ps], w=[ot])
                    else:
                        k.op("dve", lambda e, ps=ps, ot=ot, nr=nr, nn=nn: e.tensor_copy(out=ot[0:nr, 0:nn], in_=ps[0:nr, 0:nn]), r=[ps], w=[ot])
                    k.dma(S['pfm'][r0:r0 + nr, t0 + n0:t0 + n0 + nn], ot[0:nr, 0:nn], r=[ot], q=("sp", "sp")[no % 2])
            for vi, nm in enumerate(('ret_v', 'gla_v')):
                s0 = SEG[nm][0]
                k.dma(wvs[:], wbv[:, :, s0:s0 + 512], w=[wvs])
                for tb in range(tg // 128):
                    ps = pso[no % 4]
                    ot = ots[no % 4]
                    no += 1
                    for kk in range(NK):
                        k.op("pe", lambda e, ps=ps, kk=kk, tb=tb: e.matmul(
                            ps[:, :], lhsT=hT[:, kk, tb * 128:(tb + 1) * 128], rhs=wvs[:, kk, :], start=(kk == 0), stop=(kk == NK - 1)),
                            r=[wvs, hT], w=[ps])
                    ee = k.ev()
                    if ee == "act":
                        k.op("act", lambda e, ps=ps, ot=ot: e.copy(out=ot[:], in_=ps[:]), r=[ps], w=[ot])
                    else:
                        k.op("dve", lambda e, ps=ps, ot=ot: e.tensor_copy(out=ot[:], in_=ps[:]), r=[ps], w=[ot])
                    tok = t0 + tb * 128
                    k.dma(S['ptm'][tok:tok + 128, vi * 512:(vi + 1) * 512], ot[:], r=[ot], q=("sp", "sp")[no % 2])


TWO_PI = 2.0 * math.pi


def tt(k, e_, out, a, b, op, r, w):
    return k.op(e_, lambda e: e.tensor_tensor(out=out, in0=a, in1=b, op=op), r=r, w=w)


def ts(k, e_, out, a, s1, s2, op0, op1, r, w):
    if op1 is None:
        return k.op(e_, lambda e: e.tensor_scalar(out=out, in0=a, scalar1=s1, scalar2=None, op0=op0), r=r, w=w)
    return k.op(e_, lambda e: e.tensor_scalar(out=out, in0=a, scalar1=s1, scalar2=s2, op0=op0, op1=op1), r=r, w=w)


def act(k, out, a, func, r, w, bias=None, scale=None):
    kw = {}
    if bias is not None:
        kw['bias'] = bias
    if scale is not None:
        kw['scale'] = scale
    return k.op("act", lambda e: e.activation(out=out, in_=a, func=func, **kw), r=r, w=w)


def frac_inplace(k, X, Ti, r, eng_cast="pool"):
    cp(k, eng_cast, Ti, X, [r[0]], [r[1]])
    tt(k, "pool", X, X, Ti, ALU.subtract, [r[0], r[1]], [r[0]])
    k.op("dve", lambda e: e.scalar_tensor_tensor(out=X, in0=X, scalar=0.0, in1=X, op0=ALU.is_lt, op1=ALU.add), r=[r[0]], w=[r[0]])


def rsqrt_inplace(k, X, r, bias_ap, rb, scale=1.0):
    k.op("act", lambda e: e.activation(out=X, in_=X, func=AF.Sqrt, bias=bias_ap, scale=scale), r=[r, rb], w=[r])
    k.op("dve", lambda e: e.reciprocal(out=X, in_=X), r=[r], w=[r])


def frac_pos_OLD(k, ph, x, name):
    n = x.t.shape[1]
    m = ph.sb([128, n], F32, name + "_m")
    ts(k, "pool", x[:], x[:], 1.0, None, ALU.mod, None, [x], [x])
    ts(k, "dve", m[:], x[:], 0.0, None, ALU.is_lt, None, [x], [m])
    tt(k, "dve", x[:], x[:], m[:], ALU.add, [x, m], [x])


def phase_s5(g, i):
    k, I, S, L, CTX, T = g.k, g.I, g.S, g.L, g.CTX, g.T
    u0 = FM['s5_u'][0]
    with g.c.phase() as ph:
        NS = 32
        are = ph.sb([128, NS], F32, "are"); aim = ph.sb([128, NS], F32, "aim"); dtt = ph.sb([128, NS], F32, "dtt")
        k.dma(are[:], I['s5_are'][i], w=[are]); k.dma(aim[:], I['s5_aim'][i], w=[aim]); k.dma(dtt[:], I['s5_ldt'][i], w=[dtt])
        negpi = ph.sb([128, 1], F32, "negpi")
        k.op("pool", lambda e: e.memset(negpi[:], -math.pi), w=[negpi])
        neghalf = ph.sb([128, 1], F32, "neghalf"); halfpi = ph.sb([128, 1], F32, "halfpi")
        k.op("pool", lambda e: e.memset(neghalf[:], -0.5), w=[neghalf])
        k.op("pool", lambda e: e.memset(halfpi[:], 0.5 * math.pi), w=[halfpi])
        act(k, dtt[:], dtt[:], AF.Exp, [dtt], [dtt])
        rho = ph.sb([128, NS], F32, "rho")
        tt(k, "dve", rho[:], are[:], dtt[:], ALU.mult, [are, dtt], [rho])
        act(k, rho[:], rho[:], AF.Exp, [rho], [rho])
        phi = ph.sb([128, NS], F32, "phi")
        tt(k, "dve", phi[:], aim[:], dtt[:], ALU.mult, [aim, dtt], [phi])
        ts(k, "dve", phi[:], phi[:], 1.0 / TWO_PI, None, ALU.mult, None, [phi], [phi])
        phii = ph.sb([128, NS], mybir.dt.int32, "phii")
        frac_inplace(k, phi[:], phii[:], [phi, phii])
        ph2 = ph.sb([128, NS], F32, "ph2")
        nsin = ph.sb([128, NS], F32, "nsin"); ncos = ph.sb([128, NS], F32, "ncos")
        act(k, nsin[:], phi[:], AF.Sin, [phi, negpi], [nsin], bias=negpi[:, 0:1], scale=TWO_PI)
        act(k, ph2[:], phi[:], AF.Abs, [phi, neghalf], [ph2], bias=neghalf[:, 0:1])
        act(k, ncos[:], ph2[:], AF.Sin, [ph2, halfpi], [ncos], bias=halfpi[:, 0:1], scale=-TWO_PI)
        abre = ph.sb([128, NS], F32, "abre"); abim = ph.sb([128, NS], F32, "abim")
        k.op("dve", lambda e: e.scalar_tensor_tensor(out=abre[:], in0=ncos[:], scalar=-1.0, in1=rho[:], op0=ALU.mult, op1=ALU.mult), r=[ncos, rho], w=[abre])
        k.op("dve", lambda e: e.scalar_tensor_tensor(out=abim[:], in0=nsin[:], scalar=-1.0, in1=rho[:], op0=ALU.mult, op1=ALU.mult), r=[nsin, rho], w=[abim])
        nr = ph.sb([128, NS], F32, "nr"); den = ph.sb([128, NS], F32, "den"); t1 = ph.sb([128, NS], F32, "t1"); t2 = ph.sb([128, NS], F32, "t2")
        core = ph.sb([128, NS], F32, "core"); coim = ph.sb([128, NS], F32, "coim")
        ts(k, "dve", nr[:], abre[:], -1.0, None, ALU.add, None, [abre], [nr])
        tt(k, "dve", den[:], are[:], are[:], ALU.mult, [are], [den])
        tt(k, "dve", t1[:], aim[:], aim[:], ALU.mult, [aim], [t1])
        tt(k, "dve", den[:], den[:], t1[:], ALU.add, [den, t1], [den])
        k.op("dve", lambda e: e.reciprocal(out=den[:], in_=den[:]), r=[den], w=[den])
        tt(k, "dve", t1[:], nr[:], are[:], ALU.mult, [nr, are], [t1])
        tt(k, "dve", t2[:], abim[:], aim[:], ALU.mult, [abim, aim], [t2])
        tt(k, "dve", t1[:], t1[:], t2[:], ALU.add, [t1, t2], [t1])
        tt(k, "dve", core[:], t1[:], den[:], ALU.mult, [t1, den], [core])
        tt(k, "dve", t1[:], abim[:], are[:], ALU.mult, [abim, are], [t1])
        tt(k, "dve", t2[:], nr[:], aim[:], ALU.mult, [nr, aim], [t2])
        tt(k, "dve", t1[:], t1[:], t2[:], ALU.subtract, [t1, t2], [t1])
        tt(k, "dve", coim[:], t1[:], den[:], ALU.mult, [t1, den], [coim])
        tv = ph.sb([128, T], F32, "tv")
        k.dma(tv[:], I['tvals'][:, 0:T], w=[tv])
        d32 = ph.sb([32, 16], F32, "d32")
        k.dma(d32[:], I['s5_d32'][i], w=[d32])
        cosT = ph.sb([128, T], F32, "cosT"); sinT = ph.sb([128, T], F32, "sinT")
        rhoT = ph.sb([128, T], F32, "rhoT")
        zre = ph.sb([128, T], F32, "zre"); zim = ph.sb([128, T], F32, "zim")
        gre = ph.sb([128, T], F32, "gre"); gim = ph.sb([128, T], F32, "gim")
        pht = gre
        hre = ph.sb([128, T], BF16, "hre"); him = ph.sb([128, T], BF16, "him")
        uf = gim; ub = ph.sb([32, T], BF16, "ub")
        yF = ph.sb([32, T], F32, "yF")
        zr = ph.sb([128, 512], F32, "zr"); zi = ph.sb([128, 512], F32, "zi")
        ta = ph.sb([128, 512], F32, "ta"); tb_ = ph.sb([128, 512], F32, "tb")
        tc_ = zre; td_ = zim
        bre_f = ph.sb([32, 128], F32, "bref"); bim_f = ph.sb([32, 128], F32, "bimf")
        bre = ph.sb([32, 128], BF16, "bre"); bim = ph.sb([32, 128], BF16, "bim")
        cre_f = ph.sb([128, 32], F32, "cref"); cim_f = ph.sb([128, 32], F32, "cimf")
        cre = ph.sb([128, 32], BF16, "cre"); cimn = ph.sb([128, 32], BF16, "cimn")
        ct1 = ph.sb([128, 32], F32, "ct1"); ct2 = ph.sb([128, 32], F32, "ct2")
        pz = [ph.ps([128, 512], F32, "pzr"), ph.ps([128, 512], F32, "pzi")]
        py = [ph.ps([32, 512], F32, "py%d" % j) for j in range(2)]
        npy = 0
        for gp in range(16):
            k.dma(uf[0:32, :], S['pfm'][u0 + gp * 32:u0 + gp * 32 + 32, :], w=[uf])
            k.op("pool", lambda e: e.tensor_copy(out=ub[:], in_=uf[0:32, :]), r=[uf], w=[ub])
            ts(k, "dve", yF[:], uf[0:32, :], d32[:, gp:gp + 1], None, ALU.mult, None, [uf, d32], [yF])
            for dr in range(2):
                col = dr * 16 + gp
                k.dma(bre_f[:], I['s5_Bre'][i, dr, gp], w=[bre_f]); k.dma(bim_f[:], I['s5_Bim'][i, dr, gp], w=[bim_f])
                k.dma(cre_f[:], I['s5_Cre'][i, dr, gp], w=[cre_f]); k.dma(cim_f[:], I['s5_Cim'][i, dr, gp], w=[cim_f])
                k.op("pool", lambda e: e.tensor_copy(out=bre[:], in_=bre_f[:]), r=[bre_f], w=[bre])
                k.op("pool", lambda e: e.tensor_copy(out=bim[:], in_=bim_f[:]), r=[bim_f], w=[bim])
                ts(k, "dve", ct1[:], cre_f[:], core[:, col:col + 1], None, ALU.mult, None, [cre_f, core], [ct1])
                ts(k, "dve", ct2[:], cim_f[:], coim[:, col:col + 1], None, ALU.mult, None, [cim_f, coim], [ct2])
                tt(k, "dve", cre[:], ct1[:], ct2[:], ALU.subtract, [ct1, ct2], [cre])
                ts(k, "dve", ct1[:], cre_f[:], coim[:, col:col + 1], None, ALU.mult, None, [cre_f, coim], [ct1])
                ts(k, "dve", ct2[:], cim_f[:], core[:, col:col + 1], None, ALU.mult, None, [cim_f, core], [ct2])
                k.op("dve", lambda e: e.scalar_tensor_tensor(out=cimn[:], in0=ct1[:], scalar=-1.0, in1=ct2[:], op0=ALU.mult, op1=ALU.subtract), r=[ct1, ct2], w=[cimn])
                ts(k, "dve", pht[:], tv[:], phi[:, col:col + 1], None, ALU.mult, None, [tv, phi], [pht])
                frac_inplace(k, pht[:], zim[:].bitcast(mybir.dt.int32), [pht, zim])
                act(k, sinT[:], pht[:], AF.Sin, [pht, negpi], [sinT], bias=negpi[:, 0:1], scale=TWO_PI)
                act(k, pht[:], pht[:], AF.Abs, [pht, neghalf], [pht], bias=neghalf[:, 0:1])
                act(k, cosT[:], pht[:], AF.Sin, [pht, halfpi], [cosT], bias=halfpi[:, 0:1], scale=-TWO_PI)
                ts(k, "dve", rhoT[:], tv[:], 0.0, rho[:, col:col + 1], ALU.mult, ALU.add, [tv, rho], [rhoT])
                blocks = []
                if dr == 0:
                    for n0 in range(0, T, 512):
                        nn = min(512, T - n0)
                        blocks.append((n0, nn, slice(n0, n0 + nn)))
                else:
                    def rs(hi, nn):
                        lo = hi - nn
                        return slice(hi - 1, lo - 1 if lo > 0 else None, -1)
                    n0 = 0
                    for c0 in range(CTX, 0, -512):
                        nn = min(512, c0)
                        blocks.append((n0, nn, rs(c0, nn))); n0 += nn
                    for c0 in range(T, CTX, -512):
                        nn = min(512, c0 - CTX)
                        blocks.append((n0, nn, rs(c0, nn))); n0 += nn
                for (n0, nn, cs) in blocks:
                    k.op("pe", lambda e, cs=cs, nn=nn: e.matmul(pz[0][:, 0:nn], lhsT=bre[:], rhs=ub[:, cs], start=True, stop=True), r=[bre, ub], w=[pz[0]])
                    k.op("pe", lambda e, cs=cs, nn=nn: e.matmul(pz[1][:, 0:nn], lhsT=bim[:], rhs=ub[:, cs], start=True, stop=True), r=[bim, ub], w=[pz[1]])
                    k.op("act", lambda e, nn=nn: e.copy(out=zr[:, 0:nn], in_=pz[0][:, 0:nn]), r=[pz[0]], w=[zr])
                    k.op("act", lambda e, nn=nn: e.copy(out=zi[:, 0:nn], in_=pz[1][:, 0:nn]), r=[pz[1]], w=[zi])
                    ns = slice(n0, n0 + nn)
                    tt(k, "dve", ta[:, 0:nn], zr[:, 0:nn], cosT[:, ns], ALU.mult, [zr, cosT], [ta])
                    tt(k, "pool", tb_[:, 0:nn], zi[:, 0:nn], sinT[:, ns], ALU.mult, [zi, sinT], [tb_])
                    tt(k, "dve", zre[:, ns], ta[:, 0:nn], tb_[:, 0:nn], ALU.add, [ta, tb_], [zre])
                    tt(k, "pool", ta[:, 0:nn], zi[:, 0:nn], cosT[:, ns], ALU.mult, [zi, cosT], [ta])
                    tt(k, "dve", tb_[:, 0:nn], zr[:, 0:nn], sinT[:, ns], ALU.mult, [zr, sinT], [tb_])
                    tt(k, "pool", zim[:, ns], ta[:, 0:nn], tb_[:, 0:nn], ALU.subtract, [ta, tb_], [zim])
                k.op("dve", lambda e: e.tensor_tensor_scan(out=gre[:], data0=rhoT[:], data1=zre[:], initial=0.0, op0=ALU.mult, op1=ALU.add), r=[rhoT, zre], w=[gre])
                k.op("dve", lambda e: e.tensor_tensor_scan(out=gim[:], data0=rhoT[:], data1=zim[:], initial=0.0, op0=ALU.mult, op1=ALU.add), r=[rhoT, zim], w=[gim])
                tt(k, "pool", tc_[:], gre[:], cosT[:], ALU.mult, [gre, cosT], [tc_])
                tt(k, "dve", td_[:], gim[:], sinT[:], ALU.mult, [gim, sinT], [td_])
                tt(k, "pool", hre[:], tc_[:], td_[:], ALU.subtract, [tc_, td_], [hre])
                tt(k, "dve", tc_[:], gre[:], sinT[:], ALU.mult, [gre, sinT], [tc_])
                tt(k, "pool", td_[:], gim[:], cosT[:], ALU.mult, [gim, cosT], [td_])
                tt(k, "dve", him[:], tc_[:], td_[:], ALU.add, [tc_, td_], [him])
                for (n0, nn, cs) in blocks:
                    p_ = py[npy % 2]; npy += 1
                    ns = slice(n0, n0 + nn)
                    k.op("pe", lambda e, p_=p_, ns=ns, nn=nn: e.matmul(p_[:, 0:nn], lhsT=cre[:], rhs=hre[:, ns], start=True, stop=False), r=[cre, hre], w=[p_])
                    k.op("pe", lambda e, p_=p_, ns=ns, nn=nn: e.matmul(p_[:, 0:nn], lhsT=cimn[:], rhs=him[:, ns], start=False, stop=True), r=[cimn, him], w=[p_])
                    tt(k, "dve", yF[:, cs], yF[:, cs], p_[:, 0:nn], ALU.add, [yF, p_], [yF])
            k.dma(S['ys5'][gp * 32:gp * 32 + 32, :], yF[:], r=[yF])
    with g.c.phase() as ph:
        gw_f = ph.sb([128, 4, 512], F32, "gwf"); gw = ph.sb([128, 4, 512], BF16, "gw")
        k.dma(gw_f[:], I['s5_glu_w'][i].rearrange("(k p) c -> p k c", p=128), w=[gw_f])
        k.op("pool", lambda e: e.tensor_copy(out=gw[:], in_=gw_f[:]), r=[gw_f], w=[gw])
        gb = ph.sb([128, 4], F32, "gb")
        k.dma(gb[:], I['s5_glu_b4'][i], w=[gb])
        ys = [ph.sb([128, 512], F32, "ys%d" % j) for j in range(4)]
        Gf = [ph.sb([128, 512], F32, "Gf%d" % j) for j in range(4)]
        Gb = [ph.sb([128, 512], BF16, "Gb%d" % j) for j in range(4)]
        w1 = ph.sb([128, 512], F32, "w1"); w2 = ph.sb([128, 512], F32, "w2")
        gt_ = [ph.sb([128, 512], F32, "gt%d" % j) for j in range(2)]
        ob = [ph.sb([128, 512], BF16, "ob%d" % j) for j in range(2)]
        pg = [ph.ps([128, 512], F32, "pg%d" % j) for j in range(2)]
        g0 = FM['s5_g'][0]
        n_ = 0
        for n0 in range(0, T, 512):
            nn = min(512, T - n0)
            for j in range(4):
                k.dma(ys[j][:, 0:nn], S['ys5'][j * 128:(j + 1) * 128, n0:n0 + nn], w=[ys[j]])
                gelu_tanh(k, Gf[j], Gb[j], ys[j], w1, w2, nn)
            for j in range(4):
                p_ = pg[n_ % 2]; g_ = gt_[n_ % 2]; o_ = ob[n_ % 2]; n_ += 1
                for kt in range(4):
                    k.op("pe", lambda e, p_=p_, kt=kt, j=j: e.matmul(p_[:, 0:nn], lhsT=gw[:, kt, j * 128:(j + 1) * 128], rhs=Gb[kt][:, 0:nn], start=(kt == 0), stop=(kt == 3)),
                         r=[gw, Gb[kt]], w=[p_])
                act(k, w1[:, 0:nn], p_[:, 0:nn], AF.Sigmoid, [p_, gb], [w1], bias=gb[:, j:j + 1])
                tt(k, "dve", w1[:, 0:nn], w1[:, 0:nn], Gf[j][:, 0:nn], ALU.mult, [w1, Gf[j]], [w1])
                k.dma(g_[:, 0:nn], S['pfm'][g0 + j * 128:g0 + (j + 1) * 128, n0:n0 + nn], w=[g_])
                act(k, g_[:, 0:nn], g_[:, 0:nn], AF.Silu, [g_], [g_])
                tt(k, "dve", o_[:, 0:nn], w1[:, 0:nn], g_[:, 0:nn], ALU.mult, [w1, g_], [o_])
                k.dma(S['y'][j * 128:(j + 1) * 128, n0:n0 + nn], o_[:, 0:nn], r=[o_])


def gelu_tanh(k, outf, outb, x, w1, w2, nn):
    tt(k, "pool", w1[:, 0:nn], x[:, 0:nn], x[:, 0:nn], ALU.mult, [x], [w1])
    ts(k, "dve", w1[:, 0:nn], w1[:, 0:nn], 0.044715, 1.0, ALU.mult, ALU.add, [w1], [w1])
    tt(k, "dve", w2[:, 0:nn], w1[:, 0:nn], x[:, 0:nn], ALU.mult, [w1, x], [w2])
    act(k, w2[:, 0:nn], w2[:, 0:nn], AF.Sigmoid, [w2], [w2], scale=2.0 * math.sqrt(2.0 / math.pi))
    tt(k, "dve", outf[:, 0:nn], w2[:, 0:nn], x[:, 0:nn], ALU.mult, [w2, x], [outf])
    k.op("pool", lambda e: e.tensor_copy(out=outb[:, 0:nn], in_=outf[:, 0:nn]), r=[outf], w=[outb])


def lin_attn_tile(g, ph, B_, q32, k32, laF, laB, vts, C, dk, yrow0, grow0, nw_cols, layer, nwoff=0):
    k, I, S, L, CTX, T = g.k, g.I, g.S, g.L, g.CTX, g.T
    HPT = 128 // dk
    NCH = T // C
    ncc = CTX // C
    for dr in range(2):
        la = laF if dr == 0 else laB
        b = B_['b']
        if dr == 0:
            k.op("dve", lambda e: e.tensor_tensor_scan(out=b[:], data0=B_['m01'][:, 0:T], data1=la[:], initial=0.0, op0=ALU.mult, op1=ALU.add), r=[B_['m01'], la], w=[b])
        else:
            k.op("dve", lambda e: e.tensor_tensor_scan(out=b[:, ::-1], data0=B_['m01'][:, T:0:-1], data1=la[:, ::-1], initial=0.0, op0=ALU.mult, op1=ALU.add), r=[B_['m01'], la], w=[b])
        b3 = b[:].rearrange("p (n c) -> p n c", c=C)
        rc = C // 2 - 1 if dr == 0 else C // 2
        lc = C - 1 if dr == 0 else 0
        ref = b3[:, :, rc:rc + 1].to_broadcast([128, NCH, C])
        bl = b3[:, :, lc:lc + 1].to_broadcast([128, NCH, C])
        tmp = la
        t3 = tmp[:].rearrange("p (n c) -> p n c", c=C)
        tt(k, "pool", t3, b3, ref, ALU.subtract, [b], [tmp])
        act(k, tmp[:], tmp[:], AF.Exp, [tmp], [tmp])
        tt(k, "dve", B_['qt'][:], q32[:], tmp[:], ALU.mult, [q32, tmp], [B_['qt']])
        tt(k, "pool", t3, ref, b3, ALU.subtract, [b], [tmp])
        act(k, tmp[:], tmp[:], AF.Exp, [tmp], [tmp])
        tt(k, "dve", B_['kt'][:], k32[:], tmp[:], ALU.mult, [k32, tmp], [B_['kt']])
        tt(k, "pool", t3, bl, b3, ALU.subtract, [b], [tmp])
        act(k, tmp[:], tmp[:], AF.Exp, [tmp], [tmp])
        tt(k, "dve", B_['ks'][:], k32[:], tmp[:], ALU.mult, [k32, tmp], [B_['ks']])
        dec = B_['dec']
        k.op("act", lambda e: e.activation(out=dec[:], in_=b3[:, :, lc], func=AF.Exp), r=[b], w=[dec])
        if g.cut == 'prep':
            continue
        kT = B_['kT']
        per = 1024 // 128
        for j0 in range(0, NCH, per):
            nj = min(per, NCH - j0)
            p_ = B_['pT'][(j0 // per) % 2]
            for jj in range(nj):
                j = j0 + jj
                k.op("pe", lambda e, p_=p_, jj=jj, j=j: e.transpose(out=p_[0:C, jj * 128:(jj + 1) * 128], in_=B_['ks'][:, j * C:(j + 1) * C], identity=B_['ident'][:]),
                     r=[B_['ks'], B_['ident']], w=[p_])
            src = p_[0:C, 0:nj * 128].rearrange("p (a b) -> p a b", b=128)
            k.op("dve", lambda e, src=src, j0=j0, nj=nj: e.tensor_copy(out=kT[0:C, j0:j0 + nj, :], in_=src), r=[p_], w=[kT])
        act(k, tmp[:], b[:], AF.Exp, [b], [tmp])
        tt(k, "dve", B_['qs'][:], q32[:], tmp[:], ALU.mult, [q32, tmp], [B_['qs']])
        if g.cut == 'tr':
            continue
        if dr == 0:
            order = list(range(NCH))
        else:
            order = list(range(ncc - 1, -1, -1)) + list(range(NCH - 1, ncc - 1, -1))
        St = B_['St']
        Sall = B_['Sall']
        k.op("pool", lambda e: e.memset(St[:], 0.0), w=[St])
        for j in order:
            k.op("pool", lambda e, j=j: e.tensor_copy(out=Sall[:, j, :], in_=St[:]), r=[St], w=[Sall])
            for hh in range(HPT):
                p_ = B_['pkv'][hh]
                k.op("pe", lambda e, p_=p_, j=j, hh=hh: e.matmul(p_[:, 0:128], lhsT=kT[0:C, j, :], rhs=vts[hh][0:C, j, :], start=True, stop=True), r=[kT, vts[hh]], w=[p_])
                rs_ = slice(hh * dk, (hh + 1) * dk)
                k.op("dve", lambda e, p_=p_, j=j, rs_=rs_: e.scalar_tensor_tensor(out=St[rs_, :], in0=St[rs_, :], scalar=dec[rs_, j:j + 1], in1=p_[rs_, 0:128], op0=ALU.mult, op1=ALU.add),
                     r=[St, dec, p_], w=[St])
        if g.cut == 'A':
            continue
        mk = B_['maskF'] if dr == 0 else B_['maskB']
        na = 0
        for j in range(NCH):
            cs = slice(j * C, (j + 1) * C)
            for hh in range(HPT):
                rs_ = slice(hh * dk, (hh + 1) * dk)
                pa = B_['patt'][na % 2]; po = B_['po'][na % 2]; am = B_['am'][na % 2]; na += 1
                k.op("pe", lambda e, pa=pa, rs_=rs_, cs=cs: e.matmul(pa[0:C, 0:C], lhsT=B_['kt'][rs_, cs], rhs=B_['qt'][rs_, cs], start=True, stop=True), r=[B_['kt'], B_['qt']], w=[pa])
                tt(k, "dve", am[0:C, 0:C], pa[0:C, 0:C], mk[0:C, 0:C], ALU.mult, [pa, mk], [am])
                o_ = B_['o'][hh]
                if hh == 0:
                    k.op("pe", lambda e, po=po, j=j, hh=hh, am=am: e.matmul(po[:, 0:C], lhsT=vts[hh][0:C, j, :], rhs=am[0:C, 0:C], start=True, stop=False), r=[vts[hh], am], w=[po])
                    k.op("pe", lambda e, po=po, j=j, rs_=rs_, cs=cs: e.matmul(po[:, 0:C], lhsT=Sall[rs_, j, :], rhs=B_['qs'][rs_, cs], start=False, stop=True), r=[Sall, B_['qs']], w=[po])
                    po2 = None
                else:
                    po2 = B_['pkv'][na % 2]
                    k.op("pe", lambda e, po=po, j=j, hh=hh, am=am: e.matmul(po[:, 0:C], lhsT=vts[hh][0:C, j, :], rhs=am[0:C, 0:C], start=True, stop=True), r=[vts[hh], am], w=[po])
                    k.op("pe", lambda e, po2=po2, j=j, rs_=rs_, cs=cs: e.matmul(po2[:, 0:C], lhsT=Sall[rs_, j, :], rhs=B_['qs'][rs_, cs], start=True, stop=True), r=[Sall, B_['qs']], w=[po2])
                if dr == 0:
                    k.op("act", lambda e, o_=o_, po=po, cs=cs: e.copy(out=o_[:, cs], in_=po[:, 0:C]), r=[po], w=[o_])
                else:
                    tt(k, "dve", o_[:, cs], o_[:, cs], po[:, 0:C], ALU.add, [o_, po], [o_])
                if po2 is not None:
                    tt(k, "dve", o_[:, cs], o_[:, cs], po2[:, 0:C], ALU.add, [o_, po2], [o_])
    if g.cut in ('prep', 'tr', 'A', 'B'):
        return
    for hh in range(HPT):
        o_ = B_['o'][hh]
        r0 = yrow0 + hh * 128
        for n0 in range(0, T, 512):
            nn = min(512, T - n0)
            ns = slice(n0, n0 + nn)
            sq = B_['w512'][0]; gt_ = B_['w512'][1]; ob = B_['ob']
            tt(k, "pool", sq[:, 0:nn], o_[:, ns], o_[:, ns], ALU.mult, [o_], [sq])
            pm = B_['patt'][0]
            k.op("pe", lambda e, pm=pm, sq=sq, nn=nn: e.matmul(pm[:, 0:nn], lhsT=B_['onesm'][:], rhs=sq[:, 0:nn], start=True, stop=True), r=[B_['onesm'], sq], w=[pm])
            k.op("act", lambda e, sq=sq, pm=pm, nn=nn: e.activation(out=sq[:, 0:nn], in_=pm[:, 0:nn], func=AF.Sqrt, bias=B_['epsb'][:, 0:1]), r=[pm, B_['epsb']], w=[sq])
            k.op("dve", lambda e, sq=sq, nn=nn: e.reciprocal(out=sq[:, 0:nn], in_=sq[:, 0:nn]), r=[sq], w=[sq])
            tt(k, "dve", sq[:, 0:nn], sq[:, 0:nn], o_[:, ns], ALU.mult, [sq, o_], [sq])
            k.dma(gt_[:, 0:nn], S['pfm'][grow0 + hh * 128:grow0 + (hh + 1) * 128, n0:n0 + nn], w=[gt_])
            act(k, gt_[:, 0:nn], gt_[:, 0:nn], AF.Silu, [gt_], [gt_])
            k.op("dve", lambda e, sq=sq, gt_=gt_, nn=nn, hh=hh: e.scalar_tensor_tensor(out=ob[:, 0:nn], in0=sq[:, 0:nn], scalar=nw_cols[:, nwoff + hh:nwoff + hh + 1], in1=gt_[:, 0:nn], op0=ALU.mult, op1=ALU.mult),
                 r=[sq, gt_, nw_cols], w=[ob])
            k.dma(S['y'][r0:r0 + 128, n0:n0 + nn], ob[:, 0:nn], r=[ob])


def la_bufs(g, ph, C):
    k, I, T = g.k, g.I, g.T
    NCH = T // C
    B_ = {}
    for nm in ('b',):
        B_[nm] = ph.sb([128, T], F32, nm)
    for nm in ('qt', 'kt', 'ks'):
        B_[nm] = ph.sb([128, T], BF16, nm)
    B_['qs'] = B_['ks']
    B_['m01'] = ph.sb([128, T + 1], F32, "m01")
    k.dma(B_['m01'][:], I['m01_%d' % C][:, :], w=[B_['m01']])
    B_['dec'] = ph.sb([128, NCH], F32, "dec")
    B_['kT'] = ph.sb([128, NCH, 128], BF16, "kT")
    B_['Sall'] = ph.sb([128, NCH, 128], BF16, "Sall")
    B_['St'] = ph.sb([128, 128], F32, "St")
    B_['ident'] = ph.sb([128, 128], BF16, "identb")
    k.dma(B_['ident'][:], I['ident_bf'][:, :], w=[B_['ident']])
    B_['maskF'] = ph.sb([128, 128], F32, "maskF"); B_['maskB'] = ph.sb([128, 128], F32, "maskB")
    k.dma(B_['maskF'][:], I['maskF'][:, :], w=[B_['maskF']]); k.dma(B_['maskB'][:], I['maskB'][:, :], w=[B_['maskB']])
    B_['epsb'] = ph.sb([128, 1], F32, "lepsb")
    k.op("pool", lambda e: e.memset(B_['epsb'][:], EPS), w=[B_['epsb']])
    B_['onesm'] = ph.sb([128, 128], F32, "onesm")
    k.op("pool", lambda e: e.memset(B_['onesm'][:], 1.0 / 128.0), w=[B_['onesm']])
    B_['pT'] = [ph.ps([128, 1024], BF16, "lpT%d" % j) for j in range(2)]
    B_['pkv'] = [ph.ps([128, 512], F32, "pkv%d" % j) for j in range(2)]
    B_['patt'] = [ph.ps([128, 512], F32, "patt%d" % j) for j in range(2)]
    B_['po'] = [ph.ps([128, 512], F32, "po%d" % j) for j in range(2)]
    B_['am'] = [ph.sb([128, 128], BF16, "am%d" % j) for j in range(2)]
    B_['w512'] = [ph.sb([128, 512], F32, "w512_%d" % j) for j in range(2)]
    B_['ob'] = ph.sb([128, 512], BF16, "ob")
    return B_


def load_v(g, ph, C, col0, name, B_):
    k, S, T = g.k, g.S, g.T
    NCH = T // C
    halves = (NCH * 128) // T
    nph = NCH // halves
    b = B_['b']
    bv = b[0:C, :].rearrange("p (n e) -> p n e", e=128)
    vb = ph.sb([C, NCH, 128], BF16, name)
    for hf in range(halves):
        r0 = hf * nph * C
        k.dma(bv, S['ptm'][r0:r0 + nph * C, col0:col0 + 128].rearrange("(n c) e -> c n e", c=C), w=[b])
        k.op("pool", lambda e, hf=hf: e.tensor_copy(out=vb[:, hf * nph:(hf + 1) * nph, :], in_=bv), r=[b], w=[vb])
    return vb


def phase_ret(g, i):
    k, I, S, L, CTX, T = g.k, g.I, g.S, g.L, g.CTX, g.T
    for h in range(4):
        with g.c.phase() as ph:
            B_ = la_bufs(g, ph, 128)
            B_['o'] = [ph.sb([128, T], F32, "o0")]
            rc_ = ph.sb([128, T], F32, "ropeC"); rs_ = ph.sb([128, T], F32, "ropeS")
            k.dma(rc_[:], I['ropeC'][:, 0:T], w=[rc_]); k.dma(rs_[:], I['ropeS'][:, 0:T], w=[rs_])
            q32 = ph.sb([128, T], F32, "q32"); k32 = ph.sb([128, T], F32, "k32")
            x2 = B_['b']
            for (dst, a, b_, scale) in ((q32, 'ret_q', 'ret_qs', 1.0), (k32, 'ret_k', 'ret_ks', 128.0 ** -0.5)):
                k.dma(dst[:], S['pfm'][FM[a][0] + h * 128:FM[a][0] + (h + 1) * 128, :], w=[dst])
                k.dma(x2[:], S['pfm'][FM[b_][0] + h * 128:FM[b_][0] + (h + 1) * 128, :], w=[x2])
                tt(k, "dve", dst[:], dst[:], rc_[:], ALU.mult, [dst, rc_], [dst])
                tt(k, "pool", x2[:], x2[:], rs_[:], ALU.mult, [x2, rs_], [x2])
                if scale == 1.0:
                    tt(k, "dve", dst[:], dst[:], x2[:], ALU.add, [dst, x2], [dst])
                else:
                    tt(k, "dve", dst[:], dst[:], x2[:], ALU.add, [dst, x2], [dst])
                    ts(k, "dve", dst[:], dst[:], scale, None, ALU.mult, None, [dst], [dst])
            lg = math.log(1.0 - 2.0 ** (-5.0 - h))
            laF = rc_; laB = rs_
            k.op("pool", lambda e: e.memset(laF[:], lg), w=[laF])
            k.op("pool", lambda e: e.memset(laB[:], lg), w=[laB])
            vts = [load_v(g, ph, 128, h * 128, "vr", B_)]
            nwc = ph.sb([128, 4], F32, "nwc")
            k.dma(nwc[:], I['ret_nw'][i], w=[nwc])
            lin_attn_tile(g, ph, B_, q32, k32, laF, laB, vts, 128, 128, 1024 + h * 128, FM['ret_g'][0] + h * 128, nwc, i, nwoff=h)


def phase_gla(g, i):
    k, I, S, L, CTX, T = g.k, g.I, g.S, g.L, g.CTX, g.T
    for tl in range(2):
        with g.c.phase() as ph:
            B_ = la_bufs(g, ph, 64)
            B_['o'] = [ph.sb([128, T], BF16, "o%d" % j) for j in range(2)]
            las = [ph.sb([128, T], F32, "laF"), ph.sb([128, T], F32, "laB")]
            q32 = ph.sb([128, T], BF16, "q16"); k32 = ph.sb([128, T], BF16, "k16")
            k.dma(las[0][:], S['pfm'][FM['gla_q'][0] + tl * 128:FM['gla_q'][0] + (tl + 1) * 128, :], w=[las[0]])
            k.dma(las[1][:], S['pfm'][FM['gla_k'][0] + tl * 128:FM['gla_k'][0] + (tl + 1) * 128, :], w=[las[1]])
            ts(k, "dve", q32[:], las[0][:], 64.0 ** -0.5, None, ALU.mult, None, [las[0]], [q32])
            cp(k, "dve", k32[:], las[1][:], [las[1]], [k32])
            lrb = B_['b']
            lr = lrb[0:32, :]
            k.dma(lr, S['pfm'][FM['gla_lr'][0]:FM['gla_lr'][0] + 32, :], w=[lrb])
            gwt = ph.sb([32, 2, 128], F32, "gwt")
            k.dma(gwt[:], I['gla_gw'][i, tl], w=[gwt])
            gbt = ph.sb([128, 2], F32, "gbt")
            k.dma(gbt[:], I['gla_gb'][i, tl], w=[gbt])
            ts(k, "dve", gbt[:], gbt[:], -1.0, None, ALU.mult, None, [gbt], [gbt])
            pl_ = B_['patt']
            for dr in range(2):
                for bi, n0 in enumerate(range(0, T, 512)):
                    nn = min(512, T - n0)
                    p_ = pl_[bi % 2]
                    k.op("pe", lambda e, p_=p_, dr=dr, n0=n0, nn=nn: e.matmul(p_[:, 0:nn], lhsT=gwt[:, dr, :], rhs=lr[:, n0:n0 + nn], start=True, stop=True), r=[gwt, lrb], w=[p_])
                    negb = B_['w512'][0]
                    act(k, negb[:, 0:nn], p_[:, 0:nn], AF.Exp, [p_, gbt], [negb], bias=gbt[:, dr:dr + 1], scale=-1.0)
                    ts(k, "dve", negb[:, 0:nn], negb[:, 0:nn], 1.0, None, ALU.add, None, [negb], [negb])
                    act(k, negb[:, 0:nn], negb[:, 0:nn], AF.Ln, [negb], [negb])
                    ts(k, "dve", las[dr][:, n0:n0 + nn], negb[:, 0:nn], -1.0 / 16.0, None, ALU.mult, None, [negb], [las[dr]])
            vts = [load_v(g, ph, 64, 512 + (tl * 2 + hh) * 128, "vg%d" % hh, B_) for hh in range(2)]
            nwc = ph.sb([128, 4], F32, "nwc")
            k.dma(nwc[:], I['gla_nw'][i], w=[nwc])
            lin_attn_tile(g, ph, B_, q32, k32, las[0], las[1], vts, 64, 64, 1536 + tl * 256, FM['gla_g'][0] + tl * 256, nwc, i, nwoff=tl * 2)


def cp(k, e_, out, in_, r, w):
    if e_ == "act":
        return k.op("act", lambda e: e.copy(out=out, in_=in_), r=r, w=w)
    return k.op(e_, lambda e: e.tensor_copy(out=out, in_=in_), r=r, w=w)


def phase_hyena(g, i, Lh, tok0, tg):
    k, I, S = g.k, g.I, g.S
    N = 2 * Lh
    NT, LT = N // 128, Lh // 128
    hx0 = FM['hy_x'][0]
    PI = math.pi
    with g.c.phase() as ph:
        w1 = ph.sb([33, 64], F32, "hw1"); w2 = ph.sb([64, 64], F32, "hw2"); w3 = ph.sb([64, 64], F32, "hw3"); w4 = ph.sb([64, 2048], F32, "hw4")
        k.dma(w1[:], I['hy_w1'][i], w=[w1]); k.dma(w2[:], I['hy_w2'][i], w=[w2]); k.dma(w3[:], I['hy_w3'][i], w=[w3]); k.dma(w4[:], I['hy_w4'][i], w=[w4])
        bf = ph.sb([64, 6], F32, "hbf")
        k.dma(bf[:], I['hy_bf'][i], w=[bf])
        fb = ph.sb([64, 3], F32, "hfb")
        fs = ph.sb([64, 3], F32, "hfs")
        for l_ in range(3):
            tt(k, "dve", fb[:, l_:l_ + 1], bf[:, 2 * l_:2 * l_ + 1], bf[:, 2 * l_ + 1:2 * l_ + 2], ALU.mult, [bf], [fb])
            ts(k, "dve", fs[:, l_:l_ + 1], bf[:, 2 * l_ + 1:2 * l_ + 2], 1.0 / (2.0 * PI), None, ALU.mult, None, [bf], [fs])
        ts(k, "dve", fb[:], fb[:], 1.0 / (2.0 * PI), 0.5, ALU.mult, ALU.add, [fb], [fb])
        negpi = ph.sb([64, 1], F32, "hnegpi")
        k.op("pool", lambda e: e.memset(negpi[:], -PI), w=[negpi])
        zT = ph.sb([33, N], F32, "hzT")
        k.dma(zT[:], I['hy_z' + tg][:, :], w=[zT])
        hA = ph.sb([64, N], F32, "hA"); hB = ph.sb([64, N], F32, "hB")
        pm = [ph.ps([64, 512], F32, "hpm%d" % j) for j in range(2)]
        a_ = ph.sb([64, 512], F32, "ha"); ai_ = ph.sb([64, 512], mybir.dt.int32, "hai")
        srcs = [(w1, zT, 33), (w2, hA, 64), (w3, hB, 64)]
        dsts = [hA, hB, hA]
        nb = 0
        for l_ in range(3):
            w_, src, kk = srcs[l_]
            dst = dsts[l_]
            BK = min(512, N)
            for n0 in range(0, N, BK):
                p_ = pm[nb % 2]; nb += 1
                k.op("pe", lambda e, p_=p_, w_=w_, src=src, kk=kk, n0=n0: e.matmul(p_[:, 0:BK], lhsT=w_[0:kk, :], rhs=src[0:kk, n0:n0 + BK], start=True, stop=True), r=[w_, src], w=[p_])
                k.op("act", lambda e, p_=p_, l_=l_: e.activation(out=a_[:, 0:BK], in_=p_[:, 0:BK], func=AF.Identity, bias=fb[:, l_:l_ + 1], scale=fs[:, l_:l_ + 1]), r=[p_, fb, fs], w=[a_])
                frac_inplace(k, a_[:, 0:BK], ai_[:, 0:BK], [a_, ai_])
                act(k, dst[:, n0:n0 + BK], a_[:, 0:BK], AF.Sin, [a_, negpi], [dst], bias=negpi[:, 0:1], scale=2.0 * PI)
        h3 = hA
        ones = ph.sb([128, 128], F32, "hones")
        k.op("pool", lambda e: e.memset(ones[:], 1.0), w=[ones])
        pk = [ph.ps([128, 512], F32, "hpk%d" % j) for j in range(2)]
        pss = [ph.ps([128, 512], F32, "hpss%d" % j) for j in range(2)]
        dct = [ph.sb([128, 512], F32, "hdc%d" % j) for j in range(2)]
        kt_ = [ph.sb([128, 1024], F32, "hkt%d" % j) for j in range(2)]
        sq = ph.sb([128, 1024], F32, "hsq")
        kb = [ph.sb([128, 1024], BF16, "hkb%d" % j) for j in range(2)]
        for nt in range(NT):
            dr = 0 if nt * 128 < Lh else 1
            dc = dct[nt % 2]; ktile = kt_[nt % 2]; kbt = kb[nt % 2]
            k.dma(dc[:], I['hy_dec' + tg][nt * 128:(nt + 1) * 128, :], w=[dc])
            for o in range(2):
                k.op("pe", lambda e, o=o, nt=nt, dr=dr: e.matmul(pk[o][:, :], lhsT=h3[:, nt * 128:(nt + 1) * 128], rhs=w4[:, dr * 1024 + o * 512:dr * 1024 + (o + 1) * 512], start=True, stop=True), r=[h3, w4], w=[pk[o]])
                tt(k, "dve", ktile[:, o * 512:(o + 1) * 512], pk[o][:, :], dc[:], ALU.mult, [pk[o], dc], [ktile])
            tt(k, "pool", sq[:], ktile[:], ktile[:], ALU.mult, [ktile], [sq])
            for o in range(2):
                k.op("pe", lambda e, o=o, nt=nt: e.matmul(pss[o][:, :], lhsT=ones[:], rhs=sq[:, o * 512:(o + 1) * 512], start=(nt == 0), stop=(nt == NT - 1)), r=[ones, sq], w=[pss[o]])
            cp(k, "act", kbt[:], ktile[:], [ktile], [kbt])
            k.dma(S['hk'][nt * 128:(nt + 1) * 128, :], kbt[:], r=[kbt])
        rs = ph.sb([128, 1024], F32, "hrs")
        hepsb = ph.sb([128, 1], F32, "hepsb")
        k.op("pool", lambda e: e.memset(hepsb[:], EPS), w=[hepsb])
        for o in range(2):
            k.op("act", lambda e, o=o: e.activation(out=rs[:, o * 512:(o + 1) * 512], in_=pss[o][:, :], func=AF.Sqrt, bias=hepsb[:, 0:1]), r=[pss[o], hepsb], w=[rs])
        k.op("dve", lambda e: e.reciprocal(out=rs[:], in_=rs[:]), r=[rs], w=[rs])
        ts(k, "dve", rs[:], rs[:], 2.0 / N, None, ALU.mult, None, [rs], [rs])
        k.dma(S['hrs'][:, :], rs[:], r=[rs])
    if g.cut == 'h_filt':
        return
    with g.c.phase() as ph:
        rs = ph.sb([128, 1024], F32, "hrs2")
        k.dma(rs[:], S['hrs'][:, :], w=[rs])
        kbo = ph.sb([128, NT, 512], BF16, "hkbo")
        tks = [ph.sb([128, NT, 128], BF16, "htk%d" % j) for j in range(2)]
        pf = [ph.ps([128, 512], F32, "hpf%d" % j) for j in range(2)]
        ko = [ph.sb([128, 512], F32, "hko%d" % j) for j in range(2)]
        nn_ = 0
        for o in range(2):
            k.dma(kbo[:], S['hk'][0:N, o * 512:(o + 1) * 512].rearrange("(n p) c -> p n c", p=128), w=[kbo])
            for ft in range(LT):
                for ri in range(2):
                    tk = tks[nn_ % 2]; p_ = pf[nn_ % 2]; ko_ = ko[nn_ % 2]; nn_ += 1
                    k.dma(tk[:], I['hy_K%s%s' % ("re" if ri == 0 else "im", tg)][ft], w=[tk])
                    for nt in range(NT):
                        k.op("pe", lambda e, p_=p_, tk=tk, nt=nt: e.matmul(p_[:, :], lhsT=tk[:, nt, :], rhs=kbo[:, nt, :], start=(nt == 0), stop=(nt == NT - 1)), r=[tk, kbo], w=[p_])
                    tt(k, "dve", ko_[:], p_[:, :], rs[:, o * 512:(o + 1) * 512], ALU.mult, [p_, rs], [ko_])
                    if ft == 0:
                        ts(k, "dve", ko_[0:1, :], ko_[0:1, :], 0.5, None, ALU.mult, None, [ko_], [ko_])
                    k.dma(S['hKf'][o, ri, ft * 128:(ft + 1) * 128, :], ko_[:], r=[ko_])
    if g.cut == 'h_kf':
        return
    with g.c.phase() as ph:
        cw = ph.sb([128, 12, 4], F32, "hcw")
        k.dma(cw[:], I['hy_cw'][i], w=[cw])
        pt_ = [ph.sb([128, Lh + 2], F32, "hp%d" % j) for j in range(2)]
        xo = [ph.sb([128, Lh], F32, "hxo%d" % j) for j in range(2)]
        for c_ in range(12):
            p_ = pt_[c_ % 2]; x_ = xo[c_ % 2]
            k.op("pool", lambda e, p_=p_: e.memset(p_[:, 0:1], 0.0), w=[p_])
            k.op("pool", lambda e, p_=p_: e.memset(p_[:, Lh + 1:Lh + 2], 0.0), w=[p_])
            k.dma(p_[:, 1:Lh + 1], S['pfm'][hx0 + c_ * 128:hx0 + (c_ + 1) * 128, tok0:tok0 + Lh], w=[p_])
            k.op("act", lambda e, p_=p_, x_=x_, c_=c_: e.activation(out=x_[:], in_=p_[:, 1:Lh + 1], func=AF.Identity, bias=cw[:, c_, 3:4], scale=cw[:, c_, 1:2]), r=[p_, cw], w=[x_])
            k.op("dve", lambda e, p_=p_, x_=x_, c_=c_: e.scalar_tensor_tensor(out=x_[:], in0=p_[:, 0:Lh], scalar=cw[:, c_, 0:1], in1=x_[:], op0=ALU.mult, op1=ALU.add), r=[p_, cw, x_], w=[x_])
            k.op("dve", lambda e, p_=p_, x_=x_, c_=c_: e.scalar_tensor_tensor(out=x_[:], in0=p_[:, 2:Lh + 2], scalar=cw[:, c_, 2:3], in1=x_[:], op0=ALU.mult, op1=ALU.add), r=[p_, cw, x_], w=[x_])
            k.dma(S['hxs'][c_ * 128:(c_ + 1) * 128, 0:Lh], x_[:], r=[x_])
    if g.cut == 'h_sc':
        return
    for o in range(2):
        zsrc = S['hxs'][0:512, 0:Lh] if o == 0 else S['hz1'][:, 0:Lh]
        with g.c.phase() as ph:
            ident = ph.sb([128, 128], BF16, "hident")
            k.dma(ident[:], I['ident_bf'][:, :], w=[ident])
            zTt = ph.sb([128, LT, 512], BF16, "hzTt")
            zf = ph.sb([128, Lh], F32, "hzf"); zb = ph.sb([128, Lh], BF16, "hzb")
            pT = [ph.ps([128, 1024], BF16, "hpT%d" % j) for j in range(2)]
            nq = 0
            for c4 in range(4):
                k.dma(zf[:], zsrc[c4 * 128:(c4 + 1) * 128, :], w=[zf])
                cp(k, "pool", zb[:], zf[:], [zf], [zb])
                for t0 in range(0, LT, 8):
                    nj = min(8, LT - t0)
                    p_ = pT[nq % 2]; nq += 1
                    for jj in range(nj):
                        k.op("pe", lambda e, p_=p_, jj=jj, t0=t0: e.transpose(out=p_[:, jj * 128:(jj + 1) * 128], in_=zb[:, (t0 + jj) * 128:(t0 + jj + 1) * 128], identity=ident[:]), r=[zb, ident], w=[p_])
                    cp(k, k.ev(), zTt[:, t0:t0 + nj, c4 * 128:(c4 + 1) * 128], p_[:, 0:nj * 128].rearrange("p (a b) -> p a b", b=128), [p_], [zTt])
            tks = [ph.sb([128, LT, 128], BF16, "htkf%d" % j) for j in range(4)]
            pz = [ph.ps([128, 512], F32, "hpz%d" % j) for j in range(4)]
            kf = [ph.sb([128, 512], F32, "hkf%d" % j) for j in range(4)]
            wa = ph.sb([128, 512], F32, "hwa"); wb_ = ph.sb([128, 512], F32, "hwb")
            yo = [ph.sb([128, 2, 512], BF16, "hyo%d" % j) for j in range(2)]
            r0t = ph.sb([1, 2, 512], F32, "hr0t")
            for ft in range(LT):
                s_ = (ft % 2) * 2
                for ri in range(2):
                    tk = tks[s_ + ri]
                    k.dma(tk[:], I['hy_K%s%s' % ("re" if ri == 0 else "im", tg)][ft][:, 0:LT, :], w=[tk])
                    k.dma(kf[s_ + ri][:], S['hKf'][o, ri, ft * 128:(ft + 1) * 128, :], w=[kf[s_ + ri]])
                    for tt_ in range(LT):
                        k.op("pe", lambda e, ri=ri, tk=tk, tt_=tt_, s_=s_: e.matmul(pz[s_ + ri][:, :], lhsT=tk[:, tt_, :], rhs=zTt[:, tt_, :], start=(tt_ == 0), stop=(tt_ == LT - 1)), r=[tk, zTt], w=[pz[s_ + ri]])
                zr_, zi_, kr_, ki_ = pz[s_], pz[s_ + 1], kf[s_], kf[s_ + 1]
                y_ = yo[ft % 2]
                tt(k, "dve", wa[:], zr_[:, :], kr_[:], ALU.mult, [zr_, kr_], [wa])
                tt(k, "dve", wb_[:], zi_[:, :], ki_[:], ALU.mult, [zi_, ki_], [wb_])
                tt(k, "pool", y_[:, 0, :], wa[:], wb_[:], ALU.subtract, [wa, wb_], [y_])
                if ft == 0:
                    cp(k, "pool", r0t[0:1, 0, :], wa[0:1, :], [wa], [r0t])
                    cp(k, "pool", r0t[0:1, 1, :], wb_[0:1, :], [wb_], [r0t])
                tt(k, "dve", wa[:], zr_[:, :], ki_[:], ALU.mult, [zr_, ki_], [wa])
                tt(k, "dve", wb_[:], zi_[:, :], kr_[:], ALU.mult, [zi_, kr_], [wb_])
                tt(k, "pool", y_[:, 1, :], wa[:], wb_[:], ALU.add, [wa, wb_], [y_])
                if ft == 0:
                    cp(k, "pool", y_[0:1, :, :], r0t[0:1, :, :], [r0t, y_], [y_])
                k.dma(S['hY'][ft].rearrange("r p c -> p r c"), y_[:], r=[y_])
        if g.cut == 'h_fwd':
            return
        with g.c.phase() as ph:
            identf = ph.sb([128, 128], F32, "hidf")
            k.dma(identf[:], I['ident_f'][:, :], w=[identf])
            Y = ph.sb([128, LT, 2, 512], BF16, "hYs")
            for ft in range(LT):
                k.dma(Y[:, ft], S['hY'][ft].rearrange("r p c -> p r c"), w=[Y])
            cv = [ph.sb([128, Lh], F32, "hcv%d" % j) for j in range(4)]
            tis = [ph.sb([128, LT, 128], BF16, "hti%d" % j) for j in range(4)]
            py = [ph.ps([128, 512], F32, "hpy%d" % j) for j in range(2)]
            ptr = [ph.ps([128, 512], F32, "hptr%d" % j) for j in range(2)]
            yt = [ph.sb([128, 512], F32, "hyt%d" % j) for j in range(2)]
            qs4 = py + ptr
            for tt_ in range(LT):
                s_ = (tt_ % 2) * 2
                for ri in range(2):
                    ti = tis[s_ + ri]
                    k.dma(ti[:], I['hy_I%s%s' % ("re" if ri == 0 else "im", tg)][tt_], w=[ti])
                for c4 in range(4):
                    for ri in range(2):
                        ti = tis[s_ + ri]
                        for ft in range(LT):
                            q_ = qs4[c4]
                            k.op("pe", lambda e, q_=q_, ti=ti, ft=ft, ri=ri, c4=c4: e.matmul(q_[:, 0:128], lhsT=Y[:, ft, ri, c4 * 128:(c4 + 1) * 128], rhs=ti[:, ft, :],
                                                                                     start=(ri == 0 and ft == 0), stop=(ri == 1 and ft == LT - 1)), r=[ti, Y], w=[q_])
                    cp(k, k.ev(), cv[c4][:, tt_ * 128:(tt_ + 1) * 128], qs4[c4][:, 0:128], [qs4[c4]], [cv[c4]])
            if g.cut in ('h_inv1', 'h_inv2'):
                return
            hb_ = ph.sb([128, 4, 2], F32, "hbias")
            k.dma(hb_[:], I['hy_bias4'][i], w=[hb_])
            zf = ph.sb([128, Lh], F32, "hzf2"); gf = ph.sb([128, Lh], F32, "hgf"); ob = ph.sb([128, Lh], BF16, "hob")
            for c4 in range(4):
                k.dma(zf[:], zsrc[c4 * 128:(c4 + 1) * 128, :], w=[zf])
                k.dma(gf[:], S['hxs'][(o + 1) * 512 + c4 * 128:(o + 1) * 512 + (c4 + 1) * 128, 0:Lh], w=[gf])
                k.op("dve", lambda e, c4=c4: e.scalar_tensor_tensor(out=zf[:], in0=zf[:], scalar=hb_[:, c4, o:o + 1], in1=cv[c4][:], op0=ALU.mult, op1=ALU.add), r=[zf, hb_, cv[c4]], w=[zf])
                tt(k, "pool", zf[:], zf[:], gf[:], ALU.mult, [zf, gf], [zf])
                if o == 0:
                    k.dma(S['hz1'][c4 * 128:(c4 + 1) * 128, 0:Lh], zf[:], r=[zf])
                else:
                    g0 = FM['hy_g'][0]
                    k.dma(gf[:], S['pfm'][g0 + c4 * 128:g0 + (c4 + 1) * 128, tok0:tok0 + Lh], w=[gf])
                    act(k, gf[:], gf[:], AF.Silu, [gf], [gf])
                    tt(k, "dve", ob[:], zf[:], gf[:], ALU.mult, [zf, gf], [ob])
                    k.dma(S['y'][512 + c4 * 128:512 + (c4 + 1) * 128, tok0:tok0 + Lh], ob[:], r=[ob])


def phase_outproj(g, i, last):
    k, I, S, L, CTX, T = g.k, g.I, g.S, g.L, g.CTX, g.T
    with g.c.phase() as ph:
        wo = ph.sb([128, NK, D], BF16, "wo")
        k.dma(wo[:], S['wob'].rearrange("(k p) c -> p k c", p=128), w=[wo])
        gts = [load_bc(g, ph, "gt%d" % r, S['mod'][r:r + 1, 2 * D:3 * D], D) for r in range(2)]
        fnw = load_bc(g, ph, "fnw", I['final_norm_w'][0:1, :], D) if last else None
        yT = [ph.sb([128, NK, 128], BF16, "yT%d" % j) for j in range(2)]
        xr = [ph.sb([128, D], F32, "xr%d" % j) for j in range(2)]
        xn = [ph.sb([128, D], F32, "xn%d" % j) for j in range(2)]
        tmp = ph.sb([128, 512], F32, "otmp")
        junk = ph.sb([128, D], BF16, "ojunk")
        ss = ph.sb([128, 1], F32, "oss")
        epsb = ph.sb([128, 1], F32, "oepsb")
        k.op("pool", lambda e: e.memset(epsb[:], EPS), w=[epsb])
        po = [ph.ps([128, 512], F32, "opo%d" % j) for j in range(4)]
        yv = S['y'].rearrange("(k p) t -> p k t", p=128)
        nb = 0
        npo = 0
        for tok in range(0 if not last else CTX, T, 128):
            isctx = tok < CTX
            y_ = yT[nb % 2]; x_ = xr[nb % 2]; o_ = xn[nb % 2]; nb += 1
            k.dma(y_[:], yv[:, :, tok:tok + 128], w=[y_])
            if i == 0:
                src = I['ctx'][tok:tok + 128, :] if isctx else I['x'][tok - CTX:tok - CTX + 128, :]
            else:
                src = S['xc1'][tok:tok + 128, :] if isctx else S['x1'][tok - CTX:tok - CTX + 128, :]
            k.dma(x_[:], src, w=[x_])
            gt = gts[1 if isctx else 0]
            for n in range(4):
                p_ = po[npo % 4]; npo += 1
                for kk in range(NK):
                    k.op("pe", lambda e, p_=p_, kk=kk, n=n, y_=y_: e.matmul(p_[:, :], lhsT=y_[:, kk, :], rhs=wo[:, kk, n * 512:(n + 1) * 512], start=(kk == 0), stop=(kk == NK - 1)), r=[y_, wo], w=[p_])
                cs = slice(n * 512, (n + 1) * 512)
                tt(k, "dve", tmp[:], p_[:, :], gt[:, cs], ALU.mult, [p_, gt], [tmp])
                tt(k, "pool", o_[:, cs], tmp[:], x_[:, cs], ALU.add, [tmp, x_], [o_])
            if last:
                k.op("act", lambda e, o_=o_: e.activation(out=junk[:], in_=o_[:], func=AF.Square, accum_out=ss[:]), r=[o_], w=[junk, ss])
                rsqrt_inplace(k, ss[:], ss, epsb[:, 0:1], epsb, scale=1.0 / D)
                k.op("dve", lambda e, o_=o_: e.scalar_tensor_tensor(out=o_[:], in0=o_[:], scalar=ss[:, 0:1], in1=fnw[:], op0=ALU.mult, op1=ALU.mult), r=[o_, ss, fnw], w=[o_])
                k.dma(g.out[tok - CTX:tok - CTX + 128, :], o_[:], r=[o_])
            elif isctx:
                k.dma(S['xc1'][tok:tok + 128, :], o_[:], r=[o_])
            else:
                k.dma(S['x1'][tok - CTX:tok - CTX + 128, :], o_[:], r=[o_])

def s5_prep(inp, T):
    m = {}
    DEPTH = inp['s5_a_re'].shape[0]
    def st(a):
        return np.ascontiguousarray(a.reshape(DEPTH, 2, 16, 2, 64).transpose(0, 3, 4, 1, 2).reshape(DEPTH, 128, 32))
    m['s5_are'] = st(inp['s5_a_re']); m['s5_aim'] = st(inp['s5_a_im'])
    ldt = np.broadcast_to(inp['s5_log_dt'][..., None], (DEPTH, 2, 32, 64))
    m['s5_ldt'] = st(np.ascontiguousarray(ldt))
    m['s5_d32'] = np.ascontiguousarray(inp['s5_d'].reshape(DEPTH, 16, 32).transpose(0, 2, 1))
    def Bl(B):
        o = np.zeros((DEPTH, 2, 16, 32, 128), np.float32)
        Br = B.reshape(DEPTH, 2, 16, 2, 64, 16)
        for g2 in range(2):
            o[:, :, :, g2 * 16:(g2 + 1) * 16, g2 * 64:(g2 + 1) * 64] = Br[:, :, :, g2].transpose(0, 1, 2, 4, 3)
        return o
    m['s5_Bre'] = Bl(inp['s5_b_re']); m['s5_Bim'] = Bl(inp['s5_b_im'])
    def Cl(C):
        o = np.zeros((DEPTH, 2, 16, 128, 32), np.float32)
        Cr = C.reshape(DEPTH, 2, 16, 2, 16, 64)
        for g2 in range(2):
            o[:, :, :, g2 * 64:(g2 + 1) * 64, g2 * 16:(g2 + 1) * 16] = Cr[:, :, :, g2].transpose(0, 1, 2, 4, 3)
        return o
    m['s5_Cre'] = Cl(inp['s5_c_re']); m['s5_Cim'] = Cl(inp['s5_c_im'])
    m['s5_glu_w'] = inp['s5_glu_w']
    m['s5_glu_b4'] = np.ascontiguousarray(inp['s5_glu_b'].reshape(DEPTH, 4, 128).transpose(0, 2, 1))
    m['tvals'] = np.ascontiguousarray(np.broadcast_to(np.arange(T, dtype=np.float32)[None], (128, T)))
    return m


def attn_prep(inp, L, CTX, GW):
    T = L + CTX
    m = {}
    DEPTH = inp['ret_norm_w'].shape[0]
    for C in (128, 64):
        a = np.ones((128, T + 1), np.float32); a[:, ::C] = 0.0
        m['m01_%d' % C] = a
    s = np.arange(128)
    m['maskF'] = (s[:, None] <= s[None, :]).astype(np.float32)
    m['maskB'] = (s[:, None] >= s[None, :]).astype(np.float32)
    BASE = 10000.0
    inv_c = BASE ** (-np.arange(64, dtype=np.float32) / 64)
    ang_c = np.arange(CTX, dtype=np.float32)[:, None] * inv_c
    inv = BASE ** (-np.arange(32, dtype=np.float32) / 32)
    j = np.arange(L)
    ang_l = np.concatenate([(j // GW).astype(np.float32)[:, None] * inv, (j % GW).astype(np.float32)[:, None] * inv], -1)
    ang = np.concatenate([ang_c, ang_l], 0).astype(np.float32)
    cos, sin = np.cos(ang).T, np.sin(ang).T
    m['ropeC'] = np.ascontiguousarray(np.concatenate([cos, cos], 0)).astype(np.float32)
    m['ropeS'] = np.ascontiguousarray(np.concatenate([-sin, sin], 0)).astype(np.float32)
    m['ret_nw'] = np.ascontiguousarray(inp['ret_norm_w'].reshape(DEPTH, 4, 128).transpose(0, 2, 1))
    m['gla_nw'] = np.ascontiguousarray(inp['gla_norm_w'].reshape(DEPTH, 4, 128).transpose(0, 2, 1))
    gw = np.zeros((DEPTH, 2, 32, 2, 128), np.float32)
    for tl in range(2):
        for dr in range(2):
            gw[:, tl, 16 * dr:16 * dr + 16, dr, :] = inp['gla_gate_w'][:, dr, :, tl * 128:(tl + 1) * 128]
    m['gla_gw'] = gw
    m['gla_gb'] = np.ascontiguousarray(inp['gla_gate_b'].reshape(DEPTH, 2, 2, 128).transpose(0, 2, 3, 1))
    return m


def hy_tables(Lh):
    N = 2 * Lh
    t = np.linspace(0.0, 1.0, Lh, dtype=np.float32)[:, None]
    wpos = 2.0 * math.pi * np.arange(Lh, dtype=np.float32)[:, None] / Lh
    f = np.linspace(1e-4, 15, 16, dtype=np.float32)[None, :]
    z = np.concatenate([t, np.cos(f * wpos), -np.sin(f * wpos)], -1).astype(np.float32)
    zfull = np.zeros((N, 33), np.float32)
    zfull[:Lh] = z
    zfull[Lh + 1:] = z[1:][::-1]
    deltas = np.linspace(math.log(1e-2) / 1.5, math.log(1e-2) / 0.3, 512, dtype=np.float32)
    dec = np.exp(-t * np.abs(deltas)).astype(np.float32)
    dfull = np.zeros((N, 512), np.float32)
    dfull[:Lh] = dec
    dfull[Lh + 1:] = dec[1:][::-1]
    n = np.arange(N, dtype=np.float64)[:, None]
    fr = np.arange(Lh, dtype=np.float64)[None, :]
    ang = 2.0 * math.pi * ((n * fr) % N) / N
    Kre = np.cos(ang); Kim = -np.sin(ang)
    Kim[:, 0] = np.where(np.arange(N) % 2 == 0, 1.0, -1.0)
    def tile(A, KT, MT):
        return np.ascontiguousarray(A.reshape(KT, 128, MT, 128).transpose(2, 1, 0, 3)).astype(ml_dtypes.bfloat16)
    m = {}
    m['z'] = np.ascontiguousarray(zfull.T)
    m['dec'] = dfull
    m['Kre'] = tile(Kre, N // 128, Lh // 128); m['Kim'] = tile(Kim, N // 128, Lh // 128)
    ff = np.arange(Lh, dtype=np.float64)[:, None]; tt = np.arange(Lh, dtype=np.float64)[None, :]
    ang2 = 2.0 * math.pi * ((ff * tt) % N) / N
    Ire = np.cos(ang2); Iim = -np.sin(ang2)
    Iim[0, :] = np.where(np.arange(Lh) % 2 == 0, 1.0, -1.0)
    m['Ire'] = tile(Ire, Lh // 128, Lh // 128); m['Iim'] = tile(Iim, Lh // 128, Lh // 128)
    return m


def hy_prep(inp, L, CTX):
    m = {}
    DEPTH = inp['hy_w1'].shape[0]
    for tg, Lh in (('C', CTX), ('L', L)):
        tb = hy_tables(Lh)
        for kk, v in tb.items():
            m['hy_' + kk + tg] = v
    for nm in ('hy_w1', 'hy_w2', 'hy_w3', 'hy_w4'):
        m[nm] = inp[nm]
    m['hy_bf'] = np.ascontiguousarray(np.stack([inp['hy_b1'], inp['hy_f1'], inp['hy_b2'], inp['hy_f2'], inp['hy_b3'], inp['hy_f3']], -1))
    cw = np.concatenate([inp['hy_conv_w'], inp['hy_conv_b'][:, None, :]], 1)
    m['hy_cw'] = np.ascontiguousarray(cw.reshape(DEPTH, 4, 12, 128).transpose(0, 3, 2, 1))
    m['hy_bias4'] = np.ascontiguousarray(inp['hy_bias'].reshape(DEPTH, 2, 4, 128).transpose(0, 3, 2, 1))
    return m


_NC_CACHE = {}


def kernel(**inputs):
    inp = {k_: np.asarray(v) for k_, v in inputs.items()}
    L, CTX, GW = 4096, 256, 64
    T = L + CTX
    shared = {}
    for nm in ('norm_w', 'ada_w', 'ada_b', 'w_in', 'w_out'):
        shared[nm] = np.ascontiguousarray(inp[nm], dtype=np.float32)
    shared['final_norm_w'] = np.ascontiguousarray(inp['final_norm_w'][None], dtype=np.float32)
    shared['ident_bf'] = np.eye(128, dtype=np.float32).astype(ml_dtypes.bfloat16)
    shared['ident_f'] = np.eye(128, dtype=np.float32)
    shared.update(s5_prep(inp, T))
    shared.update(attn_prep(inp, L, CTX, GW))
    shared.update(hy_prep(inp, L, CTX))
    ncores = 4
    in_maps = []
    for core in range(ncores):
        b = core % 4
        m = dict(shared)
        m['x'] = np.ascontiguousarray(inp['x'][b], dtype=np.float32)
        m['ctx'] = np.ascontiguousarray(inp['ctx'][b], dtype=np.float32)
        cc = np.stack([inp['c'][b].reshape(16, 128).T, inp['c_ctx'].reshape(16, 128).T], axis=-1)
        m['cc'] = np.ascontiguousarray(cc, dtype=np.float32)
        in_maps.append(m)
    if 'nc' not in _NC_CACHE:
        _NC_CACHE['nc'] = build(L=L, CTX=CTX)
    nc = _NC_CACHE['nc']
    res = run_bass_kernel_spmd(nc, in_maps, core_ids=list(range(ncores)))
    out = np.stack([np.asarray(res.results[b]['out'], dtype=np.float32) for b in range(4)], axis=0)
    return out
```

```python
import math
import numpy as np
import ml_dtypes
import concourse.bass as bass
import concourse.mybir as mybir
from concourse.bass_utils import run_bass_kernel_spmd
from contextlib import ExitStack

F32 = mybir.dt.float32
BF16 = mybir.dt.bfloat16
ALU = mybir.AluOpType
AF = mybir.ActivationFunctionType

D = 2048
BR = 512
IN_W = 6688
DEPTH = 2
EPS = 1e-6
NK = D // 128
SAME_ENGINE_SYNC = True

SEG = {}
_off = 0
for _n, _s in (('s5_u', 512), ('s5_g', 512), ('hy_x', 1536), ('hy_g', 512), ('ret_q', 512), ('ret_k', 512),
               ('ret_v', 512), ('ret_g', 512), ('gla_q', 256), ('gla_k', 256), ('gla_v', 512), ('gla_lr', 32),
               ('gla_g', 512)):
    SEG[_n] = (_off, _s)
    _off += _s


class Buf:
    __slots__ = ("t", "w", "r", "name")

    def __init__(self, t, name=""):
        self.t = t
        self.w = None
        self.r = []
        self.name = name

    def __getitem__(self, idx):
        return self.t[idx]


class K:
    def __init__(self, nc, es, n_dma_sems=32):
        self.nc = nc
        self.eng = {"pe": nc.tensor, "act": nc.scalar, "dve": nc.vector, "pool": nc.gpsimd, "sp": nc.sync}
        self.sem, self.cnt, self.seen = {}, {}, {}
        for e in self.eng:
            self.sem[e] = es.enter_context(nc.semaphore("s_" + e))
            self.cnt[e] = 0
            self.seen[e] = {}
        self.dsem = [es.enter_context(nc.semaphore("d%d" % i)) for i in range(2 * n_dma_sems)]
        self.dval = [0] * (2 * n_dma_sems)
        self.dnext = {"sp": 0, "act": 0}
        self.npool = n_dma_sems
        self.pending_dma = []
        self.rr = 0

    def _wait(self, e, tok):
        if tok is None:
            return
        key, sh, val = tok
        if key == ("e", e) and (not SAME_ENGINE_SYNC or e in ("pe", "sp")):
            return
        if self.seen[e].get(key, 0) >= val:
            return
        self.eng[e].wait_ge(sh, val)
        self.seen[e][key] = val

    def _deps(self, e, r, w):
        for b in r:
            self._wait(e, b.w)
        for b in w:
            self._wait(e, b.w)
            for t in b.r:
                self._wait(e, t)

    def _mark(self, tok, r, w):
        for b in r:
            b.r.append(tok)
            if len(b.r) > 64:
                b.r = b.r[-48:]
        for b in w:
            b.w = tok
            b.r = []

    def op(self, e, fn, r=(), w=()):
        self._deps(e, r, w)
        inst = fn(self.eng[e])
        self.cnt[e] += 1
        inst.then_inc(self.sem[e], 1)
        tok = (("e", e), self.sem[e], self.cnt[e])
        self._mark(tok, r, w)
        return tok

    def dma(self, out, in_, r=(), w=(), q="sp"):
        if len(r) > 0 and len(w) == 0:
            q = "act"
        i = self.dnext[q] + (self.npool if q == "act" else 0)
        self.dnext[q] = (self.dnext[q] + 1) % self.npool
        key = ("d", i)
        if self.dval[i] > 0:
            self._wait(q, (key, self.dsem[i], self.dval[i]))
        self._deps(q, r, w)
        self.dval[i] += 16
        inst = self.eng[q].dma_start(out=out, in_=in_)
        inst.then_inc(self.dsem[i], 16)
        tok = (key, self.dsem[i], self.dval[i])
        self._mark(tok, r, w)
        self.pending_dma.append(tok)
        return tok

    def barrier(self):
        toks = [(("e", e), self.sem[e], self.cnt[e]) for e in self.eng if self.cnt[e] > 0]
        last = {}
        for t in self.pending_dma:
            last[t[0]] = t
        toks += list(last.values())
        for e in self.eng:
            for t in toks:
                if t[0] == ("e", e):
                    continue
                self._wait(e, t)
        self.pending_dma = []

    def ev(self):
        self.rr += 1
        return ("dve", "act")[self.rr % 2]


class Ctx:
    def __init__(self, nc, k):
        self.nc, self.k = nc, k
        self.n = 0

    def phase(self):
        return Phase(self)


class Phase:
    def __init__(self, c):
        self.c = c
        self.es = ExitStack()

    def __enter__(self):
        self.es.__enter__()
        return self

    def sb(self, shape, dt=F32, name=None):
        self.c.n += 1
        return Buf(self.es.enter_context(self.c.nc.sbuf_tensor("sb%d_%s" % (self.c.n, name or "t"), list(shape), dt)), name)

    def ps(self, shape, dt=F32, name=None):
        self.c.n += 1
        return Buf(self.es.enter_context(self.c.nc.psum_tensor("ps%d_%s" % (self.c.n, name or "p"), list(shape), dt)), name)

    def __exit__(self, *a):
        self.c.k.barrier()
        return self.es.__exit__(*a)


FM = {}
_o = 0
for _n, _s in (('s5_u', 512), ('s5_g', 512), ('hy_x', 1536), ('hy_g', 512), ('ret_q', 512), ('ret_qs', 512),
               ('ret_k', 512), ('ret_ks', 512), ('ret_g', 512), ('gla_q', 256), ('gla_k', 256), ('gla_lr', 32),
               ('gla_g', 512)):
    FM[_n] = (_o, _s)
    _o += _s
NFM = _o


def fm_tiles():
    out = []
    for name, (r0, sz) in FM.items():
        swap = name in ('ret_qs', 'ret_ks')
        src0 = SEG[name[:-1] if swap else name][0]
        for t in range(0, sz, 128):
            n = min(128, sz - t)
            if swap:
                pieces = [(src0 + t + 64, 64), (src0 + t, 64)]
            else:
                pieces = [(src0 + t, n)]
            out.append((r0 + t, n, pieces))
    return out


class G:
    pass


def build(L=4096, CTX=256, nlayers=DEPTH, debug=False, stop_after=None, only=None, skip=(), ext_p=False, cut=None):
    T = CTX + L
    NTB = T // 128
    nc = bass.Bass("TRN2", target_bir_lowering=False)
    g = G()
    g.nc, g.L, g.CTX, g.T = nc, L, CTX, T

    in_names = []

    def din(name, shape, dt=F32):
        in_names.append(name)
        return nc.dram_tensor(name, list(shape), dt, kind="ExternalInput").ap()

    def dscr(name, shape, dt=F32):
        return nc.dram_tensor(name, list(shape), dt, kind=("ExternalOutput" if debug else "Internal")).ap()

    I = {}
    I['x'] = din('x', [L, D])
    I['ctx'] = din('ctx', [CTX, D])
    I['cc'] = din('cc', [128, NK, 2])
    for nm, shp in (('norm_w', [DEPTH, D]), ('ada_w', [DEPTH, D, 3 * D]), ('ada_b', [DEPTH, 3 * D]),
                    ('w_in', [DEPTH, D, IN_W]), ('w_out', [DEPTH, D, D]), ('final_norm_w', [1, D])):
        if ext_p and nm in ('ada_w', 'w_in', 'w_out'):
            continue
        I[nm] = din(nm, shp)
    g.in_names = None
    I['ident_bf'] = din('ident_bf', [128, 128], BF16)
    I['ident_f'] = din('ident_f', [128, 128], F32)
    out = nc.dram_tensor("out", [L, D], F32, kind="ExternalOutput").ap()
    S = {}
    S['mod'] = dscr('s_mod', [2, 3 * D])
    S['wb'] = dscr('s_wb', [D, IN_W], BF16)
    S['wob'] = dscr('s_wob', [D, D], BF16)
    S['pfm'] = din('s_pfm', [NFM, T]) if ext_p else dscr('s_pfm', [NFM, T])
    S['ptm'] = din('s_ptm', [T, 1024]) if ext_p else dscr('s_ptm', [T, 1024])
    g.cut = cut
    S['y'] = dscr('s_y', [D, T], BF16)
    S['x1'] = dscr('s_x1', [L, D])
    S['xc1'] = dscr('s_xc1', [CTX, D])
    S['ys5'] = dscr('s_ys5', [512, T])
    I['tvals'] = din('tvals', [128, T])
    I['s5_are'] = din('s5_are', [DEPTH, 128, 32]); I['s5_aim'] = din('s5_aim', [DEPTH, 128, 32]); I['s5_ldt'] = din('s5_ldt', [DEPTH, 128, 32])
    I['s5_d32'] = din('s5_d32', [DEPTH, 32, 16])
    I['s5_Bre'] = din('s5_Bre', [DEPTH, 2, 16, 32, 128]); I['s5_Bim'] = din('s5_Bim', [DEPTH, 2, 16, 32, 128])
    I['s5_Cre'] = din('s5_Cre', [DEPTH, 2, 16, 128, 32]); I['s5_Cim'] = din('s5_Cim', [DEPTH, 2, 16, 128, 32])
    I['m01_128'] = din('m01_128', [128, T + 1]); I['m01_64'] = din('m01_64', [128, T + 1])
    I['maskF'] = din('maskF', [128, 128]); I['maskB'] = din('maskB', [128, 128])
    I['ropeC'] = din('ropeC', [128, T]); I['ropeS'] = din('ropeS', [128, T])
    I['ret_nw'] = din('ret_nw', [DEPTH, 128, 4]); I['gla_nw'] = din('gla_nw', [DEPTH, 128, 4])
    I['gla_gw'] = din('gla_gw', [DEPTH, 2, 32, 2, 128]); I['gla_gb'] = din('gla_gb', [DEPTH, 2, 128, 2])
    for tg_, Lh_ in (('C', CTX), ('L', L)):
        N_ = 2 * Lh_
        I['hy_z' + tg_] = din('hy_z' + tg_, [33, N_]); I['hy_dec' + tg_] = din('hy_dec' + tg_, [N_, 512])
        for ri_ in ('re', 'im'):
            I['hy_K' + ri_ + tg_] = din('hy_K' + ri_ + tg_, [Lh_ // 128, 128, N_ // 128, 128], BF16)
            I['hy_I' + ri_ + tg_] = din('hy_I' + ri_ + tg_, [Lh_ // 128, 128, Lh_ // 128, 128], BF16)
    I['hy_w1'] = din('hy_w1', [DEPTH, 33, 64]); I['hy_w2'] = din('hy_w2', [DEPTH, 64, 64]); I['hy_w3'] = din('hy_w3', [DEPTH, 64, 64]); I['hy_w4'] = din('hy_w4', [DEPTH, 64, 2048])
    I['hy_bf'] = din('hy_bf', [DEPTH, 64, 6]); I['hy_cw'] = din('hy_cw', [DEPTH, 128, 12, 4]); I['hy_bias4'] = din('hy_bias4', [DEPTH, 128, 4, 2])
    S['hk'] = dscr('s_hk', [2 * L, 1024], BF16); S['hrs'] = dscr('s_hrs', [128, 1024]); S['hKf'] = dscr('s_hKf', [2, 2, L, 512])
    S['hxs'] = dscr('s_hxs', [1536, L]); S['hz1'] = dscr('s_hz1', [512, L]); S['hY'] = dscr('s_hY', [L // 128, 2, 128, 512], BF16)
    I['s5_glu_w'] = din('s5_glu_w', [DEPTH, 512, 512]); I['s5_glu_b4'] = din('s5_glu_b4', [DEPTH, 128, 4])
    g.I, g.S, g.out = I, S, out

    with ExitStack() as es:
        k = K(nc, es)
        c = Ctx(nc, k)
        g.k, g.c = k, c
        for layer in range(nlayers):
            if not ext_p:
                phase_mod(g, layer)
                phase_wcast(g, layer)
                phase_inproj(g, layer)
            if stop_after == 'inproj':
                break
            if 's5' not in skip:
                phase_s5(g, layer)
            if stop_after == 's5':
                break
            if only in (None, 'ret') and 'attn' not in skip:
                phase_ret(g, layer)
            if only in (None, 'gla') and 'attn' not in skip:
                phase_gla(g, layer)
            if stop_after == 'attn':
                break
            if 'hy' not in skip:
                if layer < DEPTH - 1:
                    phase_hyena(g, layer, CTX, 0, 'C')
                phase_hyena(g, layer, L, CTX, 'L')
            if stop_after == 'hy':
                break
            phase_outproj(g, layer, layer == nlayers - 1)
        k.barrier()
    nc._in_names = in_names
    return nc


def phase_mod(g, i):
    k, I, S = g.k, g.I, g.S
    with g.c.phase() as ph:
        cc = ph.sb([128, NK, 2], F32, "cc")
        s2 = ph.sb([128, NK, 2], F32, "s2")
        k.dma(cc[:], I['cc'][:, :, :], w=[cc])
        k.op("act", lambda e: e.activation(out=s2[:], in_=cc[:], func=AF.Silu), r=[cc], w=[s2])
        ab = ph.sb([2, 3 * D], F32, "ab")
        k.dma(ab[0:1, :], I['ada_b'][i:i + 1, :], w=[ab])
        k.dma(ab[1:2, :], I['ada_b'][i:i + 1, :], w=[ab])
        mo = ph.sb([2, 3 * D], F32, "mo")
        HALF = 3 * D // 2
        wts = [ph.sb([128, HALF], F32, "aw%d" % j) for j in range(2)]
        pss = [ph.ps([2, 512], F32, "mps%d" % j) for j in range(HALF // 512)]
        aw = I['ada_w'][i].rearrange("(k p) c -> k p c", p=128)
        n = 0
        for half in range(2):
            for kk in range(NK):
                wt = wts[n % 2]
                n += 1
                k.dma(wt[:], aw[kk, :, half * HALF:(half + 1) * HALF], w=[wt], q=("sp", "sp")[n % 2])
                for j, ps in enumerate(pss):
                    k.op("pe", lambda e, ps=ps, wt=wt, j=j, kk=kk: e.matmul(
                        ps[:], lhsT=s2[:, kk, :], rhs=wt[:, j * 512:(j + 1) * 512], start=(kk == 0), stop=(kk == NK - 1)),
                        r=[s2, wt], w=[ps])
            for j, ps in enumerate(pss):
                c0 = half * HALF + j * 512
                k.op("dve", lambda e, ps=ps, c0=c0: e.tensor_tensor(out=mo[:, c0:c0 + 512], in0=ps[:], in1=ab[:, c0:c0 + 512], op=ALU.add),
                     r=[ps, ab], w=[mo])
        k.dma(S['mod'][:, :], mo[:], r=[mo])


def phase_wcast(g, i):
    k, I, S = g.k, g.I, g.S
    with g.c.phase() as ph:
        for (src, dst, ncol) in ((I['w_in'][i], S['wb'], IN_W), (I['w_out'][i], S['wob'], D)):
            fs = [ph.sb([128, ncol], F32, "wf%d" % j) for j in range(2)]
            bs = [ph.sb([128, ncol], BF16, "wbf%d" % j) for j in range(2)]
            sv = src.rearrange("(k p) c -> k p c", p=128)
            dv = dst.rearrange("(k p) c -> k p c", p=128)
            for kk in range(NK):
                f, b = fs[kk % 2], bs[kk % 2]
                k.dma(f[:], sv[kk], w=[f], q=("sp", "sp")[kk % 2])
                e_ = ("dve", "pool", "act")[kk % 3]
                if e_ == "act":
                    k.op("act", lambda e, f=f, b=b: e.copy(out=b[:], in_=f[:]), r=[f], w=[b])
                else:
                    k.op(e_, lambda e, f=f, b=b: e.tensor_copy(out=b[:], in_=f[:]), r=[f], w=[b])
                k.dma(dv[kk], b[:], r=[b])


def load_bc(g, ph, name, src_row_ap, n):
    t = ph.sb([128, n], F32, name)
    g.k.dma(t[:], src_row_ap.partition_broadcast(128), w=[t])
    return t


def phase_inproj(g, i):
    k, I, S, L, CTX, T = g.k, g.I, g.S, g.L, g.CTX, g.T
    with g.c.phase() as ph:
        ident = ph.sb([128, 128], BF16, "ident")
        k.dma(ident[:], I['ident_bf'][:, :], w=[ident])
        nw = load_bc(g, ph, "nw", I['norm_w'][i:i + 1, :], D)
        wmod, sh = [], []
        for r in range(2):
            sc = load_bc(g, ph, "sc%d" % r, S['mod'][r:r + 1, D:2 * D], D)
            k.op("dve", lambda e, sc=sc: e.scalar_tensor_tensor(out=sc[:], in0=sc[:], scalar=1.0, in1=nw[:], op0=ALU.add, op1=ALU.mult),
                 r=[sc, nw], w=[sc])
            wmod.append(sc)
            sh.append(load_bc(g, ph, "sh%d" % r, S['mod'][r:r + 1, 0:D], D))
        TG = min(1024, T)
        hT = ph.sb([128, NK, TG], BF16, "hT")
        xs = [ph.sb([128, D], F32, "xt%d" % j) for j in range(2)]
        junk = ph.sb([128, D], BF16, "junk")
        h1 = ph.sb([128, D], F32, "h1")
        hb = ph.sb([128, D], BF16, "hb")
        ss = ph.sb([128, 1], F32, "ss")
        epsb = ph.sb([128, 1], F32, "epsb")
        k.op("pool", lambda e: e.memset(epsb[:], EPS), w=[epsb])
        pT = [ph.ps([128, 1024], BF16, "pT%d" % j) for j in range(2)]
        pso = [ph.ps([128, 512], F32, "pso%d" % j) for j in range(4)]
        wts = [ph.sb([128, NK, 128], BF16, "wt%d" % j) for j in range(3)]
        wvs = ph.sb([128, NK, 512], BF16, "wv")
        ots = [ph.sb([128, 512], F32, "ot%d" % j) for j in range(4)]
        wbv = S['wb'].rearrange("(k p) c -> p k c", p=128)
        tiles = fm_tiles()
        nx = 0
        no = 0
        for t0 in range(0, T, TG):
            tg = min(TG, T - t0)
            for tb in range(tg // 128):
                tok = t0 + tb * 128
                isctx = tok < CTX
                src = I['ctx'][tok:tok + 128, :] if isctx else I['x'][tok - CTX:tok - CTX + 128, :]
                if i > 0:
                    src = S['xc1'][tok:tok + 128, :] if isctx else S['x1'][tok - CTX:tok - CTX + 128, :]
                x_ = xs[nx % 2]
                nx += 1
                k.dma(x_[:], src, w=[x_], q=("sp", "sp")[nx % 2])
                k.op("act", lambda e, x_=x_: e.activation(out=junk[:], in_=x_[:], func=AF.Square, accum_out=ss[:]), r=[x_], w=[junk, ss])
                rsqrt_inplace(k, ss[:], ss, epsb[:, 0:1], epsb, scale=1.0 / D)
                r_ = 1 if isctx else 0
                k.op("dve", lambda e, x_=x_, r_=r_: e.scalar_tensor_tensor(out=h1[:], in0=x_[:], scalar=ss[:, 0:1], in1=wmod[r_][:], op0=ALU.mult, op1=ALU.mult),
                     r=[x_, ss, wmod[r_]], w=[h1])
                k.op("pool", lambda e, r_=r_: e.tensor_tensor(out=hb[:], in0=h1[:], in1=sh[r_][:], op=ALU.add), r=[h1, sh[r_]], w=[hb])
                for half in range(2):
                    p_ = pT[half]
                    for j in range(8):
                        kk = half * 8 + j
                        k.op("pe", lambda e, p_=p_, j=j, kk=kk: e.transpose(out=p_[:, j * 128:(j + 1) * 128], in_=hb[:, kk * 128:(kk + 1) * 128], identity=ident[:]),
                             r=[hb, ident], w=[p_])
                    ee = k.ev()
                    dst = hT[:, half * 8:half * 8 + 8, tb * 128:(tb + 1) * 128]
                    srcp = p_[:].rearrange("p (a b) -> p a b", a=8)
                    if ee == "act":
                        k.op("act", lambda e, dst=dst, srcp=srcp: e.copy(out=dst, in_=srcp), r=[p_], w=[hT])
                    else:
                        k.op("dve", lambda e, dst=dst, srcp=srcp: e.tensor_copy(out=dst, in_=srcp), r=[p_], w=[hT])
            for ti, (r0, nr, pieces) in enumerate(tiles):
                wt = wts[ti % 3]
                c_ = 0
                for (s0, n_) in pieces:
                    k.dma(wt[:, :, c_:c_ + n_], wbv[:, :, s0:s0 + n_], w=[wt], q=("sp", "sp")[ti % 2])
                    c_ += n_
                for n0 in range(0, tg, 512):
                    nn = min(512, tg - n0)
                    ps = pso[no % 4]
                    ot = ots[no % 4]
                    no += 1
                    for kk in range(NK):
                        k.op("pe", lambda e, ps=ps, wt=wt, kk=kk, nr=nr, n0=n0, nn=nn: e.matmul(
                            ps[0:nr, 0:nn], lhsT=wt[:, kk, 0:nr], rhs=hT[:, kk, n0:n0 + nn], start=(kk == 0), stop=(kk == NK - 1)),
                            r=[wt, hT], w=[ps])
                    ee = k.ev()
                    if ee == "act":
                        k.op("act", lambda e, ps=ps, ot=ot, nr=nr, nn=nn: e.copy(out=ot[0:nr, 0:nn], in_=ps[0:nr, 0:nn]), r=[ps], w=[ot])
                    else:
                        k.op("dve", lambda e, ps=ps, ot=ot, nr=nr, nn=nn: e.tensor_copy(out=ot[0:nr, 0:nn], in_=ps[0:nr, 0:nn]), r=[ps], w=[ot])
                    k.dma(S['pfm'][r0:r0 + nr, t0 + n0:t0 + n0 + nn], ot[0:nr, 0:nn], r=[ot], q=("sp", "sp")[no % 2])
            for vi, nm in enumerate(('ret_v', 'gla_v')):
                s0 = SEG[nm][0]
                k.dma(wvs[:], wbv[:, :, s0:s0 + 512], w=[wvs])
                for tb in range(tg // 128):
                    ps = pso[no % 4]
                    ot = ots[no % 4]
                    no += 1
                    for kk in range(NK):
                        k.op("pe", lambda e, ps=ps, kk=kk, tb=tb: e.matmul(
                            ps[:, :], lhsT=hT[:, kk, tb * 128:(tb + 1) * 128], rhs=wvs[:, kk, :], start=(kk == 0), stop=(kk == NK - 1)),
                            r=[wvs, hT], w=[ps])
                    ee = k.ev()
                    if ee == "act":
                        k.op("act", lambda e, ps=ps, ot=ot: e.copy(out=ot[:], in_=ps[:]), r=[ps], w=[ot])
                    else:
                        k.op("dve", lambda e, ps=ps, ot=ot: e.tensor_copy(out=ot[:], in_=ps[:]), r=[ps], w=[ot])
                    tok = t0 + tb * 128
                    k.dma(S['ptm'][tok:tok + 128, vi * 512:(vi + 1) * 512], ot[:], r=[ot], q=("sp", "sp")[no % 2])


TWO_PI = 2.0 * math.pi


def tt(k, e_, out, a, b, op, r, w):
    return k.op(e_, lambda e: e.tensor_tensor(out=out, in0=a, in1=b, op=op), r=r, w=w)


def ts(k, e_, out, a, s1, s2, op0, op1, r, w):
    if op1 is None:
        return k.op(e_, lambda e: e.tensor_scalar(out=out, in0=a, scalar1=s1, scalar2=None, op0=op0), r=r, w=w)
    return k.op(e_, lambda e: e.tensor_scalar(out=out, in0=a, scalar1=s1, scalar2=s2, op0=op0, op1=op1), r=r, w=w)


def act(k, out, a, func, r, w, bias=None, scale=None):
    kw = {}
    if bias is not None:
        kw['bias'] = bias
    if scale is not None:
        kw['scale'] = scale
    return k.op("act", lambda e: e.activation(out=out, in_=a, func=func, **kw), r=r, w=w)


def frac_inplace(k, X, Ti, r, eng_cast="pool"):
    cp(k, eng_cast, Ti, X, [r[0]], [r[1]])
    tt(k, "pool", X, X, Ti, ALU.subtract, [r[0], r[1]], [r[0]])
    k.op("dve", lambda e: e.scalar_tensor_tensor(out=X, in0=X, scalar=0.0, in1=X, op0=ALU.is_lt, op1=ALU.add), r=[r[0]], w=[r[0]])


def rsqrt_inplace(k, X, r, bias_ap, rb, scale=1.0):
    k.op("act", lambda e: e.activation(out=X, in_=X, func=AF.Sqrt, bias=bias_ap, scale=scale), r=[r, rb], w=[r])
    k.op("dve", lambda e: e.reciprocal(out=X, in_=X), r=[r], w=[r])


def frac_pos_OLD(k, ph, x, name):
    n = x.t.shape[1]
    m = ph.sb([128, n], F32, name + "_m")
    ts(k, "pool", x[:], x[:], 1.0, None, ALU.mod, None, [x], [x])
    ts(k, "dve", m[:], x[:], 0.0, None, ALU.is_lt, None, [x], [m])
    tt(k, "dve", x[:], x[:], m[:], ALU.add, [x, m], [x])


def phase_s5(g, i):
    k, I, S, L, CTX, T = g.k, g.I, g.S, g.L, g.CTX, g.T
    u0 = FM['s5_u'][0]
    with g.c.phase() as ph:
        NS = 32
        are = ph.sb([128, NS], F32, "are"); aim = ph.sb([128, NS], F32, "aim"); dtt = ph.sb([128, NS], F32, "dtt")
        k.dma(are[:], I['s5_are'][i], w=[are]); k.dma(aim[:], I['s5_aim'][i], w=[aim]); k.dma(dtt[:], I['s5_ldt'][i], w=[dtt])
        negpi = ph.sb([128, 1], F32, "negpi")
        k.op("pool", lambda e: e.memset(negpi[:], -math.pi), w=[negpi])
        neghalf = ph.sb([128, 1], F32, "neghalf"); halfpi = ph.sb([128, 1], F32, "halfpi")
        k.op("pool", lambda e: e.memset(neghalf[:], -0.5), w=[neghalf])
        k.op("pool", lambda e: e.memset(halfpi[:], 0.5 * math.pi), w=[halfpi])
        act(k, dtt[:], dtt[:], AF.Exp, [dtt], [dtt])
        rho = ph.sb([128, NS], F32, "rho")
        tt(k, "dve", rho[:], are[:], dtt[:], ALU.mult, [are, dtt], [rho])
        act(k, rho[:], rho[:], AF.Exp, [rho], [rho])
        phi = ph.sb([128, NS], F32, "phi")
        tt(k, "dve", phi[:], aim[:], dtt[:], ALU.mult, [aim, dtt], [phi])
        ts(k, "dve", phi[:], phi[:], 1.0 / TWO_PI, None, ALU.mult, None, [phi], [phi])
        phii = ph.sb([128, NS], mybir.dt.int32, "phii")
        frac_inplace(k, phi[:], phii[:], [phi, phii])
        ph2 = ph.sb([128, NS], F32, "ph2")
        nsin = ph.sb([128, NS], F32, "nsin"); ncos = ph.sb([128, NS], F32, "ncos")
        act(k, nsin[:], phi[:], AF.Sin, [phi, negpi], [nsin], bias=negpi[:, 0:1], scale=TWO_PI)
        act(k, ph2[:], phi[:], AF.Abs, [phi, neghalf], [ph2], bias=neghalf[:, 0:1])
        act(k, ncos[:], ph2[:], AF.Sin, [ph2, halfpi], [ncos], bias=halfpi[:, 0:1], scale=-TWO_PI)
        abre = ph.sb([128, NS], F32, "abre"); abim = ph.sb([128, NS], F32, "abim")
        k.op("dve", lambda e: e.scalar_tensor_tensor(out=abre[:], in0=ncos[:], scalar=-1.0, in1=rho[:], op0=ALU.mult, op1=ALU.mult), r=[ncos, rho], w=[abre])
        k.op("dve", lambda e: e.scalar_tensor_tensor(out=abim[:], in0=nsin[:], scalar=-1.0, in1=rho[:], op0=ALU.mult, op1=ALU.mult), r=[nsin, rho], w=[abim])
        nr = ph.sb([128, NS], F32, "nr"); den = ph.sb([128, NS], F32, "den"); t1 = ph.sb([128, NS], F32, "t1"); t2 = ph.sb([128, NS], F32, "t2")
        core = ph.sb([128, NS], F32, "core"); coim = ph.sb([128, NS], F32, "coim")
        ts(k, "dve", nr[:], abre[:], -1.0, None, ALU.add, None, [abre], [nr])
        tt(k, "dve", den[:], are[:], are[:], ALU.mult, [are], [den])
        tt(k, "dve", t1[:], aim[:], aim[:], ALU.mult, [aim], [t1])
        tt(k, "dve", den[:], den[:], t1[:], ALU.add, [den, t1], [den])
        k.op("dve", lambda e: e.reciprocal(out=den[:], in_=den[:]), r=[den], w=[den])
        tt(k, "dve", t1[:], nr[:], are[:], ALU.mult, [nr, are], [t1])
        tt(k, "dve", t2[:], abim[:], aim[:], ALU.mult, [abim, aim], [t2])
        tt(k, "dve", t1[:], t1[:], t2[:], ALU.add, [t1, t2], [t1])
        tt(k, "dve", core[:], t1[:], den[:], ALU.mult, [t1, den], [core])
        tt(k, "dve", t1[:], abim[:], are[:], ALU.mult, [abim, are], [t1])
        tt(k, "dve", t2[:], nr[:], aim[:], ALU.mult, [nr, aim], [t2])
        tt(k, "dve", t1[:], t1[:], t2[:], ALU.subtract, [t1, t2], [t1])
        tt(k, "dve", coim[:], t1[:], den[:], ALU.mult, [t1, den], [coim])
        tv = ph.sb([128, T], F32, "tv")
        k.dma(tv[:], I['tvals'][:, 0:T], w=[tv])
        d32 = ph.sb([32, 16], F32, "d32")
        k.dma(d32[:], I['s5_d32'][i], w=[d32])
        cosT = ph.sb([128, T], F32, "cosT"); sinT = ph.sb([128, T], F32, "sinT")
        rhoT = ph.sb([128, T], F32, "rhoT")
        zre = ph.sb([128, T], F32, "zre"); zim = ph.sb([128, T], F32, "zim")
        gre = ph.sb([128, T], F32, "gre"); gim = ph.sb([128, T], F32, "gim")
        pht = gre
        hre = ph.sb([128, T], BF16, "hre"); him = ph.sb([128, T], BF16, "him")
        uf = gim; ub = ph.sb([32, T], BF16, "ub")
        yF = ph.sb([32, T], F32, "yF")
        zr = ph.sb([128, 512], F32, "zr"); zi = ph.sb([128, 512], F32, "zi")
        ta = ph.sb([128, 512], F32, "ta"); tb_ = ph.sb([128, 512], F32, "tb")
        tc_ = zre; td_ = zim
        bre_f = ph.sb([32, 128], F32, "bref"); bim_f = ph.sb([32, 128], F32, "bimf")
        bre = ph.sb([32, 128], BF16, "bre"); bim = ph.sb([32, 128], BF16, "bim")
        cre_f = ph.sb([128, 32], F32, "cref"); cim_f = ph.sb([128, 32], F32, "cimf")
        cre = ph.sb([128, 32], BF16, "cre"); cimn = ph.sb([128, 32], BF16, "cimn")
        ct1 = ph.sb([128, 32], F32, "ct1"); ct2 = ph.sb([128, 32], F32, "ct2")
        pz = [ph.ps([128, 512], F32, "pzr"), ph.ps([128, 512], F32, "pzi")]
        py = [ph.ps([32, 512], F32, "py%d" % j) for j in range(2)]
        npy = 0
        for gp in range(16):
            k.dma(uf[0:32, :], S['pfm'][u0 + gp * 32:u0 + gp * 32 + 32, :], w=[uf])
            k.op("pool", lambda e: e.tensor_copy(out=ub[:], in_=uf[0:32, :]), r=[uf], w=[ub])
            ts(k, "dve", yF[:], uf[0:32, :], d32[:, gp:gp + 1], None, ALU.mult, None, [uf, d32], [yF])
            for dr in range(2):
                col = dr * 16 + gp
                k.dma(bre_f[:], I['s5_Bre'][i, dr, gp], w=[bre_f]); k.dma(bim_f[:], I['s5_Bim'][i, dr, gp], w=[bim_f])
                k.dma(cre_f[:], I['s5_Cre'][i, dr, gp], w=[cre_f]); k.dma(cim_f[:], I['s5_Cim'][i, dr, gp], w=[cim_f])
                k.op("pool", lambda e: e.tensor_copy(out=bre[:], in_=bre_f[:]), r=[bre_f], w=[bre])
                k.op("pool", lambda e: e.tensor_copy(out=bim[:], in_=bim_f[:]), r=[bim_f], w=[bim])
                ts(k, "dve", ct1[:], cre_f[:], core[:, col:col + 1], None, ALU.mult, None, [cre_f, core], [ct1])
                ts(k, "dve", ct2[:], cim_f[:], coim[:, col:col + 1], None, ALU.mult, None, [cim_f, coim], [ct2])
                tt(k, "dve", cre[:], ct1[:], ct2[:], ALU.subtract, [ct1, ct2], [cre])
                ts(k, "dve", ct1[:], cre_f[:], coim[:, col:col + 1], None, ALU.mult, None, [cre_f, coim], [ct1])
                ts(k, "dve", ct2[:], cim_f[:], core[:, col:col + 1], None, ALU.mult, None, [cim_f, core], [ct2])
                k.op("dve", lambda e: e.scalar_tensor_tensor(out=cimn[:], in0=ct1[:], scalar=-1.0, in1=ct2[:], op0=ALU.mult, op1=ALU.subtract), r=[ct1, ct2], w=[cimn])
                ts(k, "dve", pht[:], tv[:], phi[:, col:col + 1], None, ALU.mult, None, [tv, phi], [pht])
                frac_inplace(k, pht[:], zim[:].bitcast(mybir.dt.int32), [pht, zim])
                act(k, sinT[:], pht[:], AF.Sin, [pht, negpi], [sinT], bias=negpi[:, 0:1], scale=TWO_PI)
                act(k, pht[:], pht[:], AF.Abs, [pht, neghalf], [pht], bias=neghalf[:, 0:1])
                act(k, cosT[:], pht[:], AF.Sin, [pht, halfpi], [cosT], bias=halfpi[:, 0:1], scale=-TWO_PI)
                ts(k, "dve", rhoT[:], tv[:], 0.0, rho[:, col:col + 1], ALU.mult, ALU.add, [tv, rho], [rhoT])
                blocks = []
                if dr == 0:
                    for n0 in range(0, T, 512):
                        nn = min(512, T - n0)
                        blocks.append((n0, nn, slice(n0, n0 + nn)))
                else:
                    def rs(hi, nn):
                        lo = hi - nn
                        return slice(hi - 1, lo - 1 if lo > 0 else None, -1)
                    n0 = 0
                    for c0 in range(CTX, 0, -512):
                        nn = min(512, c0)
                        blocks.append((n0, nn, rs(c0, nn))); n0 += nn
                    for c0 in range(T, CTX, -512):
                        nn = min(512, c0 - CTX)
                        blocks.append((n0, nn, rs(c0, nn))); n0 += nn
                for (n0, nn, cs) in blocks:
                    k.op("pe", lambda e, cs=cs, nn=nn: e.matmul(pz[0][:, 0:nn], lhsT=bre[:], rhs=ub[:, cs], start=True, stop=True), r=[bre, ub], w=[pz[0]])
                    k.op("pe", lambda e, cs=cs, nn=nn: e.matmul(pz[1][:, 0:nn], lhsT=bim[:], rhs=ub[:, cs], start=True, stop=True), r=[bim, ub], w=[pz[1]])
                    k.op("act", lambda e, nn=nn: e.copy(out=zr[:, 0:nn], in_=pz[0][:, 0:nn]), r=[pz[0]], w=[zr])
                    k.op("act", lambda e, nn=nn: e.copy(out=zi[:, 0:nn], in_=pz[1][:, 0:nn]), r=[pz[1]], w=[zi])
                    ns = slice(n0, n0 + nn)
                    tt(k, "dve", ta[:, 0:nn], zr[:, 0:nn], cosT[:, ns], ALU.mult, [zr, cosT], [ta])
                    tt(k, "pool", tb_[:, 0:nn], zi[:, 0:nn], sinT[:, ns], ALU.mult, [zi, sinT], [tb_])
                    tt(k, "dve", zre[:, ns], ta[:, 0:nn], tb_[:, 0:nn], ALU.add, [ta, tb_], [zre])
                    tt(k, "pool", ta[:, 0:nn], zi[:, 0:nn], cosT[:, ns], ALU.mult, [zi, cosT], [ta])
                    tt(k, "dve", tb_[:, 0:nn], zr[:, 0:nn], sinT[:, ns], ALU.mult, [zr, sinT], [tb_])
                    tt(k, "pool", zim[:, ns], ta[:, 0:nn], tb_[:, 0:nn], ALU.subtract, [ta, tb_], [zim])
                k.op("dve", lambda e: e.tensor_tensor_scan(out=gre[:], data0=rhoT[:], data1=zre[:], initial=0.0, op0=ALU.mult, op1=ALU.add), r=[rhoT, zre], w=[gre])
                k.op("dve", lambda e: e.tensor_tensor_scan(out=gim[:], data0=rhoT[:], data1=zim[:], initial=0.0, op0=ALU.mult, op1=ALU.add), r=[rhoT, zim], w=[gim])
                tt(k, "pool", tc_[:], gre[:], cosT[:], ALU.mult, [gre, cosT], [tc_])
                tt(k, "dve", td_[:], gim[:], sinT[:], ALU.mult, [gim, sinT], [td_])
                tt(k, "pool", hre[:], tc_[:], td_[:], ALU.subtract, [tc_, td_], [hre])
                tt(k, "dve", tc_[:], gre[:], sinT[:], ALU.mult, [gre, sinT], [tc_])
                tt(k, "pool", td_[:], gim[:], cosT[:], ALU.mult, [gim, cosT], [td_])
                tt(k, "dve", him[:], tc_[:], td_[:], ALU.add, [tc_, td_], [him])
                for (n0, nn, cs) in blocks:
                    p_ = py[npy % 2]; npy += 1
                    ns = slice(n0, n0 + nn)
                    k.op("pe", lambda e, p_=p_, ns=ns, nn=nn: e.matmul(p_[:, 0:nn], lhsT=cre[:], rhs=hre[:, ns], start=True, stop=False), r=[cre, hre], w=[p_])
                    k.op("pe", lambda e, p_=p_, ns=ns, nn=nn: e.matmul(p_[:, 0:nn], lhsT=cimn[:], rhs=him[:, ns], start=False, stop=True), r=[cimn, him], w=[p_])
                    tt(k, "dve", yF[:, cs], yF[:, cs], p_[:, 0:nn], ALU.add, [yF, p_], [yF])
            k.dma(S['ys5'][gp * 32:gp * 32 + 32, :], yF[:], r=[yF])
    with g.c.phase() as ph:
        gw_f = ph.sb([128, 4, 512], F32, "gwf"); gw = ph.sb([128, 4, 512], BF16, "gw")
        k.dma(gw_f[:], I['s5_glu_w'][i].rearrange("(k p) c -> p k c", p=128), w=[gw_f])
        k.op("pool", lambda e: e.tensor_copy(out=gw[:], in_=gw_f[:]), r=[gw_f], w=[gw])
        gb = ph.sb([128, 4], F32, "gb")
        k.dma(gb[:], I['s5_glu_b4'][i], w=[gb])
        ys = [ph.sb([128, 512], F32, "ys%d" % j) for j in range(4)]
        Gf = [ph.sb([128, 512], F32, "Gf%d" % j) for j in range(4)]
        Gb = [ph.sb([128, 512], BF16, "Gb%d" % j) for j in range(4)]
        w1 = ph.sb([128, 512], F32, "w1"); w2 = ph.sb([128, 512], F32, "w2")
        gt_ = [ph.sb([128, 512], F32, "gt%d" % j) for j in range(2)]
        ob = [ph.sb([128, 512], BF16, "ob%d" % j) for j in range(2)]
        pg = [ph.ps([128, 512], F32, "pg%d" % j) for j in range(2)]
        g0 = FM['s5_g'][0]
        n_ = 0
        for n0 in range(0, T, 512):
            nn = min(512, T - n0)
            for j in range(4):
                k.dma(ys[j][:, 0:nn], S['ys5'][j * 128:(j + 1) * 128, n0:n0 + nn], w=[ys[j]])
                gelu_tanh(k, Gf[j], Gb[j], ys[j], w1, w2, nn)
            for j in range(4):
                p_ = pg[n_ % 2]; g_ = gt_[n_ % 2]; o_ = ob[n_ % 2]; n_ += 1
                for kt in range(4):
                    k.op("pe", lambda e, p_=p_, kt=kt, j=j: e.matmul(p_[:, 0:nn], lhsT=gw[:, kt, j * 128:(j + 1) * 128], rhs=Gb[kt][:, 0:nn], start=(kt == 0), stop=(kt == 3)),
                         r=[gw, Gb[kt]], w=[p_])
                act(k, w1[:, 0:nn], p_[:, 0:nn], AF.Sigmoid, [p_, gb], [w1], bias=gb[:, j:j + 1])
                tt(k, "dve", w1[:, 0:nn], w1[:, 0:nn], Gf[j][:, 0:nn], ALU.mult, [w1, Gf[j]], [w1])
                k.dma(g_[:, 0:nn], S['pfm'][g0 + j * 128:g0 + (j + 1) * 128, n0:n0 + nn], w=[g_])
                act(k, g_[:, 0:nn], g_[:, 0:nn], AF.Silu, [g_], [g_])
                tt(k, "dve", o_[:, 0:nn], w1[:, 0:nn], g_[:, 0:nn], ALU.mult, [w1, g_], [o_])
                k.dma(S['y'][j * 128:(j + 1) * 128, n0:n0 + nn], o_[:, 0:nn], r=[o_])


def gelu_tanh(k, outf, outb, x, w1, w2, nn):
    tt(k, "pool", w1[:, 0:nn], x[:, 0:nn], x[:, 0:nn], ALU.mult, [x], [w1])
    ts(k, "dve", w1[:, 0:nn], w1[:, 0:nn], 0.044715, 1.0, ALU.mult, ALU.add, [w1], [w1])
    tt(k, "dve", w2[:, 0:nn], w1[:, 0:nn], x[:, 0:nn], ALU.mult, [w1, x], [w2])
    act(k, w2[:, 0:nn], w2[:, 0:nn], AF.Sigmoid, [w2], [w2], scale=2.0 * math.sqrt(2.0 / math.pi))
    tt(k, "dve", outf[:, 0:nn], w2[:, 0:nn], x[:, 0:nn], ALU.mult, [w2, x], [outf])
    k.op("pool", lambda e: e.tensor_copy(out=outb[:, 0:nn], in_=outf[:, 0:nn]), r=[outf], w=[outb])


def lin_attn_tile(g, ph, B_, q32, k32, laF, laB, vts, C, dk, yrow0, grow0, nw_cols, layer, nwoff=0):
    k, I, S, L, CTX, T = g.k, g.I, g.S, g.L, g.CTX, g.T
    HPT = 128 // dk
    NCH = T // C
    ncc = CTX // C
    for dr in range(2):
        la = laF if dr == 0 else laB
        b = B_['b']
        if dr == 0:
            k.op("dve", lambda e: e.tensor_tensor_scan(out=b[:], data0=B_['m01'][:, 0:T], data1=la[:], initial=0.0, op0=ALU.mult, op1=ALU.add), r=[B_['m01'], la], w=[b])
        else:
            k.op("dve", lambda e: e.tensor_tensor_scan(out=b[:, ::-1], data0=B_['m01'][:, T:0:-1], data1=la[:, ::-1], initial=0.0, op0=ALU.mult, op1=ALU.add), r=[B_['m01'], la], w=[b])
        b3 = b[:].rearrange("p (n c) -> p n c", c=C)
        rc = C // 2 - 1 if dr == 0 else C // 2
        lc = C - 1 if dr == 0 else 0
        ref = b3[:, :, rc:rc + 1].to_broadcast([128, NCH, C])
        bl = b3[:, :, lc:lc + 1].to_broadcast([128, NCH, C])
        tmp = la
        t3 = tmp[:].rearrange("p (n c) -> p n c", c=C)
        tt(k, "pool", t3, b3, ref, ALU.subtract, [b], [tmp])
        act(k, tmp[:], tmp[:], AF.Exp, [tmp], [tmp])
        tt(k, "dve", B_['qt'][:], q32[:], tmp[:], ALU.mult, [q32, tmp], [B_['qt']])
        tt(k, "pool", t3, ref, b3, ALU.subtract, [b], [tmp])
        act(k, tmp[:], tmp[:], AF.Exp, [tmp], [tmp])
        tt(k, "dve", B_['kt'][:], k32[:], tmp[:], ALU.mult, [k32, tmp], [B_['kt']])
        tt(k, "pool", t3, bl, b3, ALU.subtract, [b], [tmp])
        act(k, tmp[:], tmp[:], AF.Exp, [tmp], [tmp])
        tt(k, "dve", B_['ks'][:], k32[:], tmp[:], ALU.mult, [k32, tmp], [B_['ks']])
        dec = B_['dec']
        k.op("act", lambda e: e.activation(out=dec[:], in_=b3[:, :, lc], func=AF.Exp), r=[b], w=[dec])
        if g.cut == 'prep':
            continue
        kT = B_['kT']
        per = 1024 // 128
        for j0 in range(0, NCH, per):
            nj = min(per, NCH - j0)
            p_ = B_['pT'][(j0 // per) % 2]
            for jj in range(nj):
                j = j0 + jj
                k.op("pe", lambda e, p_=p_, jj=jj, j=j: e.transpose(out=p_[0:C, jj * 128:(jj + 1) * 128], in_=B_['ks'][:, j * C:(j + 1) * C], identity=B_['ident'][:]),
                     r=[B_['ks'], B_['ident']], w=[p_])
            src = p_[0:C, 0:nj * 128].rearrange("p (a b) -> p a b", b=128)
            k.op("dve", lambda e, src=src, j0=j0, nj=nj: e.tensor_copy(out=kT[0:C, j0:j0 + nj, :], in_=src), r=[p_], w=[kT])
        act(k, tmp[:], b[:], AF.Exp, [b], [tmp])
        tt(k, "dve", B_['qs'][:], q32[:], tmp[:], ALU.mult, [q32, tmp], [B_['qs']])
        if g.cut == 'tr':
            continue
        if dr == 0:
            order = list(range(NCH))
        else:
            order = list(range(ncc - 1, -1, -1)) + list(range(NCH - 1, ncc - 1, -1))
        St = B_['St']
        Sall = B_['Sall']
        k.op("pool", lambda e: e.memset(St[:], 0.0), w=[St])
        for j in order:
            k.op("pool", lambda e, j=j: e.tensor_copy(out=Sall[:, j, :], in_=St[:]), r=[St], w=[Sall])
            for hh in range(HPT):
                p_ = B_['pkv'][hh]
                k.op("pe", lambda e, p_=p_, j=j, hh=hh: e.matmul(p_[:, 0:128], lhsT=kT[0:C, j, :], rhs=vts[hh][0:C, j, :], start=True, stop=True), r=[kT, vts[hh]], w=[p_])
                rs_ = slice(hh * dk, (hh + 1) * dk)
                k.op("dve", lambda e, p_=p_, j=j, rs_=rs_: e.scalar_tensor_tensor(out=St[rs_, :], in0=St[rs_, :], scalar=dec[rs_, j:j + 1], in1=p_[rs_, 0:128], op0=ALU.mult, op1=ALU.add),
                     r=[St, dec, p_], w=[St])
        if g.cut == 'A':
            continue
        mk = B_['maskF'] if dr == 0 else B_['maskB']
        na = 0
        for j in range(NCH):
            cs = slice(j * C, (j + 1) * C)
            for hh in range(HPT):
                rs_ = slice(hh * dk, (hh + 1) * dk)
                pa = B_['patt'][na % 2]; po = B_['po'][na % 2]; am = B_['am'][na % 2]; na += 1
                k.op("pe", lambda e, pa=pa, rs_=rs_, cs=cs: e.matmul(pa[0:C, 0:C], lhsT=B_['kt'][rs_, cs], rhs=B_['qt'][rs_, cs], start=True, stop=True), r=[B_['kt'], B_['qt']], w=[pa])
                tt(k, "dve", am[0:C, 0:C], pa[0:C, 0:C], mk[0:C, 0:C], ALU.mult, [pa, mk], [am])
                o_ = B_['o'][hh]
                if hh == 0:
                    k.op("pe", lambda e, po=po, j=j, hh=hh, am=am: e.matmul(po[:, 0:C], lhsT=vts[hh][0:C, j, :], rhs=am[0:C, 0:C], start=True, stop=False), r=[vts[hh], am], w=[po])
                    k.op("pe", lambda e, po=po, j=j, rs_=rs_, cs=cs: e.matmul(po[:, 0:C], lhsT=Sall[rs_, j, :], rhs=B_['qs'][rs_, cs], start=False, stop=True), r=[Sall, B_['qs']], w=[po])
                    po2 = None
                else:
                    po2 = B_['pkv'][na % 2]
                    k.op("pe", lambda e, po=po, j=j, hh=hh, am=am: e.matmul(po[:, 0:C], lhsT=vts[hh][0:C, j, :], rhs=am[0:C, 0:C], start=True, stop=True), r=[vts[hh], am], w=[po])
                    k.op("pe", lambda e, po2=po2, j=j, rs_=rs_, cs=cs: e.matmul(po2[:, 0:C], lhsT=Sall[rs_, j, :], rhs=B_['qs'][rs_, cs], start=True, stop=True), r=[Sall, B_['qs']], w=[po2])
                if dr == 0:
                    k.op("act", lambda e, o_=o_, po=po, cs=cs: e.copy(out=o_[:, cs], in_=po[:, 0:C]), r=[po], w=[o_])
                else:
                    tt(k, "dve", o_[:, cs], o_[:, cs], po[:, 0:C], ALU.add, [o_, po], [o_])
                if po2 is not None:
                    tt(k, "dve", o_[:, cs], o_[:, cs], po2[:, 0:C], ALU.add, [o_, po2], [o_])
    if g.cut in ('prep', 'tr', 'A', 'B'):
        return
    for hh in range(HPT):
        o_ = B_['o'][hh]
        r0 = yrow0 + hh * 128
        for n0 in range(0, T, 512):
            nn = min(512, T - n0)
            ns = slice(n0, n0 + nn)
            sq = B_['w512'][0]; gt_ = B_['w512'][1]; ob = B_['ob']
            tt(k, "pool", sq[:, 0:nn], o_[:, ns], o_[:, ns], ALU.mult, [o_], [sq])
            pm = B_['patt'][0]
            k.op("pe", lambda e, pm=pm, sq=sq, nn=nn: e.matmul(pm[:, 0:nn], lhsT=B_['onesm'][:], rhs=sq[:, 0:nn], start=True, stop=True), r=[B_['onesm'], sq], w=[pm])
            k.op("act", lambda e, sq=sq, pm=pm, nn=nn: e.activation(out=sq[:, 0:nn], in_=pm[:, 0:nn], func=AF.Sqrt, bias=B_['epsb'][:, 0:1]), r=[pm, B_['epsb']], w=[sq])
            k.op("dve", lambda e, sq=sq, nn=nn: e.reciprocal(out=sq[:, 0:nn], in_=sq[:, 0:nn]), r=[sq], w=[sq])
            tt(k, "dve", sq[:, 0:nn], sq[:, 0:nn], o_[:, ns], ALU.mult, [sq, o_], [sq])
            k.dma(gt_[:, 0:nn], S['pfm'][grow0 + hh * 128:grow0 + (hh + 1) * 128, n0:n0 + nn], w=[gt_])
            act(k, gt_[:, 0:nn], gt_[:, 0:nn], AF.Silu, [gt_], [gt_])
            k.op("dve", lambda e, sq=sq, gt_=gt_, nn=nn, hh=hh: e.scalar_tensor_tensor(out=ob[:, 0:nn], in0=sq[:, 0:nn], scalar=nw_cols[:, nwoff + hh:nwoff + hh + 1], in1=gt_[:, 0:nn], op0=ALU.mult, op1=ALU.mult),
                 r=[sq, gt_, nw_cols], w=[ob])
            k.dma(S['y'][r0:r0 + 128, n0:n0 + nn], ob[:, 0:nn], r=[ob])


def la_bufs(g, ph, C):
    k, I, T = g.k, g.I, g.T
    NCH = T // C
    B_ = {}
    for nm in ('b',):
        B_[nm] = ph.sb([128, T], F32, nm)
    for nm in ('qt', 'kt', 'ks'):
        B_[nm] = ph.sb([128, T], BF16, nm)
    B_['qs'] = B_['ks']
    B_['m01'] = ph.sb([128, T + 1], F32, "m01")
    k.dma(B_['m01'][:], I['m01_%d' % C][:, :], w=[B_['m01']])
    B_['dec'] = ph.sb([128, NCH], F32, "dec")
    B_['kT'] = ph.sb([128, NCH, 128], BF16, "kT")
    B_['Sall'] = ph.sb([128, NCH, 128], BF16, "Sall")
    B_['St'] = ph.sb([128, 128], F32, "St")
    B_['ident'] = ph.sb([128, 128], BF16, "identb")
    k.dma(B_['ident'][:], I['ident_bf'][:, :], w=[B_['ident']])
    B_['maskF'] = ph.sb([128, 128], F32, "maskF"); B_['maskB'] = ph.sb([128, 128], F32, "maskB")
    k.dma(B_['maskF'][:], I['maskF'][:, :], w=[B_['maskF']]); k.dma(B_['maskB'][:], I['maskB'][:, :], w=[B_['maskB']])
    B_['epsb'] = ph.sb([128, 1], F32, "lepsb")
    k.op("pool", lambda e: e.memset(B_['epsb'][:], EPS), w=[B_['epsb']])
    B_['onesm'] = ph.sb([128, 128], F32, "onesm")
    k.op("pool", lambda e: e.memset(B_['onesm'][:], 1.0 / 128.0), w=[B_['onesm']])
    B_['pT'] = [ph.ps([128, 1024], BF16, "lpT%d" % j) for j in range(2)]
    B_['pkv'] = [ph.ps([128, 512], F32, "pkv%d" % j) for j in range(2)]
    B_['patt'] = [ph.ps([128, 512], F32, "patt%d" % j) for j in range(2)]
    B_['po'] = [ph.ps([128, 512], F32, "po%d" % j) for j in range(2)]
    B_['am'] = [ph.sb([128, 128], BF16, "am%d" % j) for j in range(2)]
    B_['w512'] = [ph.sb([128, 512], F32, "w512_%d" % j) for j in range(2)]
    B_['ob'] = ph.sb([128, 512], BF16, "ob")
    return B_


def load_v(g, ph, C, col0, name, B_):
    k, S, T = g.k, g.S, g.T
    NCH = T // C
    halves = (NCH * 128) // T
    nph = NCH // halves
    b = B_['b']
    bv = b[0:C, :].rearrange("p (n e) -> p n e", e=128)
    vb = ph.sb([C, NCH, 128], BF16, name)
    for hf in range(halves):
        r0 = hf * nph * C
        k.dma(bv, S['ptm'][r0:r0 + nph * C, col0:col0 + 128].rearrange("(n c) e -> c n e", c=C), w=[b])
        k.op("pool", lambda e, hf=hf: e.tensor_copy(out=vb[:, hf * nph:(hf + 1) * nph, :], in_=bv), r=[b], w=[vb])
    return vb


def phase_ret(g, i):
    k, I, S, L, CTX, T = g.k, g.I, g.S, g.L, g.CTX, g.T
    for h in range(4):
        with g.c.phase() as ph:
            B_ = la_bufs(g, ph, 128)
            B_['o'] = [ph.sb([128, T], F32, "o0")]
            rc_ = ph.sb([128, T], F32, "ropeC"); rs_ = ph.sb([128, T], F32, "ropeS")
            k.dma(rc_[:], I['ropeC'][:, 0:T], w=[rc_]); k.dma(rs_[:], I['ropeS'][:, 0:T], w=[rs_])
            q32 = ph.sb([128, T], F32, "q32"); k32 = ph.sb([128, T], F32, "k32")
            x2 = B_['b']
            for (dst, a, b_, scale) in ((q32, 'ret_q', 'ret_qs', 1.0), (k32, 'ret_k', 'ret_ks', 128.0 ** -0.5)):
                k.dma(dst[:], S['pfm'][FM[a][0] + h * 128:FM[a][0] + (h + 1) * 128, :], w=[dst])
                k.dma(x2[:], S['pfm'][FM[b_][0] + h * 128:FM[b_][0] + (h + 1) * 128, :], w=[x2])
                tt(k, "dve", dst[:], dst[:], rc_[:], ALU.mult, [dst, rc_], [dst])
                tt(k, "pool", x2[:], x2[:], rs_[:], ALU.mult, [x2, rs_], [x2])
                if scale == 1.0:
                    tt(k, "dve", dst[:], dst[:], x2[:], ALU.add, [dst, x2], [dst])
                else:
                    tt(k, "dve", dst[:], dst[:], x2[:], ALU.add, [dst, x2], [dst])
                    ts(k, "dve", dst[:], dst[:], scale, None, ALU.mult, None, [dst], [dst])
            lg = math.log(1.0 - 2.0 ** (-5.0 - h))
            laF = rc_; laB = rs_
            k.op("pool", lambda e: e.memset(laF[:], lg), w=[laF])
            k.op("pool", lambda e: e.memset(laB[:], lg), w=[laB])
            vts = [load_v(g, ph, 128, h * 128, "vr", B_)]
            nwc = ph.sb([128, 4], F32, "nwc")
            k.dma(nwc[:], I['ret_nw'][i], w=[nwc])
            lin_attn_tile(g, ph, B_, q32, k32, laF, laB, vts, 128, 128, 1024 + h * 128, FM['ret_g'][0] + h * 128, nwc, i, nwoff=h)


def phase_gla(g, i):
    k, I, S, L, CTX, T = g.k, g.I, g.S, g.L, g.CTX, g.T
    for tl in range(2):
        with g.c.phase() as ph:
            B_ = la_bufs(g, ph, 64)
            B_['o'] = [ph.sb([128, T], BF16, "o%d" % j) for j in range(2)]
            las = [ph.sb([128, T], F32, "laF"), ph.sb([128, T], F32, "laB")]
            q32 = ph.sb([128, T], BF16, "q16"); k32 = ph.sb([128, T], BF16, "k16")
            k.dma(las[0][:], S['pfm'][FM['gla_q'][0] + tl * 128:FM['gla_q'][0] + (tl + 1) * 128, :], w=[las[0]])
            k.dma(las[1][:], S['pfm'][FM['gla_k'][0] + tl * 128:FM['gla_k'][0] + (tl + 1) * 128, :], w=[las[1]])
            ts(k, "dve", q32[:], las[0][:], 64.0 ** -0.5, None, ALU.mult, None, [las[0]], [q32])
            cp(k, "dve", k32[:], las[1][:], [las[1]], [k32])
            lrb = B_['b']
            lr = lrb[0:32, :]
            k.dma(lr, S['pfm'][FM['gla_lr'][0]:FM['gla_lr'][0] + 32, :], w=[lrb])
            gwt = ph.sb([32, 2, 128], F32, "gwt")
            k.dma(gwt[:], I['gla_gw'][i, tl], w=[gwt])
            gbt = ph.sb([128, 2], F32, "gbt")
            k.dma(gbt[:], I['gla_gb'][i, tl], w=[gbt])
            ts(k, "dve", gbt[:], gbt[:], -1.0, None, ALU.mult, None, [gbt], [gbt])
            pl_ = B_['patt']
            for dr in range(2):
                for bi, n0 in enumerate(range(0, T, 512)):
                    nn = min(512, T - n0)
                    p_ = pl_[bi % 2]
                    k.op("pe", lambda e, p_=p_, dr=dr, n0=n0, nn=nn: e.matmul(p_[:, 0:nn], lhsT=gwt[:, dr, :], rhs=lr[:, n0:n0 + nn], start=True, stop=True), r=[gwt, lrb], w=[p_])
                    negb = B_['w512'][0]
                    act(k, negb[:, 0:nn], p_[:, 0:nn], AF.Exp, [p_, gbt], [negb], bias=gbt[:, dr:dr + 1], scale=-1.0)
                    ts(k, "dve", negb[:, 0:nn], negb[:, 0:nn], 1.0, None, ALU.add, None, [negb], [negb])
                    act(k, negb[:, 0:nn], negb[:, 0:nn], AF.Ln, [negb], [negb])
                    ts(k, "dve", las[dr][:, n0:n0 + nn], negb[:, 0:nn], -1.0 / 16.0, None, ALU.mult, None, [negb], [las[dr]])
            vts = [load_v(g, ph, 64, 512 + (tl * 2 + hh) * 128, "vg%d" % hh, B_) for hh in range(2)]
            nwc = ph.sb([128, 4], F32, "nwc")
            k.dma(nwc[:], I['gla_nw'][i], w=[nwc])
            lin_attn_tile(g, ph, B_, q32, k32, las[0], las[1], vts, 64, 64, 1536 + tl * 256, FM['gla_g'][0] + tl * 256, nwc, i, nwoff=tl * 2)


def cp(k, e_, out, in_, r, w):
    if e_ == "act":
        return k.op("act", lambda e: e.copy(out=out, in_=in_), r=r, w=w)
    return k.op(e_, lambda e: e.tensor_copy(out=out, in_=in_), r=r, w=w)


def phase_hyena(g, i, Lh, tok0, tg):
    k, I, S = g.k, g.I, g.S
    N = 2 * Lh
    NT, LT = N // 128, Lh // 128
    hx0 = FM['hy_x'][0]
    PI = math.pi
    with g.c.phase() as ph:
        w1 = ph.sb([33, 64], F32, "hw1"); w2 = ph.sb([64, 64], F32, "hw2"); w3 = ph.sb([64, 64], F32, "hw3"); w4 = ph.sb([64, 2048], F32, "hw4")
        k.dma(w1[:], I['hy_w1'][i], w=[w1]); k.dma(w2[:], I['hy_w2'][i], w=[w2]); k.dma(w3[:], I['hy_w3'][i], w=[w3]); k.dma(w4[:], I['hy_w4'][i], w=[w4])
        bf = ph.sb([64, 6], F32, "hbf")
        k.dma(bf[:], I['hy_bf'][i], w=[bf])
        fb = ph.sb([64, 3], F32, "hfb")
        fs = ph.sb([64, 3], F32, "hfs")
        for l_ in range(3):
            tt(k, "dve", fb[:, l_:l_ + 1], bf[:, 2 * l_:2 * l_ + 1], bf[:, 2 * l_ + 1:2 * l_ + 2], ALU.mult, [bf], [fb])
            ts(k, "dve", fs[:, l_:l_ + 1], bf[:, 2 * l_ + 1:2 * l_ + 2], 1.0 / (2.0 * PI), None, ALU.mult, None, [bf], [fs])
        ts(k, "dve", fb[:], fb[:], 1.0 / (2.0 * PI), 0.5, ALU.mult, ALU.add, [fb], [fb])
        negpi = ph.sb([64, 1], F32, "hnegpi")
        k.op("pool", lambda e: e.memset(negpi[:], -PI), w=[negpi])
        zT = ph.sb([33, N], F32, "hzT")
        k.dma(zT[:], I['hy_z' + tg][:, :], w=[zT])
        hA = ph.sb([64, N], F32, "hA"); hB = ph.sb([64, N], F32, "hB")
        pm = [ph.ps([64, 512], F32, "hpm%d" % j) for j in range(2)]
        a_ = ph.sb([64, 512], F32, "ha"); ai_ = ph.sb([64, 512], mybir.dt.int32, "hai")
        srcs = [(w1, zT, 33), (w2, hA, 64), (w3, hB, 64)]
        dsts = [hA, hB, hA]
        nb = 0
        for l_ in range(3):
            w_, src, kk = srcs[l_]
            dst = dsts[l_]
            BK = min(512, N)
            for n0 in range(0, N, BK):
                p_ = pm[nb % 2]; nb += 1
                k.op("pe", lambda e, p_=p_, w_=w_, src=src, kk=kk, n0=n0: e.matmul(p_[:, 0:BK], lhsT=w_[0:kk, :], rhs=src[0:kk, n0:n0 + BK], start=True, stop=True), r=[w_, src], w=[p_])
                k.op("act", lambda e, p_=p_, l_=l_: e.activation(out=a_[:, 0:BK], in_=p_[:, 0:BK], func=AF.Identity, bias=fb[:, l_:l_ + 1], scale=fs[:, l_:l_ + 1]), r=[p_, fb, fs], w=[a_])
                frac_inplace(k, a_[:, 0:BK], ai_[:, 0:BK], [a_, ai_])
                act(k, dst[:, n0:n0 + BK], a_[:, 0:BK], AF.Sin, [a_, negpi], [dst], bias=negpi[:, 0:1], scale=2.0 * PI)
        h3 = hA
        ones = ph.sb([128, 128], F32, "hones")
        k.op("pool", lambda e: e.memset(ones[:], 1.0), w=[ones])
        pk = [ph.ps([128, 512], F32, "hpk%d" % j) for j in range(2)]
        pss = [ph.ps([128, 512], F32, "hpss%d" % j) for j in range(2)]
        dct = [ph.sb([128, 512], F32, "hdc%d" % j) for j in range(2)]
        kt_ = [ph.sb([128, 1024], F32, "hkt%d" % j) for j in range(2)]
        sq = ph.sb([128, 1024], F32, "hsq")
        kb = [ph.sb([128, 1024], BF16, "hkb%d" % j) for j in range(2)]
        for nt in range(NT):
            dr = 0 if nt * 128 < Lh else 1
            dc = dct[nt % 2]; ktile = kt_[nt % 2]; kbt = kb[nt % 2]
            k.dma(dc[:], I['hy_dec' + tg][nt * 128:(nt + 1) * 128, :], w=[dc])
            for o in range(2):
                k.op("pe", lambda e, o=o, nt=nt, dr=dr: e.matmul(pk[o][:, :], lhsT=h3[:, nt * 128:(nt + 1) * 128], rhs=w4[:, dr * 1024 + o * 512:dr * 1024 + (o + 1) * 512], start=True, stop=True), r=[h3, w4], w=[pk[o]])
                tt(k, "dve", ktile[:, o * 512:(o + 1) * 512], pk[o][:, :], dc[:], ALU.mult, [pk[o], dc], [ktile])
            tt(k, "pool", sq[:], ktile[:], ktile[:], ALU.mult, [ktile], [sq])
            for o in range(2):
                k.op("pe", lambda e, o=o, nt=nt: e.matmul(pss[o][:, :], lhsT=ones[:], rhs=sq[:, o * 512:(o + 1) * 512], start=(nt == 0), stop=(nt == NT - 1)), r=[ones, sq], w=[pss[o]])
            cp(k, "act", kbt[:], ktile[:], [ktile], [kbt])
            k.dma(S['hk'][nt * 128:(nt + 1) * 128, :], kbt[:], r=[kbt])
        rs = ph.sb([128, 1024], F32, "hrs")
        hepsb = ph.sb([128, 1], F32, "hepsb")
        k.op("pool", lambda e: e.memset(hepsb[:], EPS), w=[hepsb])
        for o in range(2):
            k.op("act", lambda e, o=o: e.activation(out=rs[:, o * 512:(o + 1) * 512], in_=pss[o][:, :], func=AF.Sqrt, bias=hepsb[:, 0:1]), r=[pss[o], hepsb], w=[rs])
        k.op("dve", lambda e: e.reciprocal(out=rs[:], in_=rs[:]), r=[rs], w=[rs])
        ts(k, "dve", rs[:], rs[:], 2.0 / N, None, ALU.mult, None, [rs], [rs])
        k.dma(S['hrs'][:, :], rs[:], r=[rs])
    if g.cut == 'h_filt':
        return
    with g.c.phase() as ph:
        rs = ph.sb([128, 1024], F32, "hrs2")
        k.dma(rs[:], S['hrs'][:, :], w=[rs])
        kbo = ph.sb([128, NT, 512], BF16, "hkbo")
        tks = [ph.sb([128, NT, 128], BF16, "htk%d" % j) for j in range(2)]
        pf = [ph.ps([128, 512], F32, "hpf%d" % j) for j in range(2)]
        ko = [ph.sb([128, 512], F32, "hko%d" % j) for j in range(2)]
        nn_ = 0
        for o in range(2):
            k.dma(kbo[:], S['hk'][0:N, o * 512:(o + 1) * 512].rearrange("(n p) c -> p n c", p=128), w=[kbo])
            for ft in range(LT):
                for ri in range(2):
                    tk = tks[nn_ % 2]; p_ = pf[nn_ % 2]; ko_ = ko[nn_ % 2]; nn_ += 1
                    k.dma(tk[:], I['hy_K%s%s' % ("re" if ri == 0 else "im", tg)][ft], w=[tk])
                    for nt in range(NT):
                        k.op("pe", lambda e, p_=p_, tk=tk, nt=nt: e.matmul(p_[:, :], lhsT=tk[:, nt, :], rhs=kbo[:, nt, :], start=(nt == 0), stop=(nt == NT - 1)), r=[tk, kbo], w=[p_])
                    tt(k, "dve", ko_[:], p_[:, :], rs[:, o * 512:(o + 1) * 512], ALU.mult, [p_, rs], [ko_])
                    if ft == 0:
                        ts(k, "dve", ko_[0:1, :], ko_[0:1, :], 0.5, None, ALU.mult, None, [ko_], [ko_])
                    k.dma(S['hKf'][o, ri, ft * 128:(ft + 1) * 128, :], ko_[:], r=[ko_])
    if g.cut == 'h_kf':
        return
    with g.c.phase() as ph:
        cw = ph.sb([128, 12, 4], F32, "hcw")
        k.dma(cw[:], I['hy_cw'][i], w=[cw])
        pt_ = [ph.sb([128, Lh + 2], F32, "hp%d" % j) for j in range(2)]
        xo = [ph.sb([128, Lh], F32, "hxo%d" % j) for j in range(2)]
        for c_ in range(12):
            p_ = pt_[c_ % 2]; x_ = xo[c_ % 2]
            k.op("pool", lambda e, p_=p_: e.memset(p_[:, 0:1], 0.0), w=[p_])
            k.op("pool", lambda e, p_=p_: e.memset(p_[:, Lh + 1:Lh + 2], 0.0), w=[p_])
            k.dma(p_[:, 1:Lh + 1], S['pfm'][hx0 + c_ * 128:hx0 + (c_ + 1) * 128, tok0:tok0 + Lh], w=[p_])
            k.op("act", lambda e, p_=p_, x_=x_, c_=c_: e.activation(out=x_[:], in_=p_[:, 1:Lh + 1], func=AF.Identity, bias=cw[:, c_, 3:4], scale=cw[:, c_, 1:2]), r=[p_, cw], w=[x_])
            k.op("dve", lambda e, p_=p_, x_=x_, c_=c_: e.scalar_tensor_tensor(out=x_[:], in0=p_[:, 0:Lh], scalar=cw[:, c_, 0:1], in1=x_[:], op0=ALU.mult, op1=ALU.add), r=[p_, cw, x_], w=[x_])
            k.op("dve", lambda e, p_=p_, x_=x_, c_=c_: e.scalar_tensor_tensor(out=x_[:], in0=p_[:, 2:Lh + 2], scalar=cw[:, c_, 2:3], in1=x_[:], op0=ALU.mult, op1=ALU.add), r=[p_, cw, x_], w=[x_])
            k.dma(S['hxs'][c_ * 128:(c_ + 1) * 128, 0:Lh], x_[:], r=[x_])
    if g.cut == 'h_sc':
        return
    for o in range(2):
        zsrc = S['hxs'][0:512, 0:Lh] if o == 0 else S['hz1'][:, 0:Lh]
        with g.c.phase() as ph:
            ident = ph.sb([128, 128], BF16, "hident")
            k.dma(ident[:], I['ident_bf'][:, :], w=[ident])
            zTt = ph.sb([128, LT, 512], BF16, "hzTt")
            zf = ph.sb([128, Lh], F32, "hzf"); zb = ph.sb([128, Lh], BF16, "hzb")
            pT = [ph.ps([128, 1024], BF16, "hpT%d" % j) for j in range(2)]
            nq = 0
            for c4 in range(4):
                k.dma(zf[:], zsrc[c4 * 128:(c4 + 1) * 128, :], w=[zf])
                cp(k, "pool", zb[:], zf[:], [zf], [zb])
                for t0 in range(0, LT, 8):
                    nj = min(8, LT - t0)
                    p_ = pT[nq % 2]; nq += 1
                    for jj in range(nj):
                        k.op("pe", lambda e, p_=p_, jj=jj, t0=t0: e.transpose(out=p_[:, jj * 128:(jj + 1) * 128], in_=zb[:, (t0 + jj) * 128:(t0 + jj + 1) * 128], identity=ident[:]), r=[zb, ident], w=[p_])
                    cp(k, k.ev(), zTt[:, t0:t0 + nj, c4 * 128:(c4 + 1) * 128], p_[:, 0:nj * 128].rearrange("p (a b) -> p a b", b=128), [p_], [zTt])
            tks = [ph.sb([128, LT, 128], BF16, "htkf%d" % j) for j in range(4)]
            pz = [ph.ps([128, 512], F32, "hpz%d" % j) for j in range(4)]
            kf = [ph.sb([128, 512], F32, "hkf%d" % j) for j in range(4)]
            wa = ph.sb([128, 512], F32, "hwa"); wb_ = ph.sb([128, 512], F32, "hwb")
            yo = [ph.sb([128, 2, 512], BF16, "hyo%d" % j) for j in range(2)]
            r0t = ph.sb([1, 2, 512], F32, "hr0t")
            for ft in range(LT):
                s_ = (ft % 2) * 2
                for ri in range(2):
                    tk = tks[s_ + ri]
                    k.dma(tk[:], I['hy_K%s%s' % ("re" if ri == 0 else "im", tg)][ft][:, 0:LT, :], w=[tk])
                    k.dma(kf[s_ + ri][:], S['hKf'][o, ri, ft * 128:(ft + 1) * 128, :], w=[kf[s_ + ri]])
                    for tt_ in range(LT):
                        k.op("pe", lambda e, ri=ri, tk=tk, tt_=tt_, s_=s_: e.matmul(pz[s_ + ri][:, :], lhsT=tk[:, tt_, :], rhs=zTt[:, tt_, :], start=(tt_ == 0), stop=(tt_ == LT - 1)), r=[tk, zTt], w=[pz[s_ + ri]])
                zr_, zi_, kr_, ki_ = pz[s_], pz[s_ + 1], kf[s_], kf[s_ + 1]
                y_ = yo[ft % 2]
                tt(k, "dve", wa[:], zr_[:, :], kr_[:], ALU.mult, [zr_, kr_], [wa])
                tt(k, "dve", wb_[:], zi_[:, :], ki_[:], ALU.mult, [zi_, ki_], [wb_])
                tt(k, "pool", y_[:, 0, :], wa[:], wb_[:], ALU.subtract, [wa, wb_], [y_])
                if ft == 0:
                    cp(k, "pool", r0t[0:1, 0, :], wa[0:1, :], [wa], [r0t])
                    cp(k, "pool", r0t[0:1, 1, :], wb_[0:1, :], [wb_], [r0t])
                tt(k, "dve", wa[:], zr_[:, :], ki_[:], ALU.mult, [zr_, ki_], [wa])
                tt(k, "dve", wb_[:], zi_[:, :], kr_[:], ALU.mult, [zi_, kr_], [wb_])
                tt(k, "pool", y_[:, 1, :], wa[:], wb_[:], ALU.add, [wa, wb_], [y_])
                if ft == 0:
                    cp(k, "pool", y_[0:1, :, :], r0t[0:1, :, :], [r0t, y_], [y_])
                k.dma(S['hY'][ft].rearrange("r p c -> p r c"), y_[:], r=[y_])
        if g.cut == 'h_fwd':
            return
        with g.c.phase() as ph:
            identf = ph.sb([128, 128], F32, "hidf")
            k.dma(identf[:], I['ident_f'][:, :], w=[identf])
            Y = ph.sb([128, LT, 2, 512], BF16, "hYs")
            for ft in range(LT):
                k.dma(Y[:, ft], S['hY'][ft].rearrange("r p c -> p r c"), w=[Y])
            cv = [ph.sb([128, Lh], F32, "hcv%d" % j) for j in range(4)]
            tis = [ph.sb([128, LT, 128], BF16, "hti%d" % j) for j in range(4)]
            py = [ph.ps([128, 512], F32, "hpy%d" % j) for j in range(2)]
            ptr = [ph.ps([128, 512], F32, "hptr%d" % j) for j in range(2)]
            yt = [ph.sb([128, 512], F32, "hyt%d" % j) for j in range(2)]
            qs4 = py + ptr
            for tt_ in range(LT):
                s_ = (tt_ % 2) * 2
                for ri in range(2):
                    ti = tis[s_ + ri]
                    k.dma(ti[:], I['hy_I%s%s' % ("re" if ri == 0 else "im", tg)][tt_], w=[ti])
                for c4 in range(4):
                    for ri in range(2):
                        ti = tis[s_ + ri]
                        for ft in range(LT):
                            q_ = qs4[c4]
                            k.op("pe", lambda e, q_=q_, ti=ti, ft=ft, ri=ri, c4=c4: e.matmul(q_[:, 0:128], lhsT=Y[:, ft, ri, c4 * 128:(c4 + 1) * 128], rhs=ti[:, ft, :],
                                                                                     start=(ri == 0 and ft == 0), stop=(ri == 1 and ft == LT - 1)), r=[ti, Y], w=[q_])
                    cp(k, k.ev(), cv[c4][:, tt_ * 128:(tt_ + 1) * 128], qs4[c4][:, 0:128], [qs4[c4]], [cv[c4]])
            if g.cut in ('h_inv1', 'h_inv2'):
                return
            hb_ = ph.sb([128, 4, 2], F32, "hbias")
            k.dma(hb_[:], I['hy_bias4'][i], w=[hb_])
            zf = ph.sb([128, Lh], F32, "hzf2"); gf = ph.sb([128, Lh], F32, "hgf"); ob = ph.sb([128, Lh], BF16, "hob")
            for c4 in range(4):
                k.dma(zf[:], zsrc[c4 * 128:(c4 + 1) * 128, :], w=[zf])
                k.dma(gf[:], S['hxs'][(o + 1) * 512 + c4 * 128:(o + 1) * 512 + (c4 + 1) * 128, 0:Lh], w=[gf])
                k.op("dve", lambda e, c4=c4: e.scalar_tensor_tensor(out=zf[:], in0=zf[:], scalar=hb_[:, c4, o:o + 1], in1=cv[c4][:], op0=ALU.mult, op1=ALU.add), r=[zf, hb_, cv[c4]], w=[zf])
                tt(k, "pool", zf[:], zf[:], gf[:], ALU.mult, [zf, gf], [zf])
                if o == 0:
                    k.dma(S['hz1'][c4 * 128:(c4 + 1) * 128, 0:Lh], zf[:], r=[zf])
                else:
                    g0 = FM['hy_g'][0]
                    k.dma(gf[:], S['pfm'][g0 + c4 * 128:g0 + (c4 + 1) * 128, tok0:tok0 + Lh], w=[gf])
                    act(k, gf[:], gf[:], AF.Silu, [gf], [gf])
                    tt(k, "dve", ob[:], zf[:], gf[:], ALU.mult, [zf, gf], [ob])
                    k.dma(S['y'][512 + c4 * 128:512 + (c4 + 1) * 128, tok0:tok0 + Lh], ob[:], r=[ob])


def phase_outproj(g, i, last):
    k, I, S, L, CTX, T = g.k, g.I, g.S, g.L, g.CTX, g.T
    with g.c.phase() as ph:
        wo = ph.sb([128, NK, D], BF16, "wo")
        k.dma(wo[:], S['wob'].rearrange("(k p) c -> p k c", p=128), w=[wo])
        gts = [load_bc(g, ph, "gt%d" % r, S['mod'][r:r + 1, 2 * D:3 * D], D) for r in range(2)]
        fnw = load_bc(g, ph, "fnw", I['final_norm_w'][0:1, :], D) if last else None
        yT = [ph.sb([128, NK, 128], BF16, "yT%d" % j) for j in range(2)]
        xr = [ph.sb([128, D], F32, "xr%d" % j) for j in range(2)]
        xn = [ph.sb([128, D], F32, "xn%d" % j) for j in range(2)]
        tmp = ph.sb([128, 512], F32, "otmp")
        junk = ph.sb([128, D], BF16, "ojunk")
        ss = ph.sb([128, 1], F32, "oss")
        epsb = ph.sb([128, 1], F32, "oepsb")
        k.op("pool", lambda e: e.memset(epsb[:], EPS), w=[epsb])
        po = [ph.ps([128, 512], F32, "opo%d" % j) for j in range(4)]
        yv = S['y'].rearrange("(k p) t -> p k t", p=128)
        nb = 0
        npo = 0
        for tok in range(0 if not last else CTX, T, 128):
            isctx = tok < CTX
            y_ = yT[nb % 2]; x_ = xr[nb % 2]; o_ = xn[nb % 2]; nb += 1
            k.dma(y_[:], yv[:, :, tok:tok + 128], w=[y_])
            if i == 0:
                src = I['ctx'][tok:tok + 128, :] if isctx else I['x'][tok - CTX:tok - CTX + 128, :]
            else:
                src = S['xc1'][tok:tok + 128, :] if isctx else S['x1'][tok - CTX:tok - CTX + 128, :]
            k.dma(x_[:], src, w=[x_])
            gt = gts[1 if isctx else 0]
            for n in range(4):
                p_ = po[npo % 4]; npo += 1
                for kk in range(NK):
                    k.op("pe", lambda e, p_=p_, kk=kk, n=n, y_=y_: e.matmul(p_[:, :], lhsT=y_[:, kk, :], rhs=wo[:, kk, n * 512:(n + 1) * 512], start=(kk == 0), stop=(kk == NK - 1)), r=[y_, wo], w=[p_])
                cs = slice(n * 512, (n + 1) * 512)
                tt(k, "dve", tmp[:], p_[:, :], gt[:, cs], ALU.mult, [p_, gt], [tmp])
                tt(k, "pool", o_[:, cs], tmp[:], x_[:, cs], ALU.add, [tmp, x_], [o_])
            if last:
                k.op("act", lambda e, o_=o_: e.activation(out=junk[:], in_=o_[:], func=AF.Square, accum_out=ss[:]), r=[o_], w=[junk, ss])
                rsqrt_inplace(k, ss[:], ss, epsb[:, 0:1], epsb, scale=1.0 / D)
                k.op("dve", lambda e, o_=o_: e.scalar_tensor_tensor(out=o_[:], in0=o_[:], scalar=ss[:, 0:1], in1=fnw[:], op0=ALU.mult, op1=ALU.mult), r=[o_, ss, fnw], w=[o_])
                k.dma(g.out[tok - CTX:tok - CTX + 128, :], o_[:], r=[o_])
            elif isctx:
                k.dma(S['xc1'][tok:tok + 128, :], o_[:], r=[o_])
            else:
                k.dma(S['x1'][tok - CTX:tok - CTX + 128, :], o_[:], r=[o_])

def s5_prep(inp, T):
    m = {}
    DEPTH = inp['s5_a_re'].shape[0]
    def st(a):
        return np.ascontiguousarray(a.reshape(DEPTH, 2, 16, 2, 64).transpose(0, 3, 4, 1, 2).reshape(DEPTH, 128, 32))
    m['s5_are'] = st(inp['s5_a_re']); m['s5_aim'] = st(inp['s5_a_im'])
    ldt = np.broadcast_to(inp['s5_log_dt'][..., None], (DEPTH, 2, 32, 64))
    m['s5_ldt'] = st(np.ascontiguousarray(ldt))
    m['s5_d32'] = np.ascontiguousarray(inp['s5_d'].reshape(DEPTH, 16, 32).transpose(0, 2, 1))
    def Bl(B):
        o = np.zeros((DEPTH, 2, 16, 32, 128), np.float32)
        Br = B.reshape(DEPTH, 2, 16, 2, 64, 16)
        for g2 in range(2):
            o[:, :, :, g2 * 16:(g2 + 1) * 16, g2 * 64:(g2 + 1) * 64] = Br[:, :, :, g2].transpose(0, 1, 2, 4, 3)
        return o
    m['s5_Bre'] = Bl(inp['s5_b_re']); m['s5_Bim'] = Bl(inp['s5_b_im'])
    def Cl(C):
        o = np.zeros((DEPTH, 2, 16, 128, 32), np.float32)
        Cr = C.reshape(DEPTH, 2, 16, 2, 16, 64)
        for g2 in range(2):
            o[:, :, :, g2 * 64:(g2 + 1) * 64, g2 * 16:(g2 + 1) * 16] = Cr[:, :, :, g2].transpose(0, 1, 2, 4, 3)
        return o
    m['s5_Cre'] = Cl(inp['s5_c_re']); m['s5_Cim'] = Cl(inp['s5_c_im'])
    m['s5_glu_w'] = inp['s5_glu_w']
    m['s5_glu_b4'] = np.ascontiguousarray(inp['s5_glu_b'].reshape(DEPTH, 4, 128).transpose(0, 2, 1))
    m['tvals'] = np.ascontiguousarray(np.broadcast_to(np.arange(T, dtype=np.float32)[None], (128, T)))
    return m


def attn_prep(inp, L, CTX, GW):
    T = L + CTX
    m = {}
    DEPTH = inp['ret_norm_w'].shape[0]
    for C in (128, 64):
        a = np.ones((128, T + 1), np.float32); a[:, ::C] = 0.0
        m['m01_%d' % C] = a
    s = np.arange(128)
    m['maskF'] = (s[:, None] <= s[None, :]).astype(np.float32)
    m['maskB'] = (s[:, None] >= s[None, :]).astype(np.float32)
    BASE = 10000.0
    inv_c = BASE ** (-np.arange(64, dtype=np.float32) / 64)
    ang_c = np.arange(CTX, dtype=np.float32)[:, None] * inv_c
    inv = BASE ** (-np.arange(32, dtype=np.float32) / 32)
    j = np.arange(L)
    ang_l = np.concatenate([(j // GW).astype(np.float32)[:, None] * inv, (j % GW).astype(np.float32)[:, None] * inv], -1)
    ang = np.concatenate([ang_c, ang_l], 0).astype(np.float32)
    cos, sin = np.cos(ang).T, np.sin(ang).T
    m['ropeC'] = np.ascontiguousarray(np.concatenate([cos, cos], 0)).astype(np.float32)
    m['ropeS'] = np.ascontiguousarray(np.concatenate([-sin, sin], 0)).astype(np.float32)
    m['ret_nw'] = np.ascontiguousarray(inp['ret_norm_w'].reshape(DEPTH, 4, 128).transpose(0, 2, 1))
    m['gla_nw'] = np.ascontiguousarray(inp['gla_norm_w'].reshape(DEPTH, 4, 128).transpose(0, 2, 1))
    gw = np.zeros((DEPTH, 2, 32, 2, 128), np.float32)
    for tl in range(2):
        for dr in range(2):
            gw[:, tl, 16 * dr:16 * dr + 16, dr, :] = inp['gla_gate_w'][:, dr, :, tl * 128:(tl + 1) * 128]
    m['gla_gw'] = gw
    m['gla_gb'] = np.ascontiguousarray(inp['gla_gate_b'].reshape(DEPTH, 2, 2, 128).transpose(0, 2, 3, 1))
    return m


def hy_tables(Lh):
    N = 2 * Lh
    t = np.linspace(0.0, 1.0, Lh, dtype=np.float32)[:, None]
    wpos = 2.0 * math.pi * np.arange(Lh, dtype=np.float32)[:, None] / Lh
    f = np.linspace(1e-4, 15, 16, dtype=np.float32)[None, :]
    z = np.concatenate([t, np.cos(f * wpos), -np.sin(f * wpos)], -1).astype(np.float32)
    zfull = np.zeros((N, 33), np.float32)
    zfull[:Lh] = z
    zfull[Lh + 1:] = z[1:][::-1]
    deltas = np.linspace(math.log(1e-2) / 1.5, math.log(1e-2) / 0.3, 512, dtype=np.float32)
    dec = np.exp(-t * np.abs(deltas)).astype(np.float32)
    dfull = np.zeros((N, 512), np.float32)
    dfull[:Lh] = dec
    dfull[Lh + 1:] = dec[1:][::-1]
    n = np.arange(N, dtype=np.float64)[:, None]
    fr = np.arange(Lh, dtype=np.float64)[None, :]
    ang = 2.0 * math.pi * ((n * fr) % N) / N
    Kre = np.cos(ang); Kim = -np.sin(ang)
    Kim[:, 0] = np.where(np.arange(N) % 2 == 0, 1.0, -1.0)
    def tile(A, KT, MT):
        return np.ascontiguousarray(A.reshape(KT, 128, MT, 128).transpose(2, 1, 0, 3)).astype(ml_dtypes.bfloat16)
    m = {}
    m['z'] = np.ascontiguousarray(zfull.T)
    m['dec'] = dfull
    m['Kre'] = tile(Kre, N // 128, Lh // 128); m['Kim'] = tile(Kim, N // 128, Lh // 128)
    ff = np.arange(Lh, dtype=np.float64)[:, None]; tt = np.arange(Lh, dtype=np.float64)[None, :]
    ang2 = 2.0 * math.pi * ((ff * tt) % N) / N
    Ire = np.cos(ang2); Iim = -np.sin(ang2)
    Iim[0, :] = np.where(np.arange(Lh) % 2 == 0, 1.0, -1.0)
    m['Ire'] = tile(Ire, Lh // 128, Lh // 128); m['Iim'] = tile(Iim, Lh // 128, Lh // 128)
    return m


def hy_prep(inp, L, CTX):
    m = {}
    DEPTH = inp['hy_w1'].shape[0]
    for tg, Lh in (('C', CTX), ('L', L)):
        tb = hy_tables(Lh)
        for kk, v in tb.items():
            m['hy_' + kk + tg] = v
    for nm in ('hy_w1', 'hy_w2', 'hy_w3', 'hy_w4'):
        m[nm] = inp[nm]
    m['hy_bf'] = np.ascontiguousarray(np.stack([inp['hy_b1'], inp['hy_f1'], inp['hy_b2'], inp['hy_f2'], inp['hy_b3'], inp['hy_f3']], -1))
    cw = np.concatenate([inp['hy_conv_w'], inp['hy_conv_b'][:, None, :]], 1)
    m['hy_cw'] = np.ascontiguousarray(cw.reshape(DEPTH, 4, 12, 128).transpose(0, 3, 2, 1))
    m['hy_bias4'] = np.ascontiguousarray(inp['hy_bias'].reshape(DEPTH, 2, 4, 128).transpose(0, 3, 2, 1))
    return m


_NC_CACHE = {}


def kernel(**inputs):
    inp = {k_: np.asarray(v) for k_, v in inputs.items()}
    L, CTX, GW = 4096, 256, 64
    T = L + CTX
    shared = {}
    for nm in ('norm_w', 'ada_w', 'ada_b', 'w_in', 'w_out'):
        shared[nm] = np.ascontiguousarray(inp[nm], dtype=np.float32)
    shared['final_norm_w'] = np.ascontiguousarray(inp['final_norm_w'][None], dtype=np.float32)
    shared['ident_bf'] = np.eye(128, dtype=np.float32).astype(ml_dtypes.bfloat16)
    shared['ident_f'] = np.eye(128, dtype=np.float32)
    shared.update(s5_prep(inp, T))
    shared.update(attn_prep(inp, L, CTX, GW))
    shared.update(hy_prep(inp, L, CTX))
    ncores = 4
    in_maps = []
    for core in range(ncores):
        b = core % 4
        m = dict(shared)
        m['x'] = np.ascontiguousarray(inp['x'][b], dtype=np.float32)
        m['ctx'] = np.ascontiguousarray(inp['ctx'][b], dtype=np.float32)
        cc = np.stack([inp['c'][b].reshape(16, 128).T, inp['c_ctx'].reshape(16, 128).T], axis=-1)
        m['cc'] = np.ascontiguousarray(cc, dtype=np.float32)
        in_maps.append(m)
    if 'nc' not in _NC_CACHE:
        _NC_CACHE['nc'] = build(L=L, CTX=CTX)
    nc = _NC_CACHE['nc']
    res = run_bass_kernel_spmd(nc, in_maps, core_ids=list(range(ncores)))
    out = np.stack([np.asarray(res.results[b]['out'], dtype=np.float32) for b in range(4)], axis=0)
    return out
```
